# Optimizing a Trainium2 kernel written in Bass

```python
import math
import jax, jax.numpy as jnp
from jax import lax
import numpy as np

D_MODEL = 2048
BATCH = 4
SEQ = 2048
DEPTH = 2
DEC_BATCH = 128
DEC_SEQ = 1
PAST_LEN = 16384
PAGE_SIZE = 128

PLE_DIM = 256
D_A = D_MODEL // 2
N_HEADS_A = 4
HEAD_DIM_A = D_A // N_HEADS_A
CONV_W = 4
MLSTM_CHUNK = 64
D_B = D_MODEL // 2
S5_GROUP = 16
N_GROUPS = D_B // S5_GROUP
S5_STATE = 64
D_FF = 5504
N_EXPERTS = 8
TOP_K = 2
N_DENSE = (DEPTH + 1) // 2
N_MOE = DEPTH // 2
EPS = 1e-6
PROJ_SIZES = (D_A, D_A, 2 * N_HEADS_A, D_B, D_MODEL, D_MODEL)
D_IN_PROJ = sum(PROJ_SIZES)

kernel_name = 'hybrid_mlstm_s5_gated_decode_step'

F32 = jnp.float32


def rmsnorm(x, g):
    x = x.astype(F32)
    return x * lax.rsqrt(jnp.mean(x * x, axis=-1, keepdims=True) + EPS) * g.astype(F32)


def split_proj(proj):
    cuts = [int(c) for c in np.cumsum(PROJ_SIZES)[:-1]]
    return jnp.split(proj, cuts, axis=-1)


def mlstm_chunkwise(q, k, v, log_i, log_f, c0, n0, m0):
    bsz, nh, seq, dh = q.shape
    chunk = math.gcd(seq, MLSTM_CHUNK)
    nc = seq // chunk

    def to_chunks(t):
        return jnp.moveaxis(t.reshape(t.shape[:2] + (nc, chunk) + t.shape[3:]), 2, 0)

    causal = jnp.tril(jnp.ones((chunk, chunk), dtype=bool))

    def step(carry, inp):
        c, n, m = carry
        qc, kc, vc, lic, lfc = inp
        b = jnp.cumsum(lfc, axis=-1)
        d = jnp.where(causal, b[..., :, None] - b[..., None, :] + lic[..., None, :], -jnp.inf)
        inter = b + m[..., None]
        m_t = jnp.maximum(inter, jnp.max(d, axis=-1))
        w_inter = jnp.exp(inter - m_t)
        s = jnp.einsum('bhtd,bhsd->bhts', qc, kc) * jnp.exp(d - m_t[..., None])
        num = w_inter[..., None] * jnp.einsum('bhtd,bhde->bhte', qc, c) + jnp.einsum('bhts,bhse->bhte', s, vc)
        den = w_inter * jnp.einsum('bhtd,bhd->bht', qc, n) + jnp.sum(s, axis=-1)
        h = num / jnp.maximum(jnp.abs(den), jnp.exp(-m_t))[..., None]
        b_last = b[..., -1]
        g = b_last[..., None] - b + lic
        m_new = jnp.maximum(b_last + m, jnp.max(g, axis=-1))
        decay = jnp.exp(b_last + m - m_new)
        wg = jnp.exp(g - m_new[..., None])
        c_new = decay[..., None, None] * c + jnp.einsum('bhs,bhsd,bhse->bhde', wg, kc, vc)
        n_new = decay[..., None] * n + jnp.einsum('bhs,bhsd->bhd', wg, kc)
        return (c_new, n_new, m_new), h

    (c, n, m), hs = lax.scan(step, (c0, n0, m0), tuple(to_chunks(t) for t in (q, k, v, log_i, log_f)))
    h = jnp.moveaxis(hs, 0, 2).reshape(bsz, nh, seq, dh)
    return h, (c, n, m)


def mlstm_branch(u, o_pre, if_pre, conv0, c0, n0, m0, conv_w, conv_b, w_q, w_k, w_v, b_i, b_f, g_head, skip, w_proj):
    bsz, seq, _ = u.shape
    upad = jnp.concatenate([conv0.astype(F32), u], axis=1)
    conv = conv_b.astype(F32) + sum(upad[:, j:j + seq] * conv_w[j] for j in range(CONV_W))
    c = jax.nn.silu(conv)
    new_conv = upad[:, upad.shape[1] - (CONV_W - 1):]
    ch = c.reshape(bsz, seq, N_HEADS_A, HEAD_DIM_A)
    uh = u.reshape(bsz, seq, N_HEADS_A, HEAD_DIM_A)
    q = jnp.einsum('bshd,hde->bhse', ch, w_q)
    k = jnp.einsum('bshd,hde->bhse', ch, w_k) * (HEAD_DIM_A ** -0.5)
    v = jnp.einsum('bshd,hde->bhse', uh, w_v)
    log_i = jnp.moveaxis(if_pre[..., :N_HEADS_A] + b_i, -1, 1)
    log_f = jax.nn.log_sigmoid(jnp.moveaxis(if_pre[..., N_HEADS_A:] + b_f, -1, 1))
    h, (c_new, n_new, m_new) = mlstm_chunkwise(q, k, v, log_i, log_f, c0, n0, m0)
    h = rmsnorm(jnp.moveaxis(h, 1, 2), g_head).reshape(bsz, seq, D_A)
    h = (h + skip * c) * jax.nn.sigmoid(o_pre)
    return h @ w_proj, (c_new, n_new, m_new, new_conv)


def s5_combine(e1, e2):
    ar1, ai1, br1, bi1 = e1
    ar2, ai2, br2, bi2 = e2
    return (ar2 * ar1 - ai2 * ai1, ar2 * ai1 + ai2 * ar1,
            ar2 * br1 - ai2 * bi1 + br2, ar2 * bi1 + ai2 * br1 + bi2)


def s5_branch(u, x0_re, x0_im, log_dt, a_re, a_im, b_re, b_im, c_re, c_im, d_skip, w_glu):
    bsz, seq, _ = u.shape
    ug = u.reshape(bsz, seq, N_GROUPS, S5_GROUP)
    dt = jnp.exp(log_dt.astype(F32))[:, None]
    a_re = a_re.astype(F32)
    a_im = a_im.astype(F32)
    mag = jnp.exp(a_re * dt)
    ang = a_im * dt
    ab_re = mag * jnp.cos(ang)
    ab_im = mag * jnp.sin(ang)
    den = a_re * a_re + a_im * a_im
    nr = ab_re - 1.0
    ni = ab_im
    k_re = (nr * a_re + ni * a_im) / den
    k_im = (ni * a_re - nr * a_im) / den
    b_re = b_re.astype(F32)
    b_im = b_im.astype(F32)
    bb_re = k_re[..., None] * b_re - k_im[..., None] * b_im
    bb_im = k_re[..., None] * b_im + k_im[..., None] * b_re
    bu_re = jnp.einsum('bsgc,gpc->bsgp', ug, bb_re)
    bu_im = jnp.einsum('bsgc,gpc->bsgp', ug, bb_im)
    bu_re = bu_re.at[:, 0].add(ab_re * x0_re - ab_im * x0_im)
    bu_im = bu_im.at[:, 0].add(ab_re * x0_im + ab_im * x0_re)
    ar = jnp.broadcast_to(ab_re, bu_re.shape)
    ai = jnp.broadcast_to(ab_im, bu_im.shape)
    _, _, xr, xi = lax.associative_scan(s5_combine, (ar, ai, bu_re, bu_im), axis=1)
    y = (jnp.einsum('bsgp,gcp->bsgc', xr, c_re.astype(F32)) - jnp.einsum('bsgp,gcp->bsgc', xi, c_im.astype(F32))
         + d_skip.astype(F32).reshape(N_GROUPS, S5_GROUP) * ug)
    y = jax.nn.gelu(y.reshape(bsz, seq, D_B))
    val, gate = jnp.split(y @ w_glu, 2, axis=-1)
    return val * jax.nn.sigmoid(gate), (xr[:, -1], xi[:, -1])


def swiglu(x, w_gate, w_up, w_down):
    return (jax.nn.silu(x @ w_gate) * (x @ w_up)) @ w_down


def moe_swiglu(x, w_router, b_router, w_gate, w_up, w_down):
    bsz, seq, d = x.shape
    xt = x.reshape(bsz * seq, d)
    logits = (xt @ w_router).astype(F32) + b_router.astype(F32)
    top_v, top_e = lax.top_k(logits, TOP_K)
    gates = jax.nn.softmax(top_v, axis=-1)
    flat_e = top_e.reshape(-1)
    order = jnp.argsort(flat_e)
    tok = order // TOP_K
    sizes = jnp.bincount(flat_e, length=N_EXPERTS).astype(jnp.int32)
    xs = xt[tok].astype(w_gate.dtype)
    hid = jax.nn.silu(lax.ragged_dot(xs, w_gate, sizes)) * lax.ragged_dot(xs, w_up, sizes)
    ys = lax.ragged_dot(hid.astype(w_down.dtype), w_down, sizes).astype(F32)
    ys = ys * gates.reshape(-1)[order][:, None]
    return jnp.zeros((bsz * seq, d), F32).at[tok].add(ys).reshape(bsz, seq, d)


def trunk(x, p, c0, n0, m0, conv0, sre0, sim0, w):
    h = x.astype(F32)
    outs = [[] for _ in range(6)]
    for i in range(DEPTH):
        xn = rmsnorm(h, w['g_mix'][i])
        u_a, o_a, if_a, u_b, gate_a, gate_b = split_proj(xn @ w['w_in'][i])
        a_out, st_a = mlstm_branch(u_a, o_a, if_a, conv0[i], c0[i].astype(F32), n0[i].astype(F32), m0[i].astype(F32),
                                   w['conv_w'][i], w['conv_b'][i], w['w_q'][i], w['w_k'][i], w['w_v'][i],
                                   w['b_i'][i], w['b_f'][i], w['g_head'][i], w['skip_a'][i], w['w_proj_a'][i])
        b_out, st_b = s5_branch(u_b, sre0[i].astype(F32), sim0[i].astype(F32), w['s5_log_dt'][i], w['s5_A_re'][i],
                                w['s5_A_im'][i], w['s5_B_re'][i], w['s5_B_im'][i], w['s5_C_re'][i], w['s5_C_im'][i],
                                w['s5_D'][i], w['w_glu_b'][i])
        h = h + (jax.nn.sigmoid(gate_a) * a_out + jax.nn.sigmoid(gate_b) * b_out) @ w['w_out'][i]
        hn = rmsnorm(h, w['g_ffn'][i])
        j = i // 2
        if i % 2 == 0:
            h = h + swiglu(hn, w['w_ff_gate'][j], w['w_ff_up'][j], w['w_ff_down'][j])
        else:
            h = h + moe_swiglu(hn, w['w_router'][j], w['b_router'][j], w['w_moe_gate'][j], w['w_moe_up'][j], w['w_moe_down'][j])
        h = h + (p[i].astype(F32) @ w['w_ple'][i]) * jax.nn.sigmoid(rmsnorm(h, w['g_ple'][i]) @ w['w_pg'][i])
        for lst, s in zip(outs, st_a + st_b):
            lst.append(s)
    y = rmsnorm(h, w['g_final'])
    return y, [jnp.stack(lst) for lst in outs]


def setup_inputs(seed: int = 0) -> dict:
    key = jax.random.key(seed)
    ks = iter(jax.random.split(key, 64))

    def nrm(shape, scale):
        return jax.random.normal(next(ks), shape, F32) * scale

    def gain(shape):
        return 1.0 + nrm(shape, 0.02)

    d = D_MODEL
    out = {
        'x_prompt': nrm((BATCH, SEQ, d), 1.0),
        'x_sample': nrm((DEC_BATCH, DEC_SEQ, d), 1.0),
        'p_prompt': nrm((DEPTH, BATCH, SEQ, PLE_DIM), 1.0),
        'p_sample': nrm((DEPTH, DEC_BATCH, DEC_SEQ, PLE_DIM), 1.0),
        'state_mlstm_C': nrm((DEPTH, DEC_BATCH, N_HEADS_A, HEAD_DIM_A, HEAD_DIM_A), 0.1),
        'state_mlstm_n': nrm((DEPTH, DEC_BATCH, N_HEADS_A, HEAD_DIM_A), 0.1),
        'state_mlstm_m': nrm((DEPTH, DEC_BATCH, N_HEADS_A), 0.5),
        'state_mlstm_conv': nrm((DEPTH, DEC_BATCH, CONV_W - 1, D_A), 1.0),
        'state_s5_re': nrm((DEPTH, DEC_BATCH, N_GROUPS, S5_STATE), 0.1),
        'state_s5_im': nrm((DEPTH, DEC_BATCH, N_GROUPS, S5_STATE), 0.1),
        'g_mix': gain((DEPTH, d)),
        'w_in': nrm((DEPTH, d, D_IN_PROJ), d ** -0.5),
        'conv_w': nrm((DEPTH, CONV_W, D_A), CONV_W ** -0.5),
        'conv_b': nrm((DEPTH, D_A), 0.01),
        'w_q': nrm((DEPTH, N_HEADS_A, HEAD_DIM_A, HEAD_DIM_A), HEAD_DIM_A ** -0.5),
        'w_k': nrm((DEPTH, N_HEADS_A, HEAD_DIM_A, HEAD_DIM_A), HEAD_DIM_A ** -0.5),
        'w_v': nrm((DEPTH, N_HEADS_A, HEAD_DIM_A, HEAD_DIM_A), HEAD_DIM_A ** -0.5),
        'b_i': nrm((DEPTH, N_HEADS_A), 0.1),
        'b_f': jnp.linspace(3.0, 6.0, N_HEADS_A, dtype=F32)[None, :] + nrm((DEPTH, N_HEADS_A), 0.1),
        'g_head': gain((DEPTH, N_HEADS_A, HEAD_DIM_A)),
        'skip_a': gain((DEPTH, D_A)),
        'w_proj_a': nrm((DEPTH, D_A, d), D_A ** -0.5),
        's5_log_dt': jax.random.uniform(next(ks), (DEPTH, N_GROUPS), F32, math.log(1e-3), math.log(1e-1)),
        's5_A_re': -0.5 + nrm((DEPTH, N_GROUPS, S5_STATE), 0.01),
        's5_A_im': math.pi * jnp.arange(S5_STATE, dtype=F32) + nrm((DEPTH, N_GROUPS, S5_STATE), 0.01),
        's5_B_re': nrm((DEPTH, N_GROUPS, S5_STATE, S5_GROUP), (2 * S5_GROUP) ** -0.5),
        's5_B_im': nrm((DEPTH, N_GROUPS, S5_STATE, S5_GROUP), (2 * S5_GROUP) ** -0.5),
        's5_C_re': nrm((DEPTH, N_GROUPS, S5_GROUP, S5_STATE), S5_STATE ** -0.5),
        's5_C_im': nrm((DEPTH, N_GROUPS, S5_GROUP, S5_STATE), S5_STATE ** -0.5),
        's5_D': nrm((DEPTH, D_B), 1.0),
        'w_glu_b': nrm((DEPTH, D_B, 2 * d), D_B ** -0.5),
        'w_out': nrm((DEPTH, d, d), d ** -0.5),
        'g_ffn': gain((DEPTH, d)),
        'w_ff_gate': nrm((N_DENSE, d, D_FF), d ** -0.5),
        'w_ff_up': nrm((N_DENSE, d, D_FF), d ** -0.5),
        'w_ff_down': nrm((N_DENSE, D_FF, d), D_FF ** -0.5),
        'w_router': nrm((N_MOE, d, N_EXPERTS), d ** -0.5),
        'b_router': nrm((N_MOE, N_EXPERTS), 0.01),
        'w_moe_gate': nrm((N_MOE, N_EXPERTS, d, D_FF), d ** -0.5),
        'w_moe_up': nrm((N_MOE, N_EXPERTS, d, D_FF), d ** -0.5),
        'w_moe_down': nrm((N_MOE, N_EXPERTS, D_FF, d), D_FF ** -0.5),
        'g_ple': gain((DEPTH, d)),
        'w_ple': nrm((DEPTH, PLE_DIM, d), PLE_DIM ** -0.5),
        'w_pg': nrm((DEPTH, d, d), d ** -0.5),
        'g_final': gain((d,)),
    }
    return out


def reference(x_prompt, x_sample, p_prompt, p_sample, state_mlstm_C, state_mlstm_n, state_mlstm_m, state_mlstm_conv,
              state_s5_re, state_s5_im, g_mix, w_in, conv_w, conv_b, w_q, w_k, w_v, b_i, b_f, g_head, skip_a, w_proj_a,
              s5_log_dt, s5_A_re, s5_A_im, s5_B_re, s5_B_im, s5_C_re, s5_C_im, s5_D, w_glu_b, w_out, g_ffn,
              w_ff_gate, w_ff_up, w_ff_down, w_router, b_router, w_moe_gate, w_moe_up, w_moe_down,
              g_ple, w_ple, w_pg, g_final):
    w = dict(g_mix=g_mix, w_in=w_in, conv_w=conv_w, conv_b=conv_b, w_q=w_q, w_k=w_k, w_v=w_v, b_i=b_i, b_f=b_f,
             g_head=g_head, skip_a=skip_a, w_proj_a=w_proj_a, s5_log_dt=s5_log_dt, s5_A_re=s5_A_re, s5_A_im=s5_A_im,
             s5_B_re=s5_B_re, s5_B_im=s5_B_im, s5_C_re=s5_C_re, s5_C_im=s5_C_im, s5_D=s5_D, w_glu_b=w_glu_b,
             w_out=w_out, g_ffn=g_ffn, w_ff_gate=w_ff_gate, w_ff_up=w_ff_up, w_ff_down=w_ff_down,
             w_router=w_router, b_router=b_router, w_moe_gate=w_moe_gate, w_moe_up=w_moe_up, w_moe_down=w_moe_down,
             g_ple=g_ple, w_ple=w_ple, w_pg=w_pg, g_final=g_final)
    zc = jnp.zeros((DEPTH, BATCH, N_HEADS_A, HEAD_DIM_A, HEAD_DIM_A), F32)
    zn = jnp.zeros((DEPTH, BATCH, N_HEADS_A, HEAD_DIM_A), F32)
    zm = jnp.zeros((DEPTH, BATCH, N_HEADS_A), F32)
    zconv = jnp.zeros((DEPTH, BATCH, CONV_W - 1, D_A), F32)
    zs = jnp.zeros((DEPTH, BATCH, N_GROUPS, S5_STATE), F32)
    y_prompt, st_p = trunk(x_prompt, p_prompt, zc, zn, zm, zconv, zs, zs, w)
    c_p, n_p, m_p, conv_p, s5re_p, s5im_p = st_p
    y_sample, st_s = trunk(x_sample, p_sample, state_mlstm_C, state_mlstm_n, state_mlstm_m, state_mlstm_conv,
                           state_s5_re, state_s5_im, w)
    c_s, n_s, m_s, conv_s, s5re_s, s5im_s = st_s
    return (y_prompt, y_sample, c_p, n_p, m_p, conv_p, s5re_p, s5im_p, c_s, n_s, m_s, conv_s, s5re_s, s5im_s)
```

```python
import contextlib
import numpy as np
import concourse.bass as bass
import concourse.mybir as mybir
from concourse.bass_utils import run_bass_kernel_spmd

AF = mybir.ActivationFunctionType
ALU = mybir.AluOpType
AX = mybir.AxisListType
F32 = mybir.dt.float32
BF16 = mybir.dt.bfloat16

D = 2048
SEQ = 2048
NB = 4
TBP = 512
NS = 16
TB = TBP + NS
DEPTH = 2
D_A = 1024
EPS = 1e-6
NCORES = 8

ENGS = ("pe", "act", "dve", "sp", "pool")
DMA_ENGS = ("sp", "pool")
NDSEM = {"sp": 12, "pool": 16}
EPOCH = 4096


class Buf:
    def __init__(self, name, nslots=1):
        self.name = name
        self.n = nslots
        self.w = [None] * nslots
        self.r = [[] for _ in range(nslots)]


class Ins:
    __slots__ = ("eng", "idx", "fn", "deps", "dma", "signal", "dn")

    def __init__(self, eng, idx, fn, deps, dma):
        self.eng = eng
        self.idx = idx
        self.fn = fn
        self.deps = deps
        self.dma = dma
        self.signal = False
        self.dn = -1


class Prog:
    def __init__(self):
        self.q = {e: [] for e in ENGS}
        self.ndma = {e: 0 for e in DMA_ENGS}

    def op(self, eng, fn, reads=(), writes=(), dma=False):
        deps = set()
        for (b, lo, hi) in reads:
            for s in range(lo, hi):
                if b.w[s] is not None:
                    deps.add(b.w[s])
        for (b, lo, hi) in writes:
            for s in range(lo, hi):
                if b.w[s] is not None:
                    deps.add(b.w[s])
                deps.update(b.r[s])
        idx = len(self.q[eng])
        me = (eng, idx)
        deps.discard(me)
        ins = Ins(eng, idx, fn, deps, dma)
        if dma:
            ins.dn = self.ndma[eng]
            self.ndma[eng] += 1
        self.q[eng].append(ins)
        for (b, lo, hi) in reads:
            for s in range(lo, hi):
                b.r[s].append(me)
        for (b, lo, hi) in writes:
            for s in range(lo, hi):
                b.w[s] = me
                b.r[s] = []
        return ins

    def emit(self, nc):
        q = self.q
        for e in DMA_ENGS:
            K = NDSEM[e]
            lst = [i for i in q[e] if i.dma]
            for n, ins in enumerate(lst):
                if n >= K:
                    ins.deps.add((e, lst[n - K].idx))
        for e in ENGS:
            for ins in q[e]:
                for (pe, pi) in ins.deps:
                    p = q[pe][pi]
                    if not p.dma and not (pe == "pe" and e == "pe"):
                        p.signal = True
        cnt = {}
        for e in ENGS:
            c = 0
            arr = []
            for ins in q[e]:
                if ins.signal:
                    c += 1
                arr.append(c)
            cnt[e] = arr
        with contextlib.ExitStack() as st:
            csem = {e: [st.enter_context(nc.semaphore("c_%s%d" % (e, i))) for i in range(max(1, (cnt[e][-1] if cnt[e] else 0) // EPOCH + 1))]
                    for e in ENGS}
            dsem = {e: [st.enter_context(nc.semaphore("d_%s%d" % (e, i))) for i in range(NDSEM[e])]
                    for e in DMA_ENGS}
            block = st.enter_context(nc.Block())

            def run(e, eng):
                seen = {}
                for ins in q[e]:
                    need = {}
                    for (pe, pi) in ins.deps:
                        p = q[pe][pi]
                        if p.dma:
                            K = NDSEM[pe]
                            key = ("d", pe, p.dn % K)
                            val = 16 * (p.dn // K + 1)
                        else:
                            if pe == "pe" and e == "pe":
                                continue
                            c_ = cnt[pe][pi]
                            key = ("c", pe, (c_ - 1) // EPOCH)
                            val = (c_ - 1) % EPOCH + 1
                        if need.get(key, 0) < val:
                            need[key] = val
                    for key, val in need.items():
                        if seen.get(key, 0) >= val:
                            continue
                        seen[key] = val
                        sem = csem[key[1]][key[2]] if key[0] == "c" else dsem[key[1]][key[2]]
                        eng.wait_ge(sem, val)
                    if ins.fn is None:
                        continue
                    r = ins.fn(eng)
                    if ins.dma:
                        r.then_inc(dsem[e][ins.dn % NDSEM[e]], 16)
                    elif ins.signal:
                        r.then_inc(csem[e][(cnt[e][ins.idx] - 1) // EPOCH], 1)

            @block.tensor
            def _(eng):
                run("pe", eng)

            @block.scalar
            def _(eng):
                run("act", eng)

            @block.vector
            def _(eng):
                run("dve", eng)

            @block.sync
            def _(eng):
                run("sp", eng)

            @block.gpsimd
            def _(eng):
                run("pool", eng)


class TT:
    def __init__(self, t, name, nslots=1):
        self.t = t
        self.b = Buf(name, nslots)

    def s(self, lo=0, hi=None):
        return (self.b, lo, self.b.n if hi is None else hi)


class KB:
    def __init__(self, nc, st):
        self.nc = nc
        self.st = st
        self.P = Prog()
        self.dram = {}
        self._bank = 0
        self._ws = 0
        self.pinned = set()

    def din(self, name, shape):
        t = self.nc.dram_tensor(name, list(shape), F32, kind="ExternalInput")
        self.dram[name] = TT(t.ap(), name, 1)
        return self.dram[name]

    def dout(self, name, shape):
        t = self.nc.dram_tensor(name, list(shape), F32, kind="ExternalOutput")
        self.dram[name] = TT(t.ap(), name, 1)
        return self.dram[name]

    def sb(self, name, shape, dt, nslots=1):
        t = self.st.enter_context(self.nc.sbuf_tensor(name, list(shape), dt))
        return TT(t, name, nslots)

    def psum(self, name, shape, dt):
        t = self.st.enter_context(self.nc.psum_tensor(name, list(shape), dt))
        return TT(t, name, 1)

    def V(self, fn, r=(), w=()):
        return self.P.op("dve", fn, r, w)

    def A(self, fn, r=(), w=()):
        return self.P.op("act", fn, r, w)

    def M(self, fn, r=(), w=()):
        return self.P.op("pe", fn, r, w)

    def Dm(self, fn, r=(), w=()):
        return self.P.op("sp", fn, r, w, dma=True)

    def Wm(self, fn, r=(), w=()):
        return self.P.op("pool", fn, r, w, dma=True)

    def unpin(self, ws):
        self.pinned.discard(ws)

    def bank(self):
        b = self.banks[self._bank % len(self.banks)]
        self._bank += 1
        return b

    def wload(self, src_ap, pin=False):
        while True:
            ws = self.wslots[self._ws % len(self.wslots)]
            self._ws += 1
            if ws not in self.pinned:
                break
        if pin:
            self.pinned.add(ws)
        self.Wm(lambda e, o=ws.t, i=src_ap: e.dma_start(out=o[:, :], in_=i), w=[ws.s()])
        return ws


TWO_PI = 6.283185307179586
NLP = 72
LP_CW, LP_CB, LP_SKIP, LP_DSK, LP_BIF, LP_BRT = 0, 32, 40, 48, 56, 64
C_ID, C_TRI, C_JJ, C_M2, C_D16 = 0, 128, 256, 768, 776
NCONST = 1032
DFF_T = 43
RUN_CORES = None
STOP_AFTER = None


class VT:
    def __init__(self, t, buf, lo, hi):
        self.t = t
        self.b = buf
        self.lo = lo
        self.hi = hi

    def s(self, *a):
        return (self.b, self.lo, self.hi)


def build_program():
    nc = bass.Bass("TRN2", target_bir_lowering=False)
    with contextlib.ExitStack() as st:
        k = KB(nc, st)
        din = k.din

        def dout(name, shape, nslots=1):
            t = nc.dram_tensor(name, list(shape), F32, kind="ExternalOutput")
            tt = TT(t.ap(), name, nslots)
            k.dram[name] = tt
            return tt

        xTp = din("xTp", [D, SEQ]); xTs = din("xTs", [D, NS])
        pTp = din("pTp", [DEPTH, 256, SEQ]); pTs = din("pTs", [DEPTH, 256, NS])
        gains = din("gains", [128, 7 * 16])
        consts = din("consts", [128, NCONST])
        lp = din("lp", [DEPTH, 128, NLP])
        ghd = din("ghd", [DEPTH, 128, 1024])
        w_in_t = din("w_in_t", [DEPTH, 56, 128, 2048])
        w_if = din("w_if", [DEPTH, 128, 128])
        wqkv = din("wqkv", [DEPTH, 3, 128, 2048])
        w_proj_t = din("w_proj_t", [DEPTH, 8, 128, 2048])
        w_glu_t = din("w_glu_t", [DEPTH, 16, 128, 2048])
        w_out_t = din("w_out_t", [DEPTH, 16, 128, 2048])
        w_pg_t = din("w_pg_t", [DEPTH, 16, 128, 2048])
        w_ple_t = din("w_ple_t", [DEPTH, 2, 128, 2048])
        ffg = din("ffg", [DFF_T, 128, 2048]); ffu = din("ffu", [DFF_T, 128, 2048]); ffd = din("ffd", [DFF_T, 128, 2048])
        moeg = din("moeg", [8, DFF_T, 128, 2048]); moeu = din("moeu", [8, DFF_T, 128, 2048]); moed = din("moed", [8, DFF_T, 128, 2048])
        w_rt = din("w_rt", [128, 128])
        s5L1 = din("s5L1", [DEPTH, 128, 96]); s5L2 = din("s5L2", [DEPTH, 128, 1536])
        s5B2 = din("s5B2", [DEPTH, 128, 1024]); s5C1 = din("s5C1", [DEPTH, 128, 1024])
        stC = din("stC", [DEPTH, NS, 4, 256, 256]); stn = din("stn", [DEPTH, 128, 128]); stm = din("stm", [DEPTH, NS, 4])
        stconv = din("stconv", [DEPTH, 128, 8 * NS * 3]); sts5 = din("sts5", [DEPTH, 2, 128, 512])

        o_yTp = dout("o_yTp", [D, SEQ], 64); o_yTs = dout("o_yTs", [D, NS], 16)
        o_Cp = dout("o_Cp", [DEPTH, 4, 256, 256], 8); o_np = dout("o_np", [DEPTH, 128, 8], 2); o_mp = dout("o_mp", [DEPTH, 4], 2)
        o_convp = dout("o_convp", [DEPTH, D_A, 3], 2); o_s5p = dout("o_s5p", [DEPTH, 2, 128, 32], 4)
        o_Cs = dout("o_Cs", [DEPTH, NS, 4, 256, 256], 128); o_ns = dout("o_ns", [DEPTH, 128, 128], 2); o_ms = dout("o_ms", [DEPTH, NS, 4], 2)
        o_convs = dout("o_convs", [DEPTH, 128, 8 * NS * 3], 2); o_s5s = dout("o_s5s", [DEPTH, 2, 128, 512], 4)
        all_outs = [o_yTp, o_yTs, o_Cp, o_np, o_mp, o_convp, o_s5p, o_Cs, o_ns, o_ms, o_convs, o_s5s]

        h = k.sb("h", [128, 16, TB], F32, 16)
        xn = k.sb("xn", [128, 16, TB], BF16, 16)
        uab = k.sb("uab", [128, 8, 3 + TB], BF16, 8)
        ub = k.sb("ub", [128, 8, TB], BF16, 8)
        apre = k.sb("apre", [128, 8, TB], BF16, 8)
        CS = k.sb("CS", [128, 16, TB], BF16, 16)
        G1 = k.sb("G1", [128, 16, TB], BF16, 16)

        def fviews(tt, ntiles):
            f = tt.t.bitcast(F32).reshape([128, ntiles, TB])
            return [VT(f[:, q, :], tt.b, 2 * q, 2 * q + 2) for q in range(ntiles)]
        CSf = fviews(CS, 8)
        G1f = fviews(G1, 8)
        apf = fviews(apre, 4)
        pool20 = CSf + G1f + apf
        k.wslots = [k.sb("ws%d" % i, [128, 2048], BF16) for i in range(6)]
        Cst = [k.sb("Cst%d" % l, [128, 2, 4, 257], F32, 8) for l in range(DEPTH)]
        mst = k.sb("mst", [4, DEPTH], F32, DEPTH)
        x0r = k.sb("x0r", [128, DEPTH, 32], F32, DEPTH); x0i = k.sb("x0i", [128, DEPTH, 32], F32, DEPTH)
        hist = k.sb("hist", [128, DEPTH, 8, 3], BF16, DEPTH)
        SC = [k.sb("SC%d" % i, [128, TB], F32) for i in range(3)]
        SB = [k.sb("SB%d" % i, [128, TB], BF16) for i in range(4)]
        gn = k.sb("gn", [128, 7 * 16], F32)
        cst = k.sb("cst", [128, NCONST], F32)
        ident_bf = k.sb("ident_bf", [128, 128], BF16)
        ones_bf = k.sb("ones_bf", [128, 128], BF16)
        ones_f = k.sb("ones_f", [128, 128], F32)
        epsc = k.sb("epsc", [128, 1], F32)
        onec = k.sb("onec", [128, 1], F32)
        hpic = k.sb("hpic", [128, 1], F32)
        lpt = k.sb("lpt", [128, NLP], F32)
        gh = k.sb("gh", [128, 1024], F32)
        wif = k.sb("wif", [128, 128], BF16)
        wrt = k.sb("wrt", [128, 128], BF16)
        qk = [k.sb("qk%d" % i, [128, 4, TB], BF16, 4) for i in range(1)]
        gts = k.sb("gts", [128, 4, 12], F32, 4)
        sm = {}

        def small(name, shape, dt=F32):
            if name not in sm:
                sm[name] = k.sb("sm_" + name, shape, dt)
            return sm[name]
        ktok = [k.sb("ktok%d" % i, [128, 256], BF16) for i in range(2)]
        vext = [k.sb("vext%d" % i, [128, 257], BF16) for i in range(2)]
        spT = [k.sb("spT%d" % i, [128, 128], BF16) for i in range(2)]
        cbf = [k.sb("cbf%d" % i, [128, 2, 257], BF16) for i in range(2)]
        hno = [k.sb("hno%d" % i, [128, 256], BF16) for i in range(2)]
        tmpg = [k.sb("tmpg%d" % i, [128, 128], F32) for i in range(2)]
        sqv = k.sb("sqv", [128, 256], F32)
        c0t = k.sb("c0t", [128, 8, NS, 3], F32)
        cso = k.sb("cso", [128, 8, NS, 3], F32)
        cpo = k.sb("cpo", [128, 8, 3], F32)
        nall = k.sb("nall", [128, 128], F32)
        nnew = k.sb("nnew", [128, 128], F32)
        npo = k.sb("npo", [128, 8], F32)
        qmask = k.sb("qmask", [128, 2, NS, NS], BF16)
        kmask = [k.sb("kmask%d" % i, [NS, 256], BF16) for i in range(2)]
        c0bc = k.sb("c0bc", [128, 64], F32)
        ZB = [k.sb("ZB%d" % i, [128, 4, 2, 128], BF16) for i in range(1)]
        ZC = [k.sb("ZC%d" % i, [128, 4, 2, 128], BF16) for i in range(1)]
        s1 = k.sb("s1", [128, 12, 32], F32)
        kint = VT(SC[1].t.bitcast(mybir.dt.int32)[:, 0:TBP], SC[1].b, 0, 1)
        print("SBUF bytes remaining:", nc.sbuf_bytes_remaining)

        k.banks = [k.psum("ps%d" % i, [128, 512], F32) for i in range(4)]
        psA = k.psum("psA", [128, 512], F32)
        psB = k.psum("psB", [128, 512], F32)
        psS = k.psum("psS", [128, 512], F32)
        psT = k.psum("psT", [128, 1024], BF16)

        ident_f = cst.t[:, C_ID:C_ID + 128]
        tri_f = cst.t[:, C_TRI:C_TRI + 128]
        jj = cst.t[:, C_JJ:C_JJ + 512]
        d16 = cst.t[:, C_D16:C_D16 + 256].rearrange("p (a b) -> p a b", a=16)

        k.Dm(lambda e: e.dma_start(out=gn.t[:, :], in_=gains.t), w=[gn.s()])
        k.Dm(lambda e: e.dma_start(out=cst.t[:, :], in_=consts.t), w=[cst.s()])
        k.V(lambda e: e.memset(ones_bf.t[:, :], 1.0), w=[ones_bf.s()])
        k.V(lambda e: e.memset(ones_f.t[:, :], 1.0), w=[ones_f.s()])
        k.V(lambda e: e.memset(epsc.t[:, :], EPS), w=[epsc.s()])
        k.V(lambda e: e.memset(onec.t[:, :], 1.0), w=[onec.s()])
        k.V(lambda e: e.memset(hpic.t[:, :], TWO_PI / 4), w=[hpic.s()])
        k.V(lambda e: e.tensor_copy(out=ident_bf.t[:, :], in_=ident_f), r=[cst.s()], w=[ident_bf.s()])
        for l in range(DEPTH):
            k.V(lambda e, l=l: e.memset(Cst[l].t[:, :, :, :], 0.0), w=[Cst[l].s()])
        k.V(lambda e: e.memset(mst.t[:, :], 0.0), w=[mst.s()])
        k.V(lambda e: e.memset(x0r.t[:, :, :], 0.0), w=[x0r.s()])
        k.V(lambda e: e.memset(x0i.t[:, :, :], 0.0), w=[x0i.s()])
        k.V(lambda e: e.memset(hist.t[:, :, :, :], 0.0), w=[hist.s()])
        for i in range(2):
            k.V(lambda e, i=i: e.memset(vext[i].t[:, 256:257], 1.0), w=[vext[i].s()])
        vsb = small("Vsb", [NS, 257], BF16)
        k.V(lambda e: e.memset(vsb.t[:, 256:257], 1.0), w=[vsb.s()])
        k.Wm(lambda e: e.dma_start(out=wrt.t[:, :], in_=w_rt.t), w=[wrt.s()])

        def chunks(blk):
            return [(0, TBP)] + ([(TBP, NS)] if blk == NB - 1 else [])

        def lpc(c):
            return lpt.t[:, c:c + 1]

        def rms(blk, gidx, dst, out32=None):
            for (c0, n) in chunks(blk):
                bk = k.bank()
                rs = SC[0]
                for kt in range(16):
                    s_ = SB[kt % 2]
                    k.A(lambda e, s_=s_, kt=kt, c0=c0, n=n: e.activation(out=s_.t[:, :n], in_=h.t[:, kt, c0:c0 + n], func=AF.Square),
                        r=[h.s(kt, kt + 1)], w=[s_.s()])
                    k.M(lambda e, s_=s_, kt=kt, n=n, bk=bk: e.matmul(bk.t[:, :n], lhsT=ones_bf.t[:, :], rhs=s_.t[:, :n], start=(kt == 0), stop=(kt == 15)),
                        r=[s_.s(), ones_bf.s()], w=[bk.s()])
                k.A(lambda e, n=n, bk=bk, rs=rs: e.activation(out=rs.t[:, :n], in_=bk.t[:, :n], func=AF.Sqrt, bias=epsc.t[:, 0:1], scale=1.0 / D),
                    r=[bk.s(), epsc.s()], w=[rs.s()])
                k.V(lambda e, n=n, rs=rs: e.reciprocal(out=rs.t[:, :n], in_=rs.t[:, :n]), r=[rs.s()], w=[rs.s()])
                for kt in range(16):
                    if out32 is None:
                        k.V(lambda e, kt=kt, c0=c0, n=n, rs=rs: e.scalar_tensor_tensor(
                            out=dst.t[:, kt, c0:c0 + n], in0=h.t[:, kt, c0:c0 + n], scalar=gn.t[:, gidx * 16 + kt:gidx * 16 + kt + 1],
                            in1=rs.t[:, :n], op0=ALU.mult, op1=ALU.mult),
                            r=[h.s(kt, kt + 1), rs.s(), gn.s()], w=[dst.s(kt, kt + 1)])
                    else:
                        out32(kt, c0, n, rs)

        def fm_proj(blk, waps, KT, src, epi, mt_per_slot=1):
            for j, wap in enumerate(waps):
                ws = k.wload(wap)
                for mm in range(mt_per_slot):
                    mt = j * mt_per_slot + mm
                    for (c0, n) in chunks(blk):
                        bk = k.bank()
                        for kt in range(KT):
                            off = (mm * KT + kt) * 128
                            k.M(lambda e, ws=ws, off=off, kt=kt, c0=c0, n=n, bk=bk: e.matmul(
                                bk.t[:, :n], lhsT=ws.t[:, off:off + 128], rhs=src.t[:, kt, c0:c0 + n],
                                start=(kt == 0), stop=(kt == KT - 1)),
                                r=[ws.s(), src.s(kt, kt + 1)], w=[bk.s()])
                        epi(mt, c0, n, bk)

        def epi_act(dst, func, base=0, col0=0):
            def f(mt, c0, n, bk):
                k.A(lambda e, mt=mt, c0=c0, n=n, bk=bk: e.activation(out=dst.t[:, base + mt, col0 + c0:col0 + c0 + n], in_=bk.t[:, :n], func=func),
                    r=[bk.s()], w=[dst.s(base + mt, base + mt + 1)])
            return f

        def disc(get, rw, itmp):
            A_ = lambda fn: k.A(fn, r=rw + [hpic.s()], w=rw)
            V_ = lambda fn: k.V(fn, r=rw + [kint.s()], w=rw + [kint.s()])
            g = get
            A_(lambda e: e.activation(out=g(9), in_=g(2), func=AF.Exp))
            V_(lambda e: e.tensor_tensor(out=g(10), in0=g(0), in1=g(9), op=ALU.mult))
            A_(lambda e: e.activation(out=g(3), in_=g(10), func=AF.Exp))
            V_(lambda e: e.tensor_tensor(out=g(10), in0=g(1), in1=g(9), op=ALU.mult))
            V_(lambda e: e.tensor_scalar(out=g(10), in0=g(10), scalar1=1.0 / TWO_PI, scalar2=None, op0=ALU.mult))
            V_(lambda e: e.tensor_copy(out=itmp, in_=g(10)))
            V_(lambda e: e.tensor_tensor(out=g(4), in0=g(10), in1=itmp, op=ALU.subtract))
            A_(lambda e: e.activation(out=g(6), in_=g(4), func=AF.Sin, scale=TWO_PI))
            A_(lambda e: e.activation(out=g(10), in_=g(4), func=AF.Abs))
            A_(lambda e: e.activation(out=g(5), in_=g(10), func=AF.Sin, scale=-TWO_PI, bias=hpic.t[:, 0:1]))
            V_(lambda e: e.tensor_tensor(out=g(5), in0=g(5), in1=g(3), op=ALU.mult))
            V_(lambda e: e.tensor_tensor(out=g(6), in0=g(6), in1=g(3), op=ALU.mult))
            V_(lambda e: e.tensor_tensor(out=g(9), in0=g(0), in1=g(0), op=ALU.mult))
            V_(lambda e: e.tensor_tensor(out=g(10), in0=g(1), in1=g(1), op=ALU.mult))
            V_(lambda e: e.tensor_tensor(out=g(9), in0=g(9), in1=g(10), op=ALU.add))
            V_(lambda e: e.reciprocal(out=g(9), in_=g(9)))
            V_(lambda e: e.tensor_scalar(out=g(10), in0=g(5), scalar1=-1.0, scalar2=None, op0=ALU.add))
            V_(lambda e: e.tensor_tensor(out=g(7), in0=g(10), in1=g(0), op=ALU.mult))
            V_(lambda e: e.tensor_tensor(out=g(11), in0=g(6), in1=g(1), op=ALU.mult))
            V_(lambda e: e.tensor_tensor(out=g(7), in0=g(7), in1=g(11), op=ALU.add))
            V_(lambda e: e.tensor_tensor(out=g(7), in0=g(7), in1=g(9), op=ALU.mult))
            V_(lambda e: e.tensor_tensor(out=g(8), in0=g(6), in1=g(0), op=ALU.mult))
            V_(lambda e: e.tensor_tensor(out=g(11), in0=g(10), in1=g(1), op=ALU.mult))
            V_(lambda e: e.tensor_tensor(out=g(8), in0=g(8), in1=g(11), op=ALU.subtract))
            V_(lambda e: e.tensor_tensor(out=g(8), in0=g(8), in1=g(9), op=ALU.mult))

        def s5_stage(blk, l):
            last = blk == NB - 1
            k.Dm(lambda e: e.dma_start(out=s1.t[:, 0:3, :].rearrange("p a b -> p (a b)"), in_=s5L1.t[l]), w=[s1.s()])
            disc(lambda q: s1.t[:, q, :], [s1.s()], kint.t[:, 0:32])
            L2 = pool20[0:12]
            rw = [t.s() for t in L2]

            def g2(q):
                return L2[q].t[:, 0:512]
            for q in range(3):
                k.Dm(lambda e, q=q: e.dma_start(out=g2(q), in_=s5L2.t[l][:, q * 512:(q + 1) * 512]), r=rw, w=rw)
            disc(g2, rw, kint.t[:, 0:512])
            V_ = lambda fn: k.V(fn, r=rw, w=rw)
            k.Dm(lambda e: e.dma_start(out=g2(0), in_=s5B2.t[l][:, 0:512]), r=rw, w=rw)
            k.Dm(lambda e: e.dma_start(out=g2(1), in_=s5B2.t[l][:, 512:1024]), r=rw, w=rw)
            V_(lambda e: e.tensor_tensor(out=g2(2), in0=g2(7), in1=g2(0), op=ALU.mult))
            V_(lambda e: e.tensor_tensor(out=g2(10), in0=g2(8), in1=g2(1), op=ALU.mult))
            V_(lambda e: e.tensor_tensor(out=g2(2), in0=g2(2), in1=g2(10), op=ALU.subtract))
            V_(lambda e: e.tensor_tensor(out=g2(3), in0=g2(7), in1=g2(1), op=ALU.mult))
            V_(lambda e: e.tensor_tensor(out=g2(10), in0=g2(8), in1=g2(0), op=ALU.mult))
            V_(lambda e: e.tensor_tensor(out=g2(3), in0=g2(3), in1=g2(10), op=ALU.add))
            k.Dm(lambda e: e.dma_start(out=g2(4), in_=s5C1.t[l][:, 0:512]), r=rw, w=rw)
            k.Dm(lambda e: e.dma_start(out=g2(5), in_=s5C1.t[l][:, 512:1024]), r=rw, w=rw)
            V_(lambda e: e.tensor_scalar(out=g2(5), in0=g2(5), scalar1=-1.0, scalar2=None, op0=ALU.mult))
            bbv = [g2(2).rearrange("p (i q) -> p i q", q=64), g2(3).rearrange("p (i q) -> p i q", q=64)]
            ccv = [g2(4).rearrange("p (j c) -> p j c", c=16), g2(5).rearrange("p (j c) -> p j c", c=16)]
            live = [L2[q].s() for q in (2, 3, 4, 5)]
            cs_t, sn_t, y1_t, fr_t, t1_t, t2_t = pool20[0], pool20[1], pool20[6], pool20[7], pool20[8], pool20[9]
            br_t, bi_t, wr_t, wi_t, rt_t = pool20[10], pool20[11], pool20[12], pool20[13], pool20[14]
            N = TBP
            xs5 = [VT(pool20[15 + ri].t[:, 0:512].rearrange("p (a b) -> p a b", b=NS), pool20[15 + ri].b, pool20[15 + ri].lo, pool20[15 + ri].hi) for ri in range(2)]
            xs5b = [VT(apre.t[:, 2 + ri, 0:512].rearrange("p (a b) -> p a b", b=NS), apre.b, 2 + ri, 3 + ri) for ri in range(2)]
            if last:
                for ri in range(2):
                    k.Dm(lambda e, ri=ri: e.dma_start(out=xs5[ri].t.rearrange("p a b -> p (a b)"), in_=sts5.t[l, ri]), w=[xs5[ri].s()])

            def mul(o, a, b, extra_r=()):
                k.V(lambda e: e.tensor_tensor(out=o.t[:, 0:N], in0=a.t[:, 0:N], in1=b.t[:, 0:N], op=ALU.mult), r=[a.s(), b.s()] + list(extra_r), w=[o.s()])

            def addsub(o, a, b, op):
                k.V(lambda e: e.tensor_tensor(out=o.t[:, 0:N], in0=a.t[:, 0:N], in1=b.t[:, 0:N], op=op), r=[a.s(), b.s()], w=[o.s()])

            for i in range(8):
                zb = ZB[0]; zc = ZC[0]
                k.V(lambda e, zc=zc: e.memset(zc.t[:, :, :, :], 0.0), w=[zc.s()])
                for jm in range(4):
                    for ri in range(2):
                        for gg in range(2):
                            k.V(lambda e, zb=zb, jm=jm, ri=ri, gg=gg, i=i: e.tensor_scalar(
                                out=zb.t[:, jm, ri, gg * 64:(gg + 1) * 64], in0=bbv[ri][:, i, :],
                                scalar1=cst.t[:, C_M2 + 2 * jm + gg:C_M2 + 2 * jm + gg + 1], scalar2=None, op0=ALU.mult),
                                r=live + [cst.s()], w=[zb.s()])
                            col = (2 * jm + gg) * 16
                            k.V(lambda e, zc=zc, jm=jm, ri=ri, gg=gg, i=i, col=col: e.tensor_copy(
                                out=zc.t[gg * 64:(gg + 1) * 64, jm, ri, col:col + 16], in_=ccv[ri][gg * 64:(gg + 1) * 64, 4 * i + jm, :]),
                                r=live, w=[zc.s()])
                yps = psA if i % 2 == 0 else psB
                for jm in range(4):
                    j = 4 * i + jm
                    pre = k.bank(); pim = k.bank()
                    k.M(lambda e, zb=zb, jm=jm, i=i, pre=pre: e.matmul(pre.t[:, 0:N], lhsT=zb.t[:, jm, 0, :], rhs=ub.t[:, i, 0:N], start=True, stop=True),
                        r=[zb.s(), ub.s(i, i + 1)], w=[pre.s()])
                    k.M(lambda e, zb=zb, jm=jm, i=i, pim=pim: e.matmul(pim.t[:, 0:N], lhsT=zb.t[:, jm, 1, :], rhs=ub.t[:, i, 0:N], start=True, stop=True),
                        r=[zb.s(), ub.s(i, i + 1)], w=[pim.s()])
                    k.V(lambda e, j=j: e.tensor_scalar(out=y1_t.t[:, 0:N], in0=jj, scalar1=s1.t[:, 4, j:j + 1], scalar2=None, op0=ALU.mult),
                        r=[cst.s(), s1.s()], w=[y1_t.s()])
                    k.V(lambda e: e.tensor_copy(out=kint.t[:, 0:N], in_=y1_t.t[:, 0:N]), r=[y1_t.s()], w=[kint.s()])
                    k.V(lambda e: e.tensor_tensor(out=fr_t.t[:, 0:N], in0=y1_t.t[:, 0:N], in1=kint.t[:, 0:N], op=ALU.subtract),
                        r=[y1_t.s(), kint.s()], w=[fr_t.s()])
                    k.A(lambda e: e.activation(out=sn_t.t[:, 0:N], in_=fr_t.t[:, 0:N], func=AF.Sin, scale=TWO_PI), r=[fr_t.s()], w=[sn_t.s()])
                    k.A(lambda e: e.activation(out=y1_t.t[:, 0:N], in_=fr_t.t[:, 0:N], func=AF.Abs), r=[fr_t.s()], w=[y1_t.s()])
                    k.A(lambda e: e.activation(out=cs_t.t[:, 0:N], in_=y1_t.t[:, 0:N], func=AF.Sin, scale=-TWO_PI, bias=hpic.t[:, 0:1]),
                        r=[y1_t.s(), hpic.s()], w=[cs_t.s()])
                    k.V(lambda e, j=j: e.tensor_scalar(out=rt_t.t[:, 0:N], in0=ones_f.t[:, 0:1].to_broadcast([128, N]), scalar1=s1.t[:, 3, j:j + 1], scalar2=None, op0=ALU.mult),
                        r=[ones_f.s(), s1.s()], w=[rt_t.s()])
                    k.V(lambda e, pre=pre: e.tensor_tensor(out=t1_t.t[:, 0:N], in0=pre.t[:, 0:N], in1=cs_t.t[:, 0:N], op=ALU.mult), r=[pre.s(), cs_t.s()], w=[t1_t.s()])
                    k.V(lambda e, pim=pim: e.tensor_tensor(out=t2_t.t[:, 0:N], in0=pim.t[:, 0:N], in1=sn_t.t[:, 0:N], op=ALU.mult), r=[pim.s(), sn_t.s()], w=[t2_t.s()])
                    addsub(br_t, t1_t, t2_t, ALU.add)
                    k.V(lambda e, pim=pim: e.tensor_tensor(out=t1_t.t[:, 0:N], in0=pim.t[:, 0:N], in1=cs_t.t[:, 0:N], op=ALU.mult), r=[pim.s(), cs_t.s()], w=[t1_t.s()])
                    k.V(lambda e, pre=pre: e.tensor_tensor(out=t2_t.t[:, 0:N], in0=pre.t[:, 0:N], in1=sn_t.t[:, 0:N], op=ALU.mult), r=[pre.s(), sn_t.s()], w=[t2_t.s()])
                    addsub(bi_t, t1_t, t2_t, ALU.subtract)
                    k.V(lambda e, j=j: e.tensor_tensor_scan(out=wr_t.t[:, 0:N], data0=rt_t.t[:, 0:N], data1=br_t.t[:, 0:N], initial=x0r.t[:, l, j:j + 1], op0=ALU.mult, op1=ALU.add),
                        r=[rt_t.s(), br_t.s(), x0r.s(l, l + 1)], w=[wr_t.s()])
                    k.V(lambda e, j=j: e.tensor_tensor_scan(out=wi_t.t[:, 0:N], data0=rt_t.t[:, 0:N], data1=bi_t.t[:, 0:N], initial=x0i.t[:, l, j:j + 1], op0=ALU.mult, op1=ALU.add),
                        r=[rt_t.s(), bi_t.s(), x0i.s(l, l + 1)], w=[wi_t.s()])
                    mul(t1_t, wr_t, cs_t); mul(t2_t, wi_t, sn_t); addsub(br_t, t1_t, t2_t, ALU.subtract)
                    mul(t1_t, wr_t, sn_t); mul(t2_t, wi_t, cs_t); addsub(bi_t, t1_t, t2_t, ALU.add)
                    k.V(lambda e, j=j: e.tensor_copy(out=x0r.t[:, l, j:j + 1], in_=br_t.t[:, N - 1:N]), r=[br_t.s()], w=[x0r.s(l, l + 1)])
                    k.V(lambda e, j=j: e.tensor_copy(out=x0i.t[:, l, j:j + 1], in_=bi_t.t[:, N - 1:N]), r=[bi_t.s()], w=[x0i.s(l, l + 1)])
                    xrb = SB[2]; xib = SB[3]
                    k.A(lambda e: e.activation(out=xrb.t[:, 0:N], in_=br_t.t[:, 0:N], func=AF.Copy), r=[br_t.s()], w=[xrb.s()])
                    k.A(lambda e: e.activation(out=xib.t[:, 0:N], in_=bi_t.t[:, 0:N], func=AF.Copy), r=[bi_t.s()], w=[xib.s()])
                    k.M(lambda e, zc=zc, jm=jm, yps=yps: e.matmul(yps.t[:, 0:N], lhsT=zc.t[:, jm, 0, :], rhs=xrb.t[:, 0:N], start=(jm == 0), stop=False),
                        r=[zc.s(), xrb.s()], w=[yps.s()])
                    k.M(lambda e, zc=zc, jm=jm, yps=yps: e.matmul(yps.t[:, 0:N], lhsT=zc.t[:, jm, 1, :], rhs=xib.t[:, 0:N], start=False, stop=(jm == 3)),
                        r=[zc.s(), xib.s()], w=[yps.s()])
                    if last:
                        pb2 = k.bank()
                        k.M(lambda e, zb=zb, jm=jm, i=i, pb2=pb2: e.matmul(pb2.t[:, 0:NS], lhsT=zb.t[:, jm, 0, :], rhs=ub.t[:, i, TBP:TB], start=True, stop=True),
                            r=[zb.s(), ub.s(i, i + 1)], w=[pb2.s()])
                        k.M(lambda e, zb=zb, jm=jm, i=i, pb2=pb2: e.matmul(pb2.t[:, 32:32 + NS], lhsT=zb.t[:, jm, 1, :], rhs=ub.t[:, i, TBP:TB], start=True, stop=True),
                            r=[zb.s(), ub.s(i, i + 1)], w=[pb2.s()])
                        sa = small("s5a", [128, NS]); sbb = small("s5b", [128, NS]); sc_ = small("s5c", [128, NS]); sd_ = small("s5d", [128, NS])
                        k.V(lambda e, j=j, pb2=pb2: e.scalar_tensor_tensor(out=sa.t[:, :], in0=xs5[0].t[:, j, :], scalar=s1.t[:, 5, j:j + 1], in1=pb2.t[:, 0:NS], op0=ALU.mult, op1=ALU.add),
                            r=[xs5[0].s(), s1.s(), pb2.s()], w=[sa.s()])
                        k.V(lambda e, j=j, pb2=pb2: e.scalar_tensor_tensor(out=sbb.t[:, :], in0=xs5[0].t[:, j, :], scalar=s1.t[:, 6, j:j + 1], in1=pb2.t[:, 32:32 + NS], op0=ALU.mult, op1=ALU.add),
                            r=[xs5[0].s(), s1.s(), pb2.s()], w=[sbb.s()])
                        k.V(lambda e, j=j: e.tensor_scalar(out=sc_.t[:, :], in0=xs5[1].t[:, j, :], scalar1=s1.t[:, 6, j:j + 1], scalar2=None, op0=ALU.mult),
                            r=[xs5[1].s(), s1.s()], w=[sc_.s()])
                        k.V(lambda e, j=j: e.scalar_tensor_tensor(out=sd_.t[:, :], in0=xs5[1].t[:, j, :], scalar=s1.t[:, 5, j:j + 1], in1=sbb.t[:, :], op0=ALU.mult, op1=ALU.add),
                            r=[xs5[1].s(), s1.s(), sbb.s()], w=[sd_.s()])
                        k.V(lambda e, j=j: e.tensor_tensor(out=xs5[0].t[:, j, :], in0=sa.t[:, :], in1=sc_.t[:, :], op=ALU.subtract),
                            r=[sa.s(), sc_.s(), xs5[1].s()], w=[xs5[0].s()])
                        k.V(lambda e, j=j: e.tensor_copy(out=xs5[1].t[:, j, :], in_=sd_.t[:, :]), r=[sd_.s()], w=[xs5[1].s()])
                        for ri in range(2):
                            k.A(lambda e, j=j, ri=ri: e.activation(out=xs5b[ri].t[:, j, :], in_=xs5[ri].t[:, j, :], func=AF.Copy), r=[xs5[ri].s()], w=[xs5b[ri].s()])
                        k.M(lambda e, zc=zc, jm=jm, j=j, i=i: e.matmul(psS.t[:, i * 16:(i + 1) * 16], lhsT=zc.t[:, jm, 0, :], rhs=xs5b[0].t[:, j, :], start=(jm == 0), stop=False),
                            r=[zc.s(), xs5b[0].s()], w=[psS.s()])
                        k.M(lambda e, zc=zc, jm=jm, j=j, i=i: e.matmul(psS.t[:, i * 16:(i + 1) * 16], lhsT=zc.t[:, jm, 1, :], rhs=xs5b[1].t[:, j, :], start=False, stop=(jm == 3)),
                            r=[zc.s(), xs5b[1].s()], w=[psS.s()])

                def gelu_epi(src_ps, src_tt, c0, n, i=i):
                    ya, yb2 = t1_t, t2_t
                    k.V(lambda e: e.scalar_tensor_tensor(out=ya.t[:, :n], in0=ub.t[:, i, c0:c0 + n], scalar=lpc(LP_DSK + i), in1=src_ps, op0=ALU.mult, op1=ALU.add),
                        r=[ub.s(i, i + 1), lpt.s(), src_tt.s()], w=[ya.s()])
                    k.V(lambda e: e.tensor_tensor(out=yb2.t[:, :n], in0=ya.t[:, :n], in1=ya.t[:, :n], op=ALU.mult), r=[ya.s()], w=[yb2.s()])
                    k.V(lambda e: e.tensor_scalar(out=yb2.t[:, :n], in0=yb2.t[:, :n], scalar1=0.044715, scalar2=1.0, op0=ALU.mult, op1=ALU.add), r=[yb2.s()], w=[yb2.s()])
                    k.V(lambda e: e.tensor_tensor(out=yb2.t[:, :n], in0=yb2.t[:, :n], in1=ya.t[:, :n], op=ALU.mult), r=[yb2.s(), ya.s()], w=[yb2.s()])
                    k.A(lambda e: e.activation(out=yb2.t[:, :n], in_=yb2.t[:, :n], func=AF.Sigmoid, scale=1.5957691216057308), r=[yb2.s()], w=[yb2.s()])
                    k.V(lambda e: e.tensor_tensor(out=ub.t[:, i, c0:c0 + n], in0=ya.t[:, :n], in1=yb2.t[:, :n], op=ALU.mult), r=[ya.s(), yb2.s()], w=[ub.s(i, i + 1)])
                if last:
                    gelu_epi(psS.t[:, i * 16:(i + 1) * 16], psS, TBP, NS)
                gelu_epi(yps.t[:, 0:N], yps, 0, N)
            if last:
                k.Dm(lambda e: e.dma_start(out=o_s5p.t[l, 0], in_=x0r.t[:, l, :]), r=[x0r.s(l, l + 1)], w=[o_s5p.s(2 * l, 2 * l + 1)])
                k.Dm(lambda e: e.dma_start(out=o_s5p.t[l, 1], in_=x0i.t[:, l, :]), r=[x0i.s(l, l + 1)], w=[o_s5p.s(2 * l + 1, 2 * l + 2)])
                for ri in range(2):
                    k.Dm(lambda e, ri=ri: e.dma_start(out=o_s5s.t[l, ri], in_=xs5[ri].t.rearrange("p a b -> p (a b)")), r=[xs5[ri].s()], w=[o_s5s.s(2 * l + ri, 2 * l + ri + 1)])

        def conv_stage(blk, l):
            last = blk == NB - 1
            if last:
                k.Dm(lambda e: e.dma_start(out=c0t.t[:, :, :, :].rearrange("p a b c -> p (a b c)"), in_=stconv.t[l]), w=[c0t.s()])
            for kt in range(8):
                acc = SC[1 + kt % 2]
                cw = lambda j, kt=kt: lpc(LP_CW + kt * 4 + j)
                k.V(lambda e, kt=kt, acc=acc, cw=cw: e.tensor_scalar(out=acc.t[:, 0:TBP], in0=uab.t[:, kt, 0:TBP], scalar1=cw(0), scalar2=lpc(LP_CB + kt), op0=ALU.mult, op1=ALU.add),
                    r=[uab.s(kt, kt + 1), lpt.s()], w=[acc.s()])
                for j in range(1, 4):
                    k.V(lambda e, kt=kt, acc=acc, cw=cw, j=j: e.scalar_tensor_tensor(out=acc.t[:, 0:TBP], in0=uab.t[:, kt, j:j + TBP], scalar=cw(j), in1=acc.t[:, 0:TBP], op0=ALU.mult, op1=ALU.add),
                        r=[uab.s(kt, kt + 1), lpt.s(), acc.s()], w=[acc.s()])
                k.A(lambda e, kt=kt, acc=acc: e.activation(out=CS.t[:, kt, 0:TBP], in_=acc.t[:, 0:TBP], func=AF.Silu), r=[acc.s()], w=[CS.s(kt, kt + 1)])
                if last:
                    accs = small("accs%d" % (kt % 2), [128, NS])
                    k.V(lambda e, kt=kt, accs=accs, cw=cw: e.tensor_scalar(out=accs.t[:, :], in0=c0t.t[:, kt, :, 0], scalar1=cw(0), scalar2=lpc(LP_CB + kt), op0=ALU.mult, op1=ALU.add),
                        r=[c0t.s(), lpt.s()], w=[accs.s()])
                    for j in range(1, 3):
                        k.V(lambda e, kt=kt, accs=accs, cw=cw, j=j: e.scalar_tensor_tensor(out=accs.t[:, :], in0=c0t.t[:, kt, :, j], scalar=cw(j), in1=accs.t[:, :], op0=ALU.mult, op1=ALU.add),
                            r=[c0t.s(), lpt.s(), accs.s()], w=[accs.s()])
                    k.V(lambda e, kt=kt, accs=accs, cw=cw: e.scalar_tensor_tensor(out=accs.t[:, :], in0=uab.t[:, kt, 3 + TBP:3 + TB], scalar=cw(3), in1=accs.t[:, :], op0=ALU.mult, op1=ALU.add),
                        r=[uab.s(kt, kt + 1), lpt.s(), accs.s()], w=[accs.s()])
                    k.A(lambda e, kt=kt, accs=accs: e.activation(out=CS.t[:, kt, TBP:TB], in_=accs.t[:, :], func=AF.Silu), r=[accs.s()], w=[CS.s(kt, kt + 1)])
            if last:
                k.V(lambda e: e.tensor_copy(out=cpo.t[:, :, :], in_=uab.t[:, :, TBP:TBP + 3]), r=[uab.s()], w=[cpo.s()])
                cv = o_convp.t[l].rearrange("(kt p) r -> p kt r", p=128)
                k.Dm(lambda e, cv=cv: e.dma_start(out=cv, in_=cpo.t[:, :, :]), r=[cpo.s()], w=[o_convp.s(l, l + 1)])
                k.V(lambda e: e.tensor_copy(out=cso.t[:, :, :, 0:2], in_=c0t.t[:, :, :, 1:3]), r=[c0t.s()], w=[cso.s()])
                k.V(lambda e: e.tensor_copy(out=cso.t[:, :, :, 2], in_=uab.t[:, :, 3 + TBP:3 + TB]), r=[uab.s()], w=[cso.s()])
                k.Dm(lambda e: e.dma_start(out=o_convs.t[l], in_=cso.t[:, :, :, :].rearrange("p a b c -> p (a b c)")), r=[cso.s()], w=[o_convs.s(l, l + 1)])
            k.V(lambda e: e.tensor_copy(out=hist.t[:, l, :, :], in_=uab.t[:, :, TBP:TBP + 3]), r=[uab.s()], w=[hist.s(l, l + 1)])

        def logsig(src_ap, dst_ap, tmp_ap, np_, rd, wr):
            k.A(lambda e: e.activation(out=tmp_ap, in_=src_ap, func=AF.Abs), r=rd, w=wr)
            k.A(lambda e: e.activation(out=tmp_ap, in_=tmp_ap, func=AF.Exp, scale=-1.0), r=rd, w=wr)
            k.A(lambda e: e.activation(out=tmp_ap, in_=tmp_ap, func=AF.Ln, bias=onec.t[0:np_, 0:1]), r=rd + [onec.s()], w=wr)
            k.V(lambda e: e.tensor_scalar_min(out=dst_ap, in0=src_ap, scalar1=0.0), r=rd, w=wr)
            k.V(lambda e: e.tensor_tensor(out=dst_ap, in0=dst_ap, in1=tmp_ap, op=ALU.subtract), r=rd, w=wr)

        def mlstm_stage(blk, l):
            last = blk == NB - 1
            cks = chunks(blk)
            wq_s = k.wload(wqkv.t[l, 0]); wk_s = k.wload(wqkv.t[l, 1]); wv_s = k.wload(wqkv.t[l, 2])

            def wsl(ws, hd, kt2, e0, e1):
                b0 = (hd * 2 + kt2) * 256
                return ws.t[:, b0 + e0:b0 + e1]
            gw = small("gw", [128, 40])
            g4 = small("g4", [4, 16])
            GW = [gw.s()]; G4 = [g4.s()]
            for ck in range(4):
                c0 = ck * 128
                bk = k.bank()
                for kt in range(16):
                    k.M(lambda e, kt=kt, c0=c0, bk=bk: e.matmul(bk.t[:, 0:8], lhsT=xn.t[:, kt, c0:c0 + 128], rhs=wif.t[:, kt * 8:kt * 8 + 8], start=(kt == 0), stop=(kt == 15)),
                        r=[xn.s(kt, kt + 1), wif.s()], w=[bk.s()])
                k.V(lambda e, bk=bk: e.tensor_tensor(out=gw.t[:, 0:8], in0=bk.t[:, 0:8], in1=lpt.t[:, LP_BIF:LP_BIF + 8], op=ALU.add), r=[bk.s(), lpt.s()], w=GW)
                logsig(gw.t[:, 4:8], gw.t[:, 8:12], gw.t[:, 12:16], 128, GW, GW)
                b2 = k.bank()
                k.M(lambda e, b2=b2: e.matmul(b2.t[:, 0:4], lhsT=tri_f, rhs=gw.t[:, 8:12], start=True, stop=True), r=[cst.s()] + GW, w=[b2.s()])
                k.V(lambda e, b2=b2: e.tensor_copy(out=gw.t[:, 16:20], in_=b2.t[:, 0:4]), r=[b2.s()], w=GW)
                k.V(lambda e, b2=b2: e.tensor_tensor(out=gw.t[:, 20:24], in0=gw.t[:, 0:4], in1=b2.t[:, 0:4], op=ALU.subtract), r=[b2.s()] + GW, w=GW)
                b3 = k.bank()
                k.M(lambda e, b3=b3: e.matmul(b3.t[0:4, 0:128], lhsT=gw.t[:, 20:24], rhs=ident_f, start=True, stop=True), r=[cst.s()] + GW, w=[b3.s()])
                k.M(lambda e, b3=b3: e.matmul(b3.t[0:4, 128:129], lhsT=gw.t[:, 8:12], rhs=ones_f.t[:, 0:1], start=True, stop=True), r=[ones_f.s()] + GW, w=[b3.s()])
                k.V(lambda e, b3=b3: e.tensor_reduce(out=g4.t[:, 0:1], in_=b3.t[0:4, 0:128], axis=AX.X, op=ALU.max), r=[b3.s()], w=G4)
                k.V(lambda e: e.tensor_tensor(out=g4.t[:, 1:2], in0=g4.t[:, 0:1], in1=mst.t[:, l:l + 1], op=ALU.max), r=G4 + [mst.s(l, l + 1)], w=G4)
                k.V(lambda e: e.tensor_tensor(out=g4.t[:, 2:3], in0=mst.t[:, l:l + 1], in1=g4.t[:, 1:2], op=ALU.subtract), r=G4 + [mst.s(l, l + 1)], w=G4)
                k.V(lambda e: e.tensor_scalar(out=g4.t[:, 4:8], in0=cst.t[0:4, C_ID:C_ID + 4], scalar1=g4.t[:, 1:2], scalar2=None, op0=ALU.mult), r=G4 + [cst.s()], w=G4)
                k.V(lambda e: e.tensor_scalar(out=g4.t[:, 8:12], in0=cst.t[0:4, C_ID:C_ID + 4], scalar1=g4.t[:, 2:3], scalar2=None, op0=ALU.mult), r=G4 + [cst.s()], w=G4)
                b4 = k.bank()
                k.M(lambda e, b4=b4: e.matmul(b4.t[:, 0:8], lhsT=ones_f.t[0:4, 0:128], rhs=g4.t[:, 4:12], start=True, stop=True), r=G4 + [ones_f.s()], w=[b4.s()])
                k.V(lambda e, b3=b3: e.tensor_tensor(out=mst.t[:, l:l + 1], in0=b3.t[0:4, 128:129], in1=g4.t[:, 1:2], op=ALU.add), r=[b3.s()] + G4, w=[mst.s(l, l + 1)])
                k.V(lambda e, b4=b4: e.tensor_tensor(out=gw.t[:, 24:28], in0=gw.t[:, 20:24], in1=b4.t[:, 0:4], op=ALU.subtract), r=[b4.s()] + GW, w=GW)
                k.A(lambda e: e.activation(out=gw.t[:, 24:28], in_=gw.t[:, 24:28], func=AF.Exp), r=GW, w=GW)
                k.V(lambda e, ck=ck: e.tensor_scalar(out=gts.t[:, ck, 0:4], in0=gw.t[:, 24:28], scalar1=0.0625, scalar2=None, op0=ALU.mult), r=GW, w=[gts.s(ck, ck + 1)])
                k.V(lambda e, b4=b4: e.tensor_tensor(out=gw.t[:, 28:32], in0=gw.t[:, 16:20], in1=b4.t[:, 0:4], op=ALU.add), r=[b4.s()] + GW, w=GW)
                k.A(lambda e, ck=ck: e.activation(out=gts.t[:, ck, 4:8], in_=gw.t[:, 28:32], func=AF.Exp, scale=-1.0), r=GW, w=[gts.s(ck, ck + 1)])
                k.A(lambda e, ck=ck, b4=b4: e.activation(out=gts.t[:, ck, 8:12], in_=b4.t[:, 4:8], func=AF.Exp), r=[b4.s()], w=[gts.s(ck, ck + 1)])
            if last:
                gs = small("gs", [NS, 44])
                GS = [gs.s()]
                c0e = small("c0e", [NS, NS, 4])
                bk = k.bank()
                for kt in range(16):
                    k.M(lambda e, kt=kt, bk=bk: e.matmul(bk.t[0:NS, 0:8], lhsT=xn.t[:, kt, TBP:TB], rhs=wif.t[:, kt * 8:kt * 8 + 8], start=(kt == 0), stop=(kt == 15)),
                        r=[xn.s(kt, kt + 1), wif.s()], w=[bk.s()])
                k.Dm(lambda e: e.dma_start(out=gs.t[:, 40:44], in_=stm.t[l]), r=GS, w=GS)
                k.V(lambda e, bk=bk: e.tensor_tensor(out=gs.t[:, 0:8], in0=bk.t[0:NS, 0:8], in1=lpt.t[0:NS, LP_BIF:LP_BIF + 8], op=ALU.add), r=[bk.s(), lpt.s()] + GS, w=GS)
                logsig(gs.t[:, 4:8], gs.t[:, 8:12], gs.t[:, 12:16], NS, GS, GS)
                VS = lambda fn: k.V(fn, r=GS, w=GS)
                AS = lambda fn: k.A(fn, r=GS, w=GS)
                VS(lambda e: e.tensor_tensor(out=gs.t[:, 36:40], in0=gs.t[:, 0:4], in1=gs.t[:, 8:12], op=ALU.subtract))
                VS(lambda e: e.tensor_tensor(out=gs.t[:, 16:20], in0=gs.t[:, 36:40], in1=gs.t[:, 40:44], op=ALU.max))
                VS(lambda e: e.tensor_tensor(out=gs.t[:, 12:16], in0=gs.t[:, 36:40], in1=gs.t[:, 16:20], op=ALU.subtract))
                AS(lambda e: e.activation(out=gs.t[:, 20:24], in_=gs.t[:, 12:16], func=AF.Exp))
                VS(lambda e: e.tensor_tensor(out=gs.t[:, 12:16], in0=gs.t[:, 40:44], in1=gs.t[:, 16:20], op=ALU.subtract))
                AS(lambda e: e.activation(out=gs.t[:, 24:28], in_=gs.t[:, 12:16], func=AF.Exp))
                VS(lambda e: e.tensor_tensor(out=gs.t[:, 28:32], in0=gs.t[:, 8:12], in1=gs.t[:, 16:20], op=ALU.add))
                AS(lambda e: e.activation(out=gs.t[:, 32:36], in_=gs.t[:, 28:32], func=AF.Exp, scale=-1.0))
                k.Dm(lambda e: e.dma_start(out=o_ms.t[l], in_=gs.t[:, 28:32]), r=GS, w=[o_ms.s(l, l + 1)])
                for hd in range(4):
                    k.V(lambda e, hd=hd: e.tensor_scalar(out=c0e.t[:, :, hd], in0=cst.t[0:NS, C_ID:C_ID + NS], scalar1=gs.t[:, 24 + hd:25 + hd], scalar2=None, op0=ALU.mult),
                        r=GS + [cst.s()], w=[c0e.s()])
                bk = k.bank()
                k.M(lambda e, bk=bk: e.matmul(bk.t[:, 0:64], lhsT=ones_f.t[0:NS, 0:128], rhs=c0e.t[:, :, :].rearrange("p a b -> p (a b)"), start=True, stop=True),
                    r=[c0e.s(), ones_f.s()], w=[bk.s()])
                k.V(lambda e, bk=bk: e.tensor_copy(out=c0bc.t[:, :], in_=bk.t[:, 0:64]), r=[bk.s()], w=[c0bc.s()])
                k.Dm(lambda e: e.dma_start(out=nall.t[:, :], in_=stn.t[l]), w=[nall.s()])

            for hd in range(4):
                QK = qk[0]
                for which, ws in ((0, wq_s), (1, wk_s)):
                    for et in range(2):
                        for (c0, n) in cks:
                            bk = k.bank()
                            for kt2 in range(2):
                                k.M(lambda e, ws=ws, kt2=kt2, et=et, c0=c0, n=n, bk=bk, hd=hd: e.matmul(
                                    bk.t[:, :n], lhsT=wsl(ws, hd, kt2, et * 128, et * 128 + 128), rhs=CS.t[:, 2 * hd + kt2, c0:c0 + n], start=(kt2 == 0), stop=(kt2 == 1)),
                                    r=[ws.s(), CS.s(2 * hd + kt2, 2 * hd + kt2 + 1)], w=[bk.s()])
                            k.A(lambda e, QK=QK, which=which, et=et, c0=c0, n=n, bk=bk: e.activation(out=QK.t[:, which * 2 + et, c0:c0 + n], in_=bk.t[:, :n], func=AF.Copy),
                                r=[bk.s()], w=[QK.s(which * 2 + et, which * 2 + et + 1)])
                cslots = [(Cst[l].b, hd, hd + 1), (Cst[l].b, 4 + hd, 5 + hd)]
                for ck in range(4):
                    c0 = ck * 128
                    i2 = (hd * 4 + ck) % 2
                    kt_, ve, sp, cb, hn_, tg = ktok[i2], vext[i2], spT[i2], cbf[i2], hno[i2], tmpg[i2]
                    ek = gts.t[:, ck, hd:hd + 1]; thr = gts.t[:, ck, 4 + hd:5 + hd]; c0b = gts.t[:, ck, 8 + hd:9 + hd]
                    GT = [gts.s(ck, ck + 1)]
                    bk = k.bank()
                    for kt2 in range(2):
                        k.M(lambda e, kt2=kt2, c0=c0, bk=bk, hd=hd: e.matmul(bk.t[:, 0:256], lhsT=CS.t[:, 2 * hd + kt2, c0:c0 + 128], rhs=wsl(wk_s, hd, kt2, 0, 256), start=(kt2 == 0), stop=(kt2 == 1)),
                            r=[wk_s.s(), CS.s(2 * hd + kt2, 2 * hd + kt2 + 1)], w=[bk.s()])
                    k.A(lambda e, kt_=kt_, bk=bk, ek=ek: e.activation(out=kt_.t[:, :], in_=bk.t[:, 0:256], func=AF.Copy, scale=ek), r=[bk.s()] + GT, w=[kt_.s()])
                    bk = k.bank()
                    for kt2 in range(2):
                        k.M(lambda e, kt2=kt2, c0=c0, bk=bk, hd=hd: e.matmul(bk.t[:, 0:256], lhsT=uab.t[:, 2 * hd + kt2, 3 + c0:3 + c0 + 128], rhs=wsl(wv_s, hd, kt2, 0, 256), start=(kt2 == 0), stop=(kt2 == 1)),
                            r=[wv_s.s(), uab.s(2 * hd + kt2, 2 * hd + kt2 + 1)], w=[bk.s()])
                    k.V(lambda e, ve=ve, bk=bk: e.tensor_copy(out=ve.t[:, 0:256], in_=bk.t[:, 0:256]), r=[bk.s()], w=[ve.s()])
                    bk = k.bank()
                    for kt2 in range(2):
                        k.M(lambda e, kt2=kt2, c0=c0, bk=bk, QK=QK: e.matmul(bk.t[:, 0:128], lhsT=QK.t[:, 2 + kt2, c0:c0 + 128], rhs=QK.t[:, kt2, c0:c0 + 128], start=(kt2 == 0), stop=(kt2 == 1)),
                            r=[QK.s(2 + kt2, 3 + kt2), QK.s(kt2, kt2 + 1)], w=[bk.s()])
                    k.V(lambda e, sp=sp, bk=bk, ek=ek: e.scalar_tensor_tensor(out=sp.t[:, :], in0=bk.t[:, 0:128], scalar=ek, in1=tri_f, op0=ALU.mult, op1=ALU.mult),
                        r=[bk.s(), cst.s()] + GT, w=[sp.s()])
                    k.V(lambda e, cb=cb, c0b=c0b, hd=hd: e.tensor_scalar(out=cb.t[:, :, :], in0=Cst[l].t[:, :, hd, :], scalar1=c0b, scalar2=None, op0=ALU.mult),
                        r=cslots + GT, w=[cb.s()])
                    hb = k.bank()
                    k.M(lambda e, hb=hb, sp=sp, ve=ve: e.matmul(hb.t[:, 0:257], lhsT=sp.t[:, :], rhs=ve.t[:, :], start=True, stop=False), r=[sp.s(), ve.s()], w=[hb.s()])
                    for kt2 in range(2):
                        k.M(lambda e, hb=hb, kt2=kt2, c0=c0, cb=cb, QK=QK: e.matmul(hb.t[:, 0:257], lhsT=QK.t[:, kt2, c0:c0 + 128], rhs=cb.t[:, kt2, :], start=False, stop=(kt2 == 1)),
                            r=[QK.s(kt2, kt2 + 1), cb.s()], w=[hb.s()])
                    nr = small("nr%d" % i2, [128, 8])
                    NR = [nr.s()]
                    k.A(lambda e, hb=hb, nr=nr: e.activation(out=nr.t[:, 0:1], in_=hb.t[:, 256:257], func=AF.Abs), r=[hb.s()], w=NR)
                    k.V(lambda e, nr=nr, thr=thr: e.tensor_tensor(out=nr.t[:, 0:1], in0=nr.t[:, 0:1], in1=thr, op=ALU.max), r=NR + GT, w=NR)
                    k.V(lambda e, nr=nr: e.reciprocal(out=nr.t[:, 1:2], in_=nr.t[:, 0:1]), r=NR, w=NR)
                    k.A(lambda e, hb=hb, nr=nr: e.activation(out=sqv.t[:, :], in_=hb.t[:, 0:256], func=AF.Square, scale=nr.t[:, 1:2]), r=[hb.s()] + NR, w=[sqv.s()])
                    k.V(lambda e, nr=nr: e.tensor_reduce(out=nr.t[:, 2:3], in_=sqv.t[:, :], axis=AX.X, op=ALU.add), r=[sqv.s()], w=NR)
                    k.A(lambda e, nr=nr: e.activation(out=nr.t[:, 3:4], in_=nr.t[:, 2:3], func=AF.Sqrt, bias=epsc.t[:, 0:1], scale=1.0 / 256), r=NR + [epsc.s()], w=NR)
                    k.V(lambda e, nr=nr: e.reciprocal(out=nr.t[:, 4:5], in_=nr.t[:, 3:4]), r=NR, w=NR)
                    k.V(lambda e, nr=nr: e.tensor_tensor(out=nr.t[:, 5:6], in0=nr.t[:, 4:5], in1=nr.t[:, 1:2], op=ALU.mult), r=NR, w=NR)
                    k.V(lambda e, hb=hb, nr=nr, hn_=hn_, hd=hd: e.scalar_tensor_tensor(out=hn_.t[:, :], in0=hb.t[:, 0:256], scalar=nr.t[:, 5:6], in1=gh.t[:, hd * 256:(hd + 1) * 256], op0=ALU.mult, op1=ALU.mult),
                        r=[hb.s(), gh.s()] + NR, w=[hn_.s()])
                    for et in range(2):
                        k.M(lambda e, et=et, hn_=hn_: e.transpose(out=psT.t[:, et * 128:(et + 1) * 128], in_=hn_.t[:, et * 128:(et + 1) * 128], identity=ident_bf.t[:, :]),
                            r=[hn_.s(), ident_bf.s()], w=[psT.s()])
                    for et in range(2):
                        ft = 2 * hd + et
                        k.V(lambda e, et=et, ft=ft, tg=tg, c0=c0: e.scalar_tensor_tensor(out=tg.t[:, :], in0=CS.t[:, ft, c0:c0 + 128], scalar=lpc(LP_SKIP + ft), in1=psT.t[:, et * 128:(et + 1) * 128], op0=ALU.mult, op1=ALU.add),
                            r=[CS.s(ft, ft + 1), lpt.s(), psT.s()], w=[tg.s()])
                        k.V(lambda e, ft=ft, tg=tg, c0=c0: e.tensor_tensor(out=apre.t[:, ft, c0:c0 + 128], in0=tg.t[:, :], in1=CS.t[:, 8 + ft, c0:c0 + 128], op=ALU.mult),
                            r=[tg.s(), CS.s(8 + ft, 9 + ft)], w=[apre.s(ft, ft + 1)])
                    for kt2 in range(2):
                        ubk = k.bank()
                        k.M(lambda e, ubk=ubk, kt2=kt2, kt_=kt_, ve=ve: e.matmul(ubk.t[:, 0:257], lhsT=kt_.t[:, kt2 * 128:(kt2 + 1) * 128], rhs=ve.t[:, :], start=True, stop=True),
                            r=[kt_.s(), ve.s()], w=[ubk.s()])
                        sl = (Cst[l].b, kt2 * 4 + hd, kt2 * 4 + hd + 1)
                        k.V(lambda e, ubk=ubk, kt2=kt2, c0b=c0b, hd=hd: e.scalar_tensor_tensor(out=Cst[l].t[:, kt2, hd, :], in0=Cst[l].t[:, kt2, hd, :], scalar=c0b, in1=ubk.t[:, 0:257], op0=ALU.mult, op1=ALU.add),
                            r=[ubk.s(), sl] + GT, w=[sl])
                if last:
                    head_sample(l, hd, QK, wq_s, wk_s, wv_s, wsl)
            if last:
                for hd in range(4):
                    cpv = o_Cp.t[l, hd].rearrange("(kt p) e -> p kt e", p=128)
                    k.Dm(lambda e, cpv=cpv, hd=hd: e.dma_start(out=cpv, in_=Cst[l].t[:, :, hd, 0:256]), r=[Cst[l].s()], w=[o_Cp.s(l * 4 + hd, l * 4 + hd + 1)])
                k.V(lambda e: e.tensor_copy(out=npo.t[:, :].rearrange("p (a b) -> p a b", a=2), in_=Cst[l].t[:, :, :, 256]), r=[Cst[l].s()], w=[npo.s()])
                k.Dm(lambda e: e.dma_start(out=o_np.t[l], in_=npo.t[:, :]), r=[npo.s()], w=[o_np.s(l, l + 1)])
                k.Dm(lambda e: e.dma_start(out=o_mp.t[l].rearrange("(h o) -> h o", o=1), in_=mst.t[:, l:l + 1]), r=[mst.s(l, l + 1)], w=[o_mp.s(l, l + 1)])
                k.Dm(lambda e: e.dma_start(out=o_ns.t[l], in_=nnew.t[:, :]), r=[nnew.s()], w=[o_ns.s(l, l + 1)])

        def head_sample(l, hd, QK, wq_s, wk_s, wv_s, wsl):
            gs = sm["gs"]; GS = [gs.s()]
            e_hd = gs.t[:, 20 + hd:21 + hd]; c0_hd = gs.t[:, 24 + hd:25 + hd]; thr_hd = gs.t[:, 32 + hd:33 + hd]
            Qs = small("Qs", [NS, 256]); Ks = small("Ks", [NS, 256]); Vs = small("Vs", [NS, 256]); Ke = small("Ke", [NS, 256])
            num = small("num", [NS, 256]); hnos = small("hnos", [NS, 256], BF16)
            sr = small("sr", [NS, 12])
            SR = [sr.s()]
            bq = k.bank(); bkk = k.bank(); bv = k.bank()
            for kt2 in range(2):
                k.M(lambda e, kt2=kt2, bq=bq: e.matmul(bq.t[0:NS, 0:256], lhsT=CS.t[:, 2 * hd + kt2, TBP:TB], rhs=wsl(wq_s, hd, kt2, 0, 256), start=(kt2 == 0), stop=(kt2 == 1)),
                    r=[wq_s.s(), CS.s(2 * hd + kt2, 2 * hd + kt2 + 1)], w=[bq.s()])
            for kt2 in range(2):
                k.M(lambda e, kt2=kt2, bkk=bkk: e.matmul(bkk.t[0:NS, 0:256], lhsT=CS.t[:, 2 * hd + kt2, TBP:TB], rhs=wsl(wk_s, hd, kt2, 0, 256), start=(kt2 == 0), stop=(kt2 == 1)),
                    r=[wk_s.s(), CS.s(2 * hd + kt2, 2 * hd + kt2 + 1)], w=[bkk.s()])
            for kt2 in range(2):
                k.M(lambda e, kt2=kt2, bv=bv: e.matmul(bv.t[0:NS, 0:256], lhsT=uab.t[:, 2 * hd + kt2, 3 + TBP:3 + TB], rhs=wsl(wv_s, hd, kt2, 0, 256), start=(kt2 == 0), stop=(kt2 == 1)),
                    r=[wv_s.s(), uab.s(2 * hd + kt2, 2 * hd + kt2 + 1)], w=[bv.s()])
            k.A(lambda e: e.activation(out=Qs.t[:, :], in_=bq.t[0:NS, 0:256], func=AF.Copy), r=[bq.s()], w=[Qs.s()])
            k.A(lambda e: e.activation(out=Ks.t[:, :], in_=bkk.t[0:NS, 0:256], func=AF.Copy, scale=0.0625), r=[bkk.s()], w=[Ks.s()])
            k.V(lambda e: e.tensor_copy(out=Vs.t[:, :], in_=bv.t[0:NS, 0:256]), r=[bv.s()], w=[Vs.s()])
            k.V(lambda e: e.tensor_copy(out=vsb.t[:, 0:256], in_=bv.t[0:NS, 0:256]), r=[bv.s()], w=[vsb.s()])
            k.V(lambda e: e.tensor_tensor(out=num.t[:, :], in0=Qs.t[:, :], in1=Ks.t[:, :], op=ALU.mult), r=[Qs.s(), Ks.s()], w=[num.s()])
            k.V(lambda e: e.tensor_reduce(out=sr.t[:, 0:1], in_=num.t[:, :], axis=AX.X, op=ALU.add), r=[num.s()], w=SR)
            k.V(lambda e: e.tensor_scalar(out=Ke.t[:, :], in0=Ks.t[:, :], scalar1=e_hd, scalar2=None, op0=ALU.mult), r=[Ks.s()] + GS, w=[Ke.s()])
            for kt2 in range(2):
                k.V(lambda e, kt2=kt2: e.tensor_tensor(out=qmask.t[:, kt2, :, :], in0=QK.t[:, kt2:kt2 + 1, TBP:TB].to_broadcast([128, NS, NS]), in1=d16, op=ALU.mult),
                    r=[QK.s(kt2, kt2 + 1), cst.s()], w=[qmask.s()])
            HS = psA
            for j in range(NS):
                cj = VT(G1f[j % 3].t[:, 0:514].rearrange("p (a b) -> p a b", a=2), G1.b, 2 * (j % 3), 2 * (j % 3) + 2)
                cn = VT(G1f[3 + j % 2].t[:, 0:514].rearrange("p (a b) -> p a b", a=2), G1.b, 2 * (3 + j % 2), 2 * (3 + j % 2) + 2)
                rq = 5 + j % 2
                rj = VT(G1.t[:, 2 * rq, 0:514].rearrange("p (a b) -> p a b", a=2), G1.b, 2 * rq, 2 * rq + 1)
                km = kmask[j % 2]
                idx = (j * 4 + hd) * 2
                src = stC.t[l, j, hd].rearrange("(kt p) e -> p kt e", p=128)
                k.Dm(lambda e, cj=cj, src=src: e.dma_start(out=cj.t[:, :, 0:256], in_=src), w=[cj.s()])
                k.V(lambda e, cj=cj, idx=idx: e.tensor_copy(out=cj.t[:, :, 256], in_=nall.t[:, idx:idx + 2]), r=[nall.s()], w=[cj.s()])
                k.A(lambda e, cj=cj, rj=rj: e.activation(out=rj.t[:, :, :], in_=cj.t[:, :, :], func=AF.Copy), r=[cj.s()], w=[rj.s()])
                for kt2 in range(2):
                    k.M(lambda e, j=j, kt2=kt2, rj=rj: e.matmul(HS.t[0:NS, 0:257], lhsT=qmask.t[:, kt2, j, :], rhs=rj.t[:, kt2, :], start=(j == 0 and kt2 == 0), stop=(j == NS - 1 and kt2 == 1)),
                        r=[qmask.s(), rj.s()], w=[HS.s()])
                k.V(lambda e, km=km, j=j: e.tensor_scalar(out=km.t[:, :], in0=Ke.t[:, :], scalar1=cst.t[0:NS, C_ID + j:C_ID + j + 1], scalar2=None, op0=ALU.mult),
                    r=[Ke.s(), cst.s()], w=[km.s()])
                for kt2 in range(2):
                    ubk = k.bank()
                    k.M(lambda e, ubk=ubk, km=km, kt2=kt2: e.matmul(ubk.t[:, 0:257], lhsT=km.t[:, kt2 * 128:(kt2 + 1) * 128], rhs=vsb.t[:, :], start=True, stop=True),
                        r=[km.s(), vsb.s()], w=[ubk.s()])
                    k.V(lambda e, ubk=ubk, kt2=kt2, cj=cj, cn=cn, j=j: e.scalar_tensor_tensor(out=cn.t[:, kt2, :], in0=cj.t[:, kt2, :], scalar=c0bc.t[:, j * 4 + hd:j * 4 + hd + 1], in1=ubk.t[:, 0:257], op0=ALU.mult, op1=ALU.add),
                        r=[ubk.s(), cj.s(), c0bc.s()], w=[cn.s()])
                dst = o_Cs.t[l, j, hd].rearrange("(kt p) e -> p kt e", p=128)
                slot = (l * NS + j) * 4 + hd
                k.Dm(lambda e, cn=cn, dst=dst: e.dma_start(out=dst, in_=cn.t[:, :, 0:256]), r=[cn.s()], w=[o_Cs.s(slot, slot + 1)])
                k.V(lambda e, cn=cn, idx=idx: e.tensor_copy(out=nnew.t[:, idx:idx + 2], in_=cn.t[:, :, 256]), r=[cn.s()], w=[nnew.s()])
            k.V(lambda e: e.tensor_scalar(out=num.t[:, :], in0=HS.t[0:NS, 0:256], scalar1=c0_hd, scalar2=None, op0=ALU.mult), r=[HS.s()] + GS, w=[num.s()])
            k.V(lambda e: e.tensor_tensor(out=sr.t[:, 1:2], in0=sr.t[:, 0:1], in1=e_hd, op=ALU.mult), r=SR + GS, w=SR)
            k.V(lambda e: e.scalar_tensor_tensor(out=num.t[:, :], in0=Vs.t[:, :], scalar=sr.t[:, 1:2], in1=num.t[:, :], op0=ALU.mult, op1=ALU.add), r=[Vs.s(), num.s()] + SR, w=[num.s()])
            k.V(lambda e: e.scalar_tensor_tensor(out=sr.t[:, 2:3], in0=HS.t[0:NS, 256:257], scalar=c0_hd, in1=sr.t[:, 1:2], op0=ALU.mult, op1=ALU.add), r=[HS.s()] + SR + GS, w=SR)
            k.A(lambda e: e.activation(out=sr.t[:, 3:4], in_=sr.t[:, 2:3], func=AF.Abs), r=SR, w=SR)
            k.V(lambda e: e.tensor_tensor(out=sr.t[:, 3:4], in0=sr.t[:, 3:4], in1=thr_hd, op=ALU.max), r=SR + GS, w=SR)
            k.V(lambda e: e.reciprocal(out=sr.t[:, 4:5], in_=sr.t[:, 3:4]), r=SR, w=SR)
            k.A(lambda e: e.activation(out=sqv.t[0:NS, :], in_=num.t[:, :], func=AF.Square, scale=sr.t[:, 4:5]), r=[num.s()] + SR, w=[sqv.s()])
            k.V(lambda e: e.tensor_reduce(out=sr.t[:, 5:6], in_=sqv.t[0:NS, :], axis=AX.X, op=ALU.add), r=[sqv.s()], w=SR)
            k.A(lambda e: e.activation(out=sr.t[:, 6:7], in_=sr.t[:, 5:6], func=AF.Sqrt, bias=epsc.t[0:NS, 0:1], scale=1.0 / 256), r=SR + [epsc.s()], w=SR)
            k.V(lambda e: e.reciprocal(out=sr.t[:, 7:8], in_=sr.t[:, 6:7]), r=SR, w=SR)
            k.V(lambda e: e.tensor_tensor(out=sr.t[:, 8:9], in0=sr.t[:, 7:8], in1=sr.t[:, 4:5], op=ALU.mult), r=SR, w=SR)
            k.V(lambda e: e.scalar_tensor_tensor(out=hnos.t[:, :], in0=num.t[:, :], scalar=sr.t[:, 8:9], in1=gh.t[0:NS, hd * 256:(hd + 1) * 256], op0=ALU.mult, op1=ALU.mult),
                r=[num.s(), gh.s()] + SR, w=[hnos.s()])
            for et in range(2):
                k.M(lambda e, et=et: e.transpose(out=psT.t[:, 512 + et * NS:512 + (et + 1) * NS], in_=hnos.t[:, et * 128:(et + 1) * 128], identity=ident_bf.t[0:NS, 0:NS]),
                    r=[hnos.s(), ident_bf.s()], w=[psT.s()])
            tg = tmpg[0]
            for et in range(2):
                ft = 2 * hd + et
                k.V(lambda e, et=et, ft=ft: e.scalar_tensor_tensor(out=tg.t[:, 0:NS], in0=CS.t[:, ft, TBP:TB], scalar=lpc(LP_SKIP + ft), in1=psT.t[:, 512 + et * NS:512 + (et + 1) * NS], op0=ALU.mult, op1=ALU.add),
                    r=[CS.s(ft, ft + 1), lpt.s(), psT.s()], w=[tg.s()])
                k.V(lambda e, ft=ft: e.tensor_tensor(out=apre.t[:, ft, TBP:TB], in0=tg.t[:, 0:NS], in1=CS.t[:, 8 + ft, TBP:TB], op=ALU.mult),
                    r=[tg.s(), CS.s(8 + ft, 9 + ft)], w=[apre.s(ft, ft + 1)])

        def mixer(blk, l):
            k.Dm(lambda e: e.dma_start(out=lpt.t[:, :], in_=lp.t[l]), w=[lpt.s()])
            k.Dm(lambda e: e.dma_start(out=gh.t[:, :], in_=ghd.t[l]), w=[gh.s()])
            k.Wm(lambda e: e.dma_start(out=wif.t[:, :], in_=w_if.t[l]), w=[wif.s()])
            rms(blk, l, xn)
            k.V(lambda e: e.tensor_copy(out=uab.t[:, :, 0:3], in_=hist.t[:, l, :, :]), r=[hist.s(l, l + 1)], w=[uab.s()])
            fm_proj(blk, [w_in_t.t[l, j] for j in range(8)], 16, xn, epi_act(uab, AF.Copy, 0, col0=3))
            fm_proj(blk, [w_in_t.t[l, 16 + j] for j in range(8)], 16, xn, epi_act(ub, AF.Copy))
            s5_stage(blk, l)
            conv_stage(blk, l)
            fm_proj(blk, [w_in_t.t[l, 8 + j] for j in range(8)], 16, xn, epi_act(CS, AF.Sigmoid, base=8))
            mlstm_stage(blk, l)
            if STOP_AFTER == ("mlstm", l):
                return
            fm_proj(blk, [w_in_t.t[l, 24 + j] for j in range(16)], 16, xn, epi_act(G1, AF.Sigmoid))

            def epi_mul(mt, c0, n, bk):
                k.V(lambda e, mt=mt, c0=c0, n=n, bk=bk: e.tensor_tensor(out=G1.t[:, mt, c0:c0 + n], in0=bk.t[:, :n], in1=G1.t[:, mt, c0:c0 + n], op=ALU.mult),
                    r=[bk.s(), G1.s(mt, mt + 1)], w=[G1.s(mt, mt + 1)])
            fm_proj(blk, [w_proj_t.t[l, j] for j in range(8)], 8, apre, epi_mul, mt_per_slot=2)
            wv_slot = wg_slot = None
            for j in range(16):
                wb = k.wload(w_in_t.t[l, 40 + j])
                if j % 2 == 0:
                    wv_slot = k.wload(w_glu_t.t[l, j // 2]); wg_slot = k.wload(w_glu_t.t[l, 8 + j // 2])
                mm = j % 2
                for (c0, n) in chunks(blk):
                    bb = k.bank(); bv = k.bank(); bg = k.bank()
                    for kt in range(16):
                        k.M(lambda e, wb=wb, kt=kt, c0=c0, n=n, bb=bb: e.matmul(bb.t[:, :n], lhsT=wb.t[:, kt * 128:(kt + 1) * 128], rhs=xn.t[:, kt, c0:c0 + n], start=(kt == 0), stop=(kt == 15)),
                            r=[wb.s(), xn.s(kt, kt + 1)], w=[bb.s()])
                    for kt in range(8):
                        off = (mm * 8 + kt) * 128
                        k.M(lambda e, ws=wv_slot, off=off, kt=kt, c0=c0, n=n, bv=bv: e.matmul(bv.t[:, :n], lhsT=ws.t[:, off:off + 128], rhs=ub.t[:, kt, c0:c0 + n], start=(kt == 0), stop=(kt == 7)),
                            r=[wv_slot.s(), ub.s(kt, kt + 1)], w=[bv.s()])
                    for kt in range(8):
                        off = (mm * 8 + kt) * 128
                        k.M(lambda e, ws=wg_slot, off=off, kt=kt, c0=c0, n=n, bg=bg: e.matmul(bg.t[:, :n], lhsT=ws.t[:, off:off + 128], rhs=ub.t[:, kt, c0:c0 + n], start=(kt == 0), stop=(kt == 7)),
                            r=[wg_slot.s(), ub.s(kt, kt + 1)], w=[bg.s()])
                    s_b = SC[1]; s_g = SC[2]
                    k.A(lambda e, n=n, bb=bb: e.activation(out=s_b.t[:, :n], in_=bb.t[:, :n], func=AF.Sigmoid), r=[bb.s()], w=[s_b.s()])
                    k.A(lambda e, n=n, bg=bg: e.activation(out=s_g.t[:, :n], in_=bg.t[:, :n], func=AF.Sigmoid), r=[bg.s()], w=[s_g.s()])
                    k.V(lambda e, n=n, bv=bv: e.tensor_tensor(out=s_g.t[:, :n], in0=bv.t[:, :n], in1=s_g.t[:, :n], op=ALU.mult), r=[bv.s(), s_g.s()], w=[s_g.s()])
                    k.V(lambda e, n=n: e.tensor_tensor(out=s_g.t[:, :n], in0=s_g.t[:, :n], in1=s_b.t[:, :n], op=ALU.mult), r=[s_g.s(), s_b.s()], w=[s_g.s()])
                    k.V(lambda e, n=n, c0=c0, j=j: e.tensor_tensor(out=G1.t[:, j, c0:c0 + n], in0=s_g.t[:, :n], in1=G1.t[:, j, c0:c0 + n], op=ALU.add), r=[s_g.s(), G1.s(j, j + 1)], w=[G1.s(j, j + 1)])

            def epi_addh(mt, c0, n, bk):
                k.V(lambda e, mt=mt, c0=c0, n=n, bk=bk: e.tensor_tensor(out=h.t[:, mt, c0:c0 + n], in0=bk.t[:, :n], in1=h.t[:, mt, c0:c0 + n], op=ALU.add),
                    r=[bk.s(), h.s(mt, mt + 1)], w=[h.s(mt, mt + 1)])
            fm_proj(blk, [w_out_t.t[l, j] for j in range(16)], 16, G1, epi_addh)

        def ffn_group_list():
            gl = []
            f = 0
            while f < DFF_T:
                gl.append(list(range(f, min(f + 2, DFF_T))))
                f += 2
            return gl

        def ffn(blk, wg_t, wu_t, wd_t, gate_tiles=None, parity0=0):
            for gi, grp in enumerate(ffn_group_list()):
                hb0 = ((gi + parity0) % 4) * 2
                for fi, f in enumerate(grp):
                    wg = k.wload(wg_t[f]); wu = k.wload(wu_t[f])
                    for (c0, n) in chunks(blk):
                        bg = k.bank(); bu = k.bank()
                        for kt in range(16):
                            k.M(lambda e, wg=wg, kt=kt, c0=c0, n=n, bg=bg: e.matmul(bg.t[:, :n], lhsT=wg.t[:, kt * 128:(kt + 1) * 128], rhs=xn.t[:, kt, c0:c0 + n], start=(kt == 0), stop=(kt == 15)),
                                r=[wg.s(), xn.s(kt, kt + 1)], w=[bg.s()])
                        for kt in range(16):
                            k.M(lambda e, wu=wu, kt=kt, c0=c0, n=n, bu=bu: e.matmul(bu.t[:, :n], lhsT=wu.t[:, kt * 128:(kt + 1) * 128], rhs=xn.t[:, kt, c0:c0 + n], start=(kt == 0), stop=(kt == 15)),
                                r=[wu.s(), xn.s(kt, kt + 1)], w=[bu.s()])
                        sg = SC[1 + (fi % 2)]
                        k.A(lambda e, n=n, bg=bg, sg=sg: e.activation(out=sg.t[:, :n], in_=bg.t[:, :n], func=AF.Silu), r=[bg.s()], w=[sg.s()])
                        if gate_tiles is None:
                            k.V(lambda e, n=n, c0=c0, bu=bu, sg=sg, hb0=hb0, fi=fi: e.tensor_tensor(out=apre.t[:, hb0 + fi, c0:c0 + n], in0=sg.t[:, :n], in1=bu.t[:, :n], op=ALU.mult),
                                r=[sg.s(), bu.s()], w=[apre.s(hb0 + fi, hb0 + fi + 1)])
                        else:
                            k.V(lambda e, n=n, bu=bu, sg=sg: e.tensor_tensor(out=sg.t[:, :n], in0=sg.t[:, :n], in1=bu.t[:, :n], op=ALU.mult), r=[sg.s(), bu.s()], w=[sg.s()])
                            k.V(lambda e, n=n, c0=c0, sg=sg, hb0=hb0, fi=fi: e.tensor_tensor(out=apre.t[:, hb0 + fi, c0:c0 + n], in0=sg.t[:, :n], in1=gate_tiles.t[:, c0:c0 + n], op=ALU.mult),
                                r=[sg.s(), gate_tiles.s()], w=[apre.s(hb0 + fi, hb0 + fi + 1)])
                wds = [k.wload(wd_t[f]) for f in grp]
                for dt_ in range(16):
                    for (c0, n) in chunks(blk):
                        bk = k.bank()
                        for fi in range(len(grp)):
                            k.M(lambda e, wd=wds[fi], dt_=dt_, fi=fi, c0=c0, n=n, bk=bk, hb0=hb0: e.matmul(bk.t[:, :n], lhsT=wd.t[:, dt_ * 128:(dt_ + 1) * 128], rhs=apre.t[:, hb0 + fi, c0:c0 + n], start=(fi == 0), stop=(fi == len(grp) - 1)),
                                r=[wds[fi].s(), apre.s(hb0 + fi, hb0 + fi + 1)], w=[bk.s()])
                        k.V(lambda e, dt_=dt_, c0=c0, n=n, bk=bk: e.tensor_tensor(out=h.t[:, dt_, c0:c0 + n], in0=bk.t[:, :n], in1=h.t[:, dt_, c0:c0 + n], op=ALU.add),
                            r=[bk.s(), h.s(dt_, dt_ + 1)], w=[h.s(dt_, dt_ + 1)])

        def moe(blk):
            last = blk == NB - 1
            gT = small("gT", [8, TB])
            rt = small("rt", [128, 48])
            RT = [rt.s()]
            tch = [(c * 128, 128) for c in range(4)] + ([(TBP, NS)] if last else [])
            for (c0, n) in tch:
                bk = k.bank()
                for kt in range(16):
                    k.M(lambda e, kt=kt, c0=c0, n=n, bk=bk: e.matmul(bk.t[0:n, 0:8], lhsT=xn.t[:, kt, c0:c0 + n], rhs=wrt.t[:, kt * 8:kt * 8 + 8], start=(kt == 0), stop=(kt == 15)),
                        r=[xn.s(kt, kt + 1), wrt.s()], w=[bk.s()])
                R = (lambda n: (lambda a, b: rt.t[0:n, a:b]))(n)
                VR = lambda fn, extra=(): k.V(fn, r=RT + list(extra), w=RT)
                VR(lambda e, bk=bk, n=n, R=R: e.tensor_tensor(out=R(0, 8), in0=bk.t[0:n, 0:8], in1=lpt.t[0:n, LP_BRT:LP_BRT + 8], op=ALU.add), [bk.s(), lpt.s()])
                VR(lambda e, R=R: e.tensor_reduce(out=R(40, 41), in_=R(0, 8), axis=AX.X, op=ALU.max))
                VR(lambda e, R=R: e.tensor_scalar(out=R(8, 16), in0=R(0, 8), scalar1=R(40, 41), scalar2=None, op0=ALU.is_equal))
                VR(lambda e, R=R: e.scalar_tensor_tensor(out=R(16, 24), in0=R(8, 16), scalar=-1e30, in1=R(0, 8), op0=ALU.mult, op1=ALU.add))
                VR(lambda e, R=R: e.tensor_reduce(out=R(41, 42), in_=R(16, 24), axis=AX.X, op=ALU.max))
                VR(lambda e, R=R: e.tensor_scalar(out=R(24, 32), in0=R(16, 24), scalar1=R(41, 42), scalar2=None, op0=ALU.is_equal))
                VR(lambda e, R=R: e.tensor_tensor(out=R(42, 43), in0=R(41, 42), in1=R(40, 41), op=ALU.subtract))
                k.A(lambda e, R=R: e.activation(out=R(43, 44), in_=R(42, 43), func=AF.Sigmoid, scale=-1.0), r=RT, w=RT)
                VR(lambda e, R=R: e.tensor_scalar(out=R(44, 45), in0=R(43, 44), scalar1=-1.0, scalar2=1.0, op0=ALU.mult, op1=ALU.add))
                VR(lambda e, R=R: e.tensor_scalar(out=R(32, 40), in0=R(8, 16), scalar1=R(43, 44), scalar2=None, op0=ALU.mult))
                VR(lambda e, R=R: e.scalar_tensor_tensor(out=R(32, 40), in0=R(24, 32), scalar=R(44, 45), in1=R(32, 40), op0=ALU.mult, op1=ALU.add))
                b2 = k.bank()
                k.M(lambda e, b2=b2, n=n, R=R: e.matmul(b2.t[0:8, 0:n], lhsT=R(32, 40), rhs=cst.t[0:n, C_ID:C_ID + n], start=True, stop=True), r=RT + [cst.s()], w=[b2.s()])
                k.V(lambda e, b2=b2, n=n, c0=c0: e.tensor_copy(out=gT.t[:, c0:c0 + n], in_=b2.t[0:8, 0:n]), r=[b2.s()], w=[gT.s()])
            sel = small("sel", [8, 128])
            for ex in range(8):
                k.V(lambda e, ex=ex: e.tensor_scalar(out=sel.t[:, :], in0=ones_f.t[0:8, :], scalar1=cst.t[0:8, C_ID + ex:C_ID + ex + 1], scalar2=None, op0=ALU.mult),
                    r=[ones_f.s(), cst.s()], w=[sel.s()])
                for (c0, n) in chunks(blk):
                    bk = k.bank()
                    k.M(lambda e, ex=ex, c0=c0, n=n, bk=bk: e.matmul(bk.t[:, :n], lhsT=sel.t[:, :], rhs=gT.t[:, c0:c0 + n], start=True, stop=True), r=[sel.s(), gT.s()], w=[bk.s()])
                    k.A(lambda e, ex=ex, c0=c0, n=n, bk=bk: e.activation(out=CSf[ex].t[:, c0:c0 + n], in_=bk.t[:, :n], func=AF.Copy), r=[bk.s()], w=[CSf[ex].s()])
            for ex in range(8):
                ffn(blk, moeg.t[ex], moeu.t[ex], moed.t[ex], gate_tiles=CSf[ex], parity0=ex)

        def ple(blk, l):
            last = blk == NB - 1
            pv = pTp.t[l].rearrange("(kt p) t -> p kt t", p=128)
            pbv = [SB[2], SB[3]]
            for kt in range(2):
                k.Wm(lambda e, kt=kt: e.dma_start(out=pbv[kt].t[:, 0:TBP], in_=pv[:, kt, blk * TBP:(blk + 1) * TBP]), w=[pbv[kt].s()])
                if last:
                    psv = pTs.t[l].rearrange("(kt p) t -> p kt t", p=128)
                    k.Wm(lambda e, kt=kt, psv=psv: e.dma_start(out=pbv[kt].t[:, TBP:TB], in_=psv[:, kt, :]), w=[pbv[kt].s()])
            rms(blk, 4 + l, xn)
            wple = [None, None]
            for mt in range(16):
                if mt % 8 == 0:
                    if mt == 8:
                        k.unpin(wple[0])
                    wple[mt // 8] = k.wload(w_ple_t.t[l, mt // 8], pin=True)
                wp = k.wload(w_pg_t.t[l, mt])
                for (c0, n) in chunks(blk):
                    bg = k.bank(); bp = k.bank()
                    for kt in range(16):
                        k.M(lambda e, wp=wp, kt=kt, c0=c0, n=n, bg=bg: e.matmul(bg.t[:, :n], lhsT=wp.t[:, kt * 128:(kt + 1) * 128], rhs=xn.t[:, kt, c0:c0 + n], start=(kt == 0), stop=(kt == 15)),
                            r=[wp.s(), xn.s(kt, kt + 1)], w=[bg.s()])
                    wsl_ = wple[mt // 8]
                    for kt in range(2):
                        off = ((mt % 8) * 2 + kt) * 128
                        k.M(lambda e, wsl_=wsl_, off=off, kt=kt, c0=c0, n=n, bp=bp: e.matmul(bp.t[:, :n], lhsT=wsl_.t[:, off:off + 128], rhs=pbv[kt].t[:, c0:c0 + n], start=(kt == 0), stop=(kt == 1)),
                            r=[wsl_.s(), pbv[kt].s()], w=[bp.s()])
                    sg = SC[1 + mt % 2]
                    k.A(lambda e, n=n, bg=bg, sg=sg: e.activation(out=sg.t[:, :n], in_=bg.t[:, :n], func=AF.Sigmoid), r=[bg.s()], w=[sg.s()])
                    k.V(lambda e, n=n, bp=bp, sg=sg: e.tensor_tensor(out=sg.t[:, :n], in0=bp.t[:, :n], in1=sg.t[:, :n], op=ALU.mult), r=[bp.s(), sg.s()], w=[sg.s()])
                    k.V(lambda e, n=n, c0=c0, sg=sg, mt=mt: e.tensor_tensor(out=h.t[:, mt, c0:c0 + n], in0=sg.t[:, :n], in1=h.t[:, mt, c0:c0 + n], op=ALU.add),
                        r=[sg.s(), h.s(mt, mt + 1)], w=[h.s(mt, mt + 1)])
            k.unpin(wple[1])

        xv = xTp.t.rearrange("(kt p) t -> p kt t", p=128)
        xsv = xTs.t.rearrange("(kt p) t -> p kt t", p=128)
        yv = o_yTp.t.rearrange("(kt p) t -> p kt t", p=128)
        ysv = o_yTs.t.rearrange("(kt p) t -> p kt t", p=128)
        stop = False
        for blk in range(NB):
            last = blk == NB - 1
            k.Dm(lambda e, blk=blk: e.dma_start(out=h.t[:, :, 0:TBP], in_=xv[:, :, blk * TBP:(blk + 1) * TBP]), w=[h.s()])
            if last:
                k.Dm(lambda e: e.dma_start(out=h.t[:, :, TBP:TB], in_=xsv), w=[h.s()])
            for l in range(DEPTH):
                mixer(blk, l)
                if STOP_AFTER in (("mlstm", l), ("mixer", l)):
                    stop = True
                    break
                rms(blk, 2 + l, xn)
                if l % 2 == 0:
                    ffn(blk, ffg.t, ffu.t, ffd.t)
                else:
                    moe(blk)
                if STOP_AFTER == ("ffn", l):
                    stop = True
                    break
                ple(blk, l)
                if STOP_AFTER == ("layer", l):
                    stop = True
                    break

            def out32(kt, c0, n, rs, blk=blk):
                yo = SC[1 + kt % 2]
                k.V(lambda e, kt=kt, c0=c0, n=n, rs=rs, yo=yo: e.scalar_tensor_tensor(
                    out=yo.t[:, :n], in0=h.t[:, kt, c0:c0 + n], scalar=gn.t[:, 6 * 16 + kt:6 * 16 + kt + 1], in1=rs.t[:, :n], op0=ALU.mult, op1=ALU.mult),
                    r=[h.s(kt, kt + 1), rs.s(), gn.s()], w=[yo.s()])
                if c0 == 0:
                    k.Dm(lambda e, kt=kt, yo=yo, blk=blk: e.dma_start(out=yv[:, kt, blk * TBP:(blk + 1) * TBP], in_=yo.t[:, 0:TBP]), r=[yo.s()], w=[o_yTp.s(blk * 16 + kt, blk * 16 + kt + 1)])
                else:
                    k.Dm(lambda e, kt=kt, yo=yo: e.dma_start(out=ysv[:, kt, :], in_=yo.t[:, 0:NS]), r=[yo.s()], w=[o_yTs.s(kt, kt + 1)])
            rms(blk, 6, None, out32=out32)
        k.P.op("sp", None, reads=[o.s() for o in all_outs])
        k.P.emit(nc)
        print("instr counts:", {e: len(v) for e, v in k.P.q.items()})
    return nc


def _fm(v):
    return np.ascontiguousarray(v.reshape(-1, 128).T)


def _wt(W, cols=None):
    Wc = W if cols is None else W[:, cols]
    K = Wc.shape[0]
    nt = Wc.shape[1] // 128
    return np.ascontiguousarray(Wc.reshape(K // 128, 128, nt, 128).transpose(2, 1, 0, 3).reshape(nt, 128, K))


def _pack(T, g):
    nt, _, K = T.shape
    return np.ascontiguousarray(T.reshape(nt // g, g, 128, K).transpose(0, 2, 1, 3).reshape(nt // g, 128, g * K))


def _consts():
    c = np.zeros((128, NCONST), np.float32)
    c[:, C_ID:C_ID + 128] = np.eye(128)
    c[:, C_TRI:C_TRI + 128] = np.triu(np.ones((128, 128)))
    c[:, C_JJ:C_JJ + 512] = np.arange(1, 513)[None, :]
    g8 = np.arange(128) // 16
    c[:, C_M2:C_M2 + 8] = (g8[:, None] == np.arange(8)[None, :])
    c[:, C_D16:C_D16 + 256] = np.eye(16).reshape(1, 256)
    return c


def prep_shared(inp):
    f32 = np.float32
    L = DEPTH
    sh = {}
    sh["gains"] = np.concatenate([_fm(inp["g_mix"][0]), _fm(inp["g_mix"][1]), _fm(inp["g_ffn"][0]), _fm(inp["g_ffn"][1]),
                                  _fm(inp["g_ple"][0]), _fm(inp["g_ple"][1]), _fm(inp["g_final"])], axis=1).astype(f32)
    sh["consts"] = _consts()
    lp = np.zeros((L, 128, NLP), f32)
    for l in range(L):
        lp[l, :, LP_CW:LP_CW + 32] = inp["conv_w"][l].reshape(4, 8, 128).transpose(2, 1, 0).reshape(128, 32)
        lp[l, :, LP_CB:LP_CB + 8] = _fm(inp["conv_b"][l])
        lp[l, :, LP_SKIP:LP_SKIP + 8] = _fm(inp["skip_a"][l])
        lp[l, :, LP_DSK:LP_DSK + 8] = _fm(inp["s5_D"][l])
        lp[l, :, LP_BIF:LP_BIF + 4] = inp["b_i"][l][None, :]
        lp[l, :, LP_BIF + 4:LP_BIF + 8] = inp["b_f"][l][None, :]
        lp[l, :, LP_BRT:LP_BRT + 8] = inp["b_router"][0][None, :]
    sh["lp"] = lp
    sh["ghd"] = np.ascontiguousarray(np.broadcast_to(inp["g_head"].reshape(L, 1, 1024), (L, 128, 1024))).astype(f32)
    cols = np.concatenate([np.arange(0, 1024), np.arange(1024, 2048), np.arange(2056, 3080), np.arange(3080, 5128), np.arange(5128, 7176)])
    sh["w_in_t"] = np.stack([_wt(inp["w_in"][l], cols) for l in range(L)])
    sh["w_if"] = np.stack([inp["w_in"][l][:, 2048:2056].reshape(16, 128, 8).transpose(1, 0, 2).reshape(128, 128) for l in range(L)]).astype(f32)
    sh["wqkv"] = np.stack([np.stack([inp[nm][l].reshape(4, 2, 128, 256).transpose(2, 0, 1, 3).reshape(128, 2048) for nm in ("w_q", "w_k", "w_v")]) for l in range(L)]).astype(f32)
    sh["w_proj_t"] = np.stack([_pack(_wt(inp["w_proj_a"][l]), 2) for l in range(L)])
    sh["w_glu_t"] = np.stack([_pack(_wt(inp["w_glu_b"][l]), 2) for l in range(L)])
    sh["w_out_t"] = np.stack([_wt(inp["w_out"][l]) for l in range(L)])
    sh["w_pg_t"] = np.stack([_wt(inp["w_pg"][l]) for l in range(L)])
    sh["w_ple_t"] = np.stack([_pack(_wt(inp["w_ple"][l]), 8) for l in range(L)])
    sh["ffg"] = _wt(inp["w_ff_gate"][0]); sh["ffu"] = _wt(inp["w_ff_up"][0])
    sh["ffd"] = np.ascontiguousarray(inp["w_ff_down"][0].reshape(DFF_T, 128, 2048))
    sh["moeg"] = np.stack([_wt(inp["w_moe_gate"][0, e]) for e in range(8)])
    sh["moeu"] = np.stack([_wt(inp["w_moe_up"][0, e]) for e in range(8)])
    sh["moed"] = np.ascontiguousarray(inp["w_moe_down"][0].reshape(8, DFF_T, 128, 2048))
    sh["w_rt"] = np.ascontiguousarray(inp["w_router"][0].reshape(16, 128, 8).transpose(1, 0, 2).reshape(128, 128)).astype(f32)

    def L1(a):
        return a.reshape(32, 2, 64).transpose(1, 2, 0).reshape(128, 32)

    def L2(a):
        t = a.reshape(8, 8, 64).transpose(1, 0, 2)
        return np.broadcast_to(t[:, None, :, :], (8, 16, 8, 64)).reshape(128, 512)
    s5L1 = np.zeros((L, 128, 96), f32); s5L2 = np.zeros((L, 128, 1536), f32)
    s5B2 = np.zeros((L, 128, 1024), f32); s5C1 = np.zeros((L, 128, 1024), f32)
    for l in range(L):
        ldt = np.broadcast_to(inp["s5_log_dt"][l][:, None], (64, 64))
        for q, a in enumerate((inp["s5_A_re"][l], inp["s5_A_im"][l], ldt)):
            s5L1[l, :, q * 32:(q + 1) * 32] = L1(a)
            s5L2[l, :, q * 512:(q + 1) * 512] = L2(a)
        for q, b in enumerate((inp["s5_B_re"][l], inp["s5_B_im"][l])):
            s5B2[l, :, q * 512:(q + 1) * 512] = b.reshape(8, 8, 64, 16).transpose(1, 3, 0, 2).reshape(128, 512)
        for q, c in enumerate((inp["s5_C_re"][l], inp["s5_C_im"][l])):
            s5C1[l, :, q * 512:(q + 1) * 512] = c.reshape(32, 2, 16, 64).transpose(1, 3, 0, 2).reshape(128, 512)
    sh["s5L1"], sh["s5L2"], sh["s5B2"], sh["s5C1"] = s5L1, s5L2, s5B2, s5C1
    return {k_: np.ascontiguousarray(v, dtype=f32) for k_, v in sh.items()}


def kernel(**inp):
    f32 = np.float32
    L = DEPTH
    sh = prep_shared(inp)
    nc = build_program()
    in_maps = []
    cores = list(range(NCORES)) if RUN_CORES is None else list(RUN_CORES)
    for c in cores:
        sq = c % 4
        smp = slice(c * NS, (c + 1) * NS)
        m = dict(sh)
        m["xTp"] = np.ascontiguousarray(inp["x_prompt"][sq].T)
        m["xTs"] = np.ascontiguousarray(inp["x_sample"][smp, 0, :].T)
        m["pTp"] = np.ascontiguousarray(inp["p_prompt"][:, sq].transpose(0, 2, 1))
        m["pTs"] = np.ascontiguousarray(inp["p_sample"][:, smp, 0, :].transpose(0, 2, 1))
        m["stC"] = np.ascontiguousarray(inp["state_mlstm_C"][:, smp])
        m["stn"] = np.ascontiguousarray(inp["state_mlstm_n"][:, smp].reshape(L, NS, 4, 2, 128).transpose(0, 4, 1, 2, 3).reshape(L, 128, 128))
        m["stm"] = np.ascontiguousarray(inp["state_mlstm_m"][:, smp])
        m["stconv"] = np.ascontiguousarray(inp["state_mlstm_conv"][:, smp].reshape(L, NS, 3, 8, 128).transpose(0, 4, 3, 1, 2).reshape(L, 128, 8 * NS * 3))
        s5 = np.stack([inp["state_s5_re"][:, smp], inp["state_s5_im"][:, smp]], axis=1)
        m["sts5"] = np.ascontiguousarray(s5.reshape(L, 2, NS, 32, 2, 64).transpose(0, 1, 4, 5, 3, 2).reshape(L, 2, 128, 512))
        in_maps.append({k_: np.ascontiguousarray(v, dtype=f32) for k_, v in m.items()})
    res = run_bass_kernel_spmd(nc, in_maps, core_ids=list(range(len(cores))))
    R = res.results
    B, S = 4, SEQ
    y_p = np.zeros((B, S, D), f32); y_s = np.zeros((128, 1, D), f32)
    C_p = np.zeros((L, B, 4, 256, 256), f32); n_p = np.zeros((L, B, 4, 256), f32); m_p = np.zeros((L, B, 4), f32)
    conv_p = np.zeros((L, B, 3, D_A), f32); s5re_p = np.zeros((L, B, 64, 64), f32); s5im_p = np.zeros((L, B, 64, 64), f32)
    C_s = np.zeros((L, 128, 4, 256, 256), f32); n_s = np.zeros((L, 128, 4, 256), f32); m_s = np.zeros((L, 128, 4), f32)
    conv_s = np.zeros((L, 128, 3, D_A), f32); s5re_s = np.zeros((L, 128, 64, 64), f32); s5im_s = np.zeros((L, 128, 64, 64), f32)
    for ci, c in enumerate(cores):
        r = R[ci]
        smp = slice(c * NS, (c + 1) * NS)
        if c < 4:
            y_p[c] = r["o_yTp"].T
            C_p[:, c] = r["o_Cp"]
            n_p[:, c] = r["o_np"].reshape(L, 128, 2, 4).transpose(0, 3, 2, 1).reshape(L, 4, 256)
            m_p[:, c] = r["o_mp"]
            conv_p[:, c] = r["o_convp"].transpose(0, 2, 1)
            sp = r["o_s5p"].reshape(L, 2, 2, 64, 32).transpose(0, 1, 4, 2, 3).reshape(L, 2, 64, 64)
            s5re_p[:, c] = sp[:, 0]; s5im_p[:, c] = sp[:, 1]
        y_s[smp, 0, :] = r["o_yTs"].T
        C_s[:, smp] = r["o_Cs"]
        n_s[:, smp] = r["o_ns"].reshape(L, 128, NS, 4, 2).transpose(0, 2, 3, 4, 1).reshape(L, NS, 4, 256)
        m_s[:, smp] = r["o_ms"]
        conv_s[:, smp] = r["o_convs"].reshape(L, 128, 8, NS, 3).transpose(0, 3, 4, 2, 1).reshape(L, NS, 3, D_A)
        ss = r["o_s5s"].reshape(L, 2, 2, 64, 32, NS).transpose(0, 1, 5, 4, 2, 3).reshape(L, 2, NS, 64, 64)
        s5re_s[:, smp] = ss[:, 0]; s5im_s[:, smp] = ss[:, 1]
    return (y_p, y_s, C_p, n_p, m_p, conv_p, s5re_p, s5im_p, C_s, n_s, m_s, conv_s, s5re_s, s5im_s)
```

```python
import contextlib
import numpy as np
import concourse.bass as bass
import concourse.mybir as mybir
from concourse.bass_utils import run_bass_kernel_spmd

AF = mybir.ActivationFunctionType
ALU = mybir.AluOpType
AX = mybir.AxisListType
F32 = mybir.dt.float32
BF16 = mybir.dt.bfloat16

D = 2048
SEQ = 2048
NB = 4
TBP = 512
NS = 16
TB = TBP + NS
DEPTH = 2
D_A = 1024
EPS = 1e-6
NCORES = 8

ENGS = ("pe", "act", "dve", "sp", "pool")
DMA_ENGS = ("sp", "pool")
NDSEM = {"sp": 12, "pool": 16}
EPOCH = 4096


class Buf:
    def __init__(self, name, nslots=1):
        self.name = name
        self.n = nslots
        self.w = [None] * nslots
        self.r = [[] for _ in range(nslots)]


class Ins:
    __slots__ = ("eng", "idx", "fn", "deps", "dma", "signal", "dn")

    def __init__(self, eng, idx, fn, deps, dma):
        self.eng = eng
        self.idx = idx
        self.fn = fn
        self.deps = deps
        self.dma = dma
        self.signal = False
        self.dn = -1


class Prog:
    def __init__(self):
        self.q = {e: [] for e in ENGS}
        self.ndma = {e: 0 for e in DMA_ENGS}

    def op(self, eng, fn, reads=(), writes=(), dma=False):
        deps = set()
        for (b, lo, hi) in reads:
            for s in range(lo, hi):
                if b.w[s] is not None:
                    deps.add(b.w[s])
        for (b, lo, hi) in writes:
            for s in range(lo, hi):
                if b.w[s] is not None:
                    deps.add(b.w[s])
                deps.update(b.r[s])
        idx = len(self.q[eng])
        me = (eng, idx)
        deps.discard(me)
        ins = Ins(eng, idx, fn, deps, dma)
        if dma:
            ins.dn = self.ndma[eng]
            self.ndma[eng] += 1
        self.q[eng].append(ins)
        for (b, lo, hi) in reads:
            for s in range(lo, hi):
                b.r[s].append(me)
        for (b, lo, hi) in writes:
            for s in range(lo, hi):
                b.w[s] = me
                b.r[s] = []
        return ins

    def emit(self, nc):
        q = self.q
        for e in DMA_ENGS:
            K = NDSEM[e]
            lst = [i for i in q[e] if i.dma]
            for n, ins in enumerate(lst):
                if n >= K:
                    ins.deps.add((e, lst[n - K].idx))
        for e in ENGS:
            for ins in q[e]:
                for (pe, pi) in ins.deps:
                    p = q[pe][pi]
                    if not p.dma and not (pe == "pe" and e == "pe"):
                        p.signal = True
        cnt = {}
        for e in ENGS:
            c = 0
            arr = []
            for ins in q[e]:
                if ins.signal:
                    c += 1
                arr.append(c)
            cnt[e] = arr
        with contextlib.ExitStack() as st:
            csem = {e: [st.enter_context(nc.semaphore("c_%s%d" % (e, i))) for i in range(max(1, (cnt[e][-1] if cnt[e] else 0) // EPOCH + 1))]
                    for e in ENGS}
            dsem = {e: [st.enter_context(nc.semaphore("d_%s%d" % (e, i))) for i in range(NDSEM[e])]
                    for e in DMA_ENGS}
            block = st.enter_context(nc.Block())

            def run(e, eng):
                seen = {}
                for ins in q[e]:
                    need = {}
                    for (pe, pi) in ins.deps:
                        p = q[pe][pi]
                        if p.dma:
                            K = NDSEM[pe]
                            key = ("d", pe, p.dn % K)
                            val = 16 * (p.dn // K + 1)
                        else:
                            if pe == "pe" and e == "pe":
                                continue
                            c_ = cnt[pe][pi]
                            key = ("c", pe, (c_ - 1) // EPOCH)
                            val = (c_ - 1) % EPOCH + 1
                        if need.get(key, 0) < val:
                            need[key] = val
                    for key, val in need.items():
                        if seen.get(key, 0) >= val:
                            continue
                        seen[key] = val
                        sem = csem[key[1]][key[2]] if key[0] == "c" else dsem[key[1]][key[2]]
                        eng.wait_ge(sem, val)
                    if ins.fn is None:
                        continue
                    r = ins.fn(eng)
                    if ins.dma:
                        r.then_inc(dsem[e][ins.dn % NDSEM[e]], 16)
                    elif ins.signal:
                        r.then_inc(csem[e][(cnt[e][ins.idx] - 1) // EPOCH], 1)

            @block.tensor
            def _(eng):
                run("pe", eng)

            @block.scalar
            def _(eng):
                run("act", eng)

            @block.vector
            def _(eng):
                run("dve", eng)

            @block.sync
            def _(eng):
                run("sp", eng)

            @block.gpsimd
            def _(eng):
                run("pool", eng)


class TT:
    def __init__(self, t, name, nslots=1):
        self.t = t
        self.b = Buf(name, nslots)

    def s(self, lo=0, hi=None):
        return (self.b, lo, self.b.n if hi is None else hi)


class KB:
    def __init__(self, nc, st):
        self.nc = nc
        self.st = st
        self.P = Prog()
        self.dram = {}
        self._bank = 0
        self._ws = 0
        self.pinned = set()

    def din(self, name, shape):
        t = self.nc.dram_tensor(name, list(shape), F32, kind="ExternalInput")
        self.dram[name] = TT(t.ap(), name, 1)
        return self.dram[name]

    def dout(self, name, shape):
        t = self.nc.dram_tensor(name, list(shape), F32, kind="ExternalOutput")
        self.dram[name] = TT(t.ap(), name, 1)
        return self.dram[name]

    def sb(self, name, shape, dt, nslots=1):
        t = self.st.enter_context(self.nc.sbuf_tensor(name, list(shape), dt))
        return TT(t, name, nslots)

    def psum(self, name, shape, dt):
        t = self.st.enter_context(self.nc.psum_tensor(name, list(shape), dt))
        return TT(t, name, 1)

    def V(self, fn, r=(), w=()):
        return self.P.op("dve", fn, r, w)

    def A(self, fn, r=(), w=()):
        return self.P.op("act", fn, r, w)

    def M(self, fn, r=(), w=()):
        return self.P.op("pe", fn, r, w)

    def Dm(self, fn, r=(), w=()):
        return self.P.op("sp", fn, r, w, dma=True)

    def Wm(self, fn, r=(), w=()):
        return self.P.op("pool", fn, r, w, dma=True)

    def unpin(self, ws):
        self.pinned.discard(ws)

    def bank(self):
        b = self.banks[self._bank % len(self.banks)]
        self._bank += 1
        return b

    def wload(self, src_ap, pin=False):
        while True:
            ws = self.wslots[self._ws % len(self.wslots)]
            self._ws += 1
            if ws not in self.pinned:
                break
        if pin:
            self.pinned.add(ws)
        self.Wm(lambda e, o=ws.t, i=src_ap: e.dma_start(out=o[:, :], in_=i), w=[ws.s()])
        return ws


TWO_PI = 6.283185307179586
NLP = 72
LP_CW, LP_CB, LP_SKIP, LP_DSK, LP_BIF, LP_BRT = 0, 32, 40, 48, 56, 64
C_ID, C_TRI, C_JJ, C_M2, C_D16 = 0, 128, 256, 768, 776
NCONST = 1032
DFF_T = 43
PROMPT_CORES = {0: 0, 1: 1, 4: 2, 5: 3}
RUN_CORES = None
STOP_AFTER = None


class VT:
    def __init__(self, t, buf, lo, hi):
        self.t = t
        self.b = buf
        self.lo = lo
        self.hi = hi

    def s(self, *a):
        return (self.b, self.lo, self.hi)


def build_program():
    nc = bass.Bass("TRN2", target_bir_lowering=False)
    with contextlib.ExitStack() as st:
        k = KB(nc, st)
        din = k.din

        def dout(name, shape, nslots=1):
            t = nc.dram_tensor(name, list(shape), F32, kind="ExternalOutput")
            tt = TT(t.ap(), name, nslots)
            k.dram[name] = tt
            return tt

        xTp = din("xTp", [D, SEQ]); xTs = din("xTs", [D, NS])
        pTp = din("pTp", [DEPTH, 256, SEQ]); pTs = din("pTs", [DEPTH, 256, NS])
        gains = din("gains", [128, 7 * 16])
        consts = din("consts", [128, NCONST])
        lp = din("lp", [DEPTH, 128, NLP])
        ghd = din("ghd", [DEPTH, 128, 1024])
        w_in_t = din("w_in_t", [DEPTH, 56, 128, 2048])
        w_if = din("w_if", [DEPTH, 128, 128])
        wqkv = din("wqkv", [DEPTH, 3, 128, 2048])
        w_proj_t = din("w_proj_t", [DEPTH, 8, 128, 2048])
        w_glu_t = din("w_glu_t", [DEPTH, 16, 128, 2048])
        w_out_t = din("w_out_t", [DEPTH, 16, 128, 2048])
        w_pg_t = din("w_pg_t", [DEPTH, 16, 128, 2048])
        w_ple_t = din("w_ple_t", [DEPTH, 2, 128, 2048])
        ffg = din("ffg", [DFF_T, 128, 2048]); ffu = din("ffu", [DFF_T, 128, 2048]); ffd = din("ffd", [DFF_T, 128, 2048])
        moeg = din("moeg", [8, DFF_T, 128, 2048]); moeu = din("moeu", [8, DFF_T, 128, 2048]); moed = din("moed", [8, DFF_T, 128, 2048])
        w_rt = din("w_rt", [128, 128])
        s5L1 = din("s5L1", [DEPTH, 128, 96]); s5L2 = din("s5L2", [DEPTH, 128, 1536])
        s5B2 = din("s5B2", [DEPTH, 128, 1024]); s5C1 = din("s5C1", [DEPTH, 128, 1024])
        stC = din("stC", [DEPTH, NS, 4, 256, 256]); stn = din("stn", [DEPTH, 128, 128]); stm = din("stm", [DEPTH, NS, 4])
        stconv = din("stconv", [DEPTH, 128, 8 * NS * 3]); sts5 = din("sts5", [DEPTH, 2, 128, 512])

        o_yTp = dout("o_yTp", [D, SEQ], 64); o_yTs = dout("o_yTs", [D, NS], 16)
        o_Cp = dout("o_Cp", [DEPTH, 4, 256, 256], 8); o_np = dout("o_np", [DEPTH, 128, 8], 2); o_mp = dout("o_mp", [DEPTH, 4], 2)
        o_convp = dout("o_convp", [DEPTH, D_A, 3], 2); o_s5p = dout("o_s5p", [DEPTH, 2, 128, 32], 4)
        o_Cs = dout("o_Cs", [DEPTH, NS, 4, 256, 256], 128); o_ns = dout("o_ns", [DEPTH, 128, 128], 2); o_ms = dout("o_ms", [DEPTH, NS, 4], 2)
        o_convs = dout("o_convs", [DEPTH, 128, 8 * NS * 3], 2); o_s5s = dout("o_s5s", [DEPTH, 2, 128, 512], 4)
        all_outs = [o_yTp, o_yTs, o_Cp, o_np, o_mp, o_convp, o_s5p, o_Cs, o_ns, o_ms, o_convs, o_s5s]

        h = k.sb("h", [128, 16, TB], F32, 16)
        xn = k.sb("xn", [128, 16, TB], BF16, 16)
        uab = k.sb("uab", [128, 8, 3 + TB], BF16, 8)
        ub = k.sb("ub", [128, 8, TB], BF16, 8)
        apre = k.sb("apre", [128, 8, TB], BF16, 8)
        CS = k.sb("CS", [128, 16, TB], BF16, 16)
        G1 = k.sb("G1", [128, 16, TB], BF16, 16)

        def fviews(tt, ntiles):
            f = tt.t.bitcast(F32).reshape([128, ntiles, TB])
            return [VT(f[:, q, :], tt.b, 2 * q, 2 * q + 2) for q in range(ntiles)]
        CSf = fviews(CS, 8)
        G1f = fviews(G1, 8)
        apf = fviews(apre, 4)
        pool20 = CSf + G1f + apf
        k.wslots = [k.sb("ws%d" % i, [128, 2048], BF16) for i in range(6)]
        Cst = [k.sb("Cst%d" % l, [128, 2, 4, 257], F32, 8) for l in range(DEPTH)]
        mst = k.sb("mst", [4, DEPTH], F32, DEPTH)
        x0r = k.sb("x0r", [128, DEPTH, 32], F32, DEPTH); x0i = k.sb("x0i", [128, DEPTH, 32], F32, DEPTH)
        hist = k.sb("hist", [128, DEPTH, 8, 3], BF16, DEPTH)
        SC = [k.sb("SC%d" % i, [128, TB], F32) for i in range(3)]
        SB = [k.sb("SB%d" % i, [128, TB], BF16) for i in range(4)]
        gn = k.sb("gn", [128, 7 * 16], F32)
        cst = k.sb("cst", [128, NCONST], F32)
        ident_bf = k.sb("ident_bf", [128, 128], BF16)
        ones_bf = k.sb("ones_bf", [128, 128], BF16)
        ones_f = k.sb("ones_f", [128, 128], F32)
        epsc = k.sb("epsc", [128, 1], F32)
        onec = k.sb("onec", [128, 1], F32)
        hpic = k.sb("hpic", [128, 1], F32)
        lpt = k.sb("lpt", [128, NLP], F32)
        gh = k.sb("gh", [128, 1024], F32)
        wif = k.sb("wif", [128, 128], BF16)
        wrt = k.sb("wrt", [128, 128], BF16)
        qk = [k.sb("qk%d" % i, [128, 4, TB], BF16, 4) for i in range(1)]
        gts = k.sb("gts", [128, 4, 12], F32, 4)
        sm = {}

        def small(name, shape, dt=F32):
            if name not in sm:
                sm[name] = k.sb("sm_" + name, shape, dt)
            return sm[name]
        ktok = [k.sb("ktok%d" % i, [128, 256], BF16) for i in range(2)]
        vext = [k.sb("vext%d" % i, [128, 257], BF16) for i in range(2)]
        spT = [k.sb("spT%d" % i, [128, 128], BF16) for i in range(2)]
        cbf = [k.sb("cbf%d" % i, [128, 2, 257], BF16) for i in range(2)]
        hno = [k.sb("hno%d" % i, [128, 256], BF16) for i in range(2)]
        tmpg = [k.sb("tmpg%d" % i, [128, 128], F32) for i in range(2)]
        sqv = k.sb("sqv", [128, 256], F32)
        c0t = k.sb("c0t", [128, 8, NS, 3], F32)
        cso = k.sb("cso", [128, 8, NS, 3], F32)
        cpo = k.sb("cpo", [128, 8, 3], F32)
        nall = k.sb("nall", [128, 128], F32)
        nnew = k.sb("nnew", [128, 128], F32)
        npo = k.sb("npo", [128, 8], F32)
        qmask = k.sb("qmask", [128, 2, NS, NS], BF16)
        kmask = [k.sb("kmask%d" % i, [NS, 256], BF16) for i in range(2)]
        c0bc = k.sb("c0bc", [128, 64], F32)
        ZB = [k.sb("ZB%d" % i, [128, 4, 2, 128], BF16) for i in range(1)]
        ZC = [k.sb("ZC%d" % i, [128, 4, 2, 128], BF16) for i in range(1)]
        s1 = k.sb("s1", [128, 12, 32], F32)
        kint = VT(SC[1].t.bitcast(mybir.dt.int32)[:, 0:TBP], SC[1].b, 0, 1)
        print("SBUF bytes remaining:", nc.sbuf_bytes_remaining)

        rot4 = [k.psum("ps%d" % i, [128, 512], F32) for i in range(4)]
        k.banks = rot4
        psA = k.psum("psA", [128, 512], F32)
        psB = k.psum("psB", [128, 512], F32)
        psS = k.psum("psS", [128, 512], F32)
        rot7 = rot4 + [psA, psB, psS]
        psT = k.psum("psT", [128, 1024], BF16)

        ident_f = cst.t[:, C_ID:C_ID + 128]
        tri_f = cst.t[:, C_TRI:C_TRI + 128]
        jj = cst.t[:, C_JJ:C_JJ + 512]
        d16 = cst.t[:, C_D16:C_D16 + 256].rearrange("p (a b) -> p a b", a=16)

        k.Dm(lambda e: e.dma_start(out=gn.t[:, :], in_=gains.t), w=[gn.s()])
        k.Dm(lambda e: e.dma_start(out=cst.t[:, :], in_=consts.t), w=[cst.s()])
        k.V(lambda e: e.memset(ones_bf.t[:, :], 1.0), w=[ones_bf.s()])
        k.V(lambda e: e.memset(ones_f.t[:, :], 1.0), w=[ones_f.s()])
        k.V(lambda e: e.memset(epsc.t[:, :], EPS), w=[epsc.s()])
        k.V(lambda e: e.memset(onec.t[:, :], 1.0), w=[onec.s()])
        k.V(lambda e: e.memset(hpic.t[:, :], TWO_PI / 4), w=[hpic.s()])
        k.V(lambda e: e.tensor_copy(out=ident_bf.t[:, :], in_=ident_f), r=[cst.s()], w=[ident_bf.s()])
        for l in range(DEPTH):
            k.V(lambda e, l=l: e.memset(Cst[l].t[:, :, :, :], 0.0), w=[Cst[l].s()])
        k.V(lambda e: e.memset(mst.t[:, :], 0.0), w=[mst.s()])
        k.V(lambda e: e.memset(x0r.t[:, :, :], 0.0), w=[x0r.s()])
        k.V(lambda e: e.memset(x0i.t[:, :, :], 0.0), w=[x0i.s()])
        k.V(lambda e: e.memset(hist.t[:, :, :, :], 0.0), w=[hist.s()])
        for i in range(2):
            k.V(lambda e, i=i: e.memset(vext[i].t[:, 256:257], 1.0), w=[vext[i].s()])
        vsb = small("Vsb", [NS, 257], BF16)
        k.V(lambda e: e.memset(vsb.t[:, 256:257], 1.0), w=[vsb.s()])
        k.Wm(lambda e: e.dma_start(out=wrt.t[:, :], in_=w_rt.t), w=[wrt.s()])

        def chunks(blk):
            return [(0, TBP)] + ([(TBP, NS)] if blk == NB - 1 else [])

        def lpc(c):
            return lpt.t[:, c:c + 1]

        def rms(blk, gidx, dst, out32=None):
            for (c0, n) in chunks(blk):
                bk = k.bank()
                rs = SC[0]
                for kt in range(16):
                    s_ = SB[kt % 2]
                    k.A(lambda e, s_=s_, kt=kt, c0=c0, n=n: e.activation(out=s_.t[:, :n], in_=h.t[:, kt, c0:c0 + n], func=AF.Square),
                        r=[h.s(kt, kt + 1)], w=[s_.s()])
                    k.M(lambda e, s_=s_, kt=kt, n=n, bk=bk: e.matmul(bk.t[:, :n], lhsT=ones_bf.t[:, :], rhs=s_.t[:, :n], start=(kt == 0), stop=(kt == 15)),
                        r=[s_.s(), ones_bf.s()], w=[bk.s()])
                k.A(lambda e, n=n, bk=bk, rs=rs: e.activation(out=rs.t[:, :n], in_=bk.t[:, :n], func=AF.Sqrt, bias=epsc.t[:, 0:1], scale=1.0 / D),
                    r=[bk.s(), epsc.s()], w=[rs.s()])
                k.V(lambda e, n=n, rs=rs: e.reciprocal(out=rs.t[:, :n], in_=rs.t[:, :n]), r=[rs.s()], w=[rs.s()])
                for kt in range(16):
                    if out32 is None:
                        k.V(lambda e, kt=kt, c0=c0, n=n, rs=rs: e.scalar_tensor_tensor(
                            out=dst.t[:, kt, c0:c0 + n], in0=h.t[:, kt, c0:c0 + n], scalar=gn.t[:, gidx * 16 + kt:gidx * 16 + kt + 1],
                            in1=rs.t[:, :n], op0=ALU.mult, op1=ALU.mult),
                            r=[h.s(kt, kt + 1), rs.s(), gn.s()], w=[dst.s(kt, kt + 1)])
                    else:
                        out32(kt, c0, n, rs)

        def fm_proj(blk, waps, KT, src, epi, mt_per_slot=1):
            for j, wap in enumerate(waps):
                ws = k.wload(wap)
                for mm in range(mt_per_slot):
                    mt = j * mt_per_slot + mm
                    for (c0, n) in chunks(blk):
                        bk = k.bank()
                        for kt in range(KT):
                            off = (mm * KT + kt) * 128
                            k.M(lambda e, ws=ws, off=off, kt=kt, c0=c0, n=n, bk=bk: e.matmul(
                                bk.t[:, :n], lhsT=ws.t[:, off:off + 128], rhs=src.t[:, kt, c0:c0 + n],
                                start=(kt == 0), stop=(kt == KT - 1)),
                                r=[ws.s(), src.s(kt, kt + 1)], w=[bk.s()])
                        epi(mt, c0, n, bk)

        def epi_act(dst, func, base=0, col0=0):
            def f(mt, c0, n, bk):
                k.A(lambda e, mt=mt, c0=c0, n=n, bk=bk: e.activation(out=dst.t[:, base + mt, col0 + c0:col0 + c0 + n], in_=bk.t[:, :n], func=func),
                    r=[bk.s()], w=[dst.s(base + mt, base + mt + 1)])
            return f

        def disc(get, rw, itmp):
            A_ = lambda fn: k.A(fn, r=rw + [hpic.s()], w=rw)
            V_ = lambda fn: k.V(fn, r=rw + [kint.s()], w=rw + [kint.s()])
            g = get
            A_(lambda e: e.activation(out=g(9), in_=g(2), func=AF.Exp))
            V_(lambda e: e.tensor_tensor(out=g(10), in0=g(0), in1=g(9), op=ALU.mult))
            A_(lambda e: e.activation(out=g(3), in_=g(10), func=AF.Exp))
            V_(lambda e: e.tensor_tensor(out=g(10), in0=g(1), in1=g(9), op=ALU.mult))
            V_(lambda e: e.tensor_scalar(out=g(10), in0=g(10), scalar1=1.0 / TWO_PI, scalar2=None, op0=ALU.mult))
            V_(lambda e: e.tensor_copy(out=itmp, in_=g(10)))
            V_(lambda e: e.tensor_tensor(out=g(4), in0=g(10), in1=itmp, op=ALU.subtract))
            A_(lambda e: e.activation(out=g(6), in_=g(4), func=AF.Sin, scale=TWO_PI))
            A_(lambda e: e.activation(out=g(10), in_=g(4), func=AF.Abs))
            A_(lambda e: e.activation(out=g(5), in_=g(10), func=AF.Sin, scale=-TWO_PI, bias=hpic.t[:, 0:1]))
            V_(lambda e: e.tensor_tensor(out=g(5), in0=g(5), in1=g(3), op=ALU.mult))
            V_(lambda e: e.tensor_tensor(out=g(6), in0=g(6), in1=g(3), op=ALU.mult))
            V_(lambda e: e.tensor_tensor(out=g(9), in0=g(0), in1=g(0), op=ALU.mult))
            V_(lambda e: e.tensor_tensor(out=g(10), in0=g(1), in1=g(1), op=ALU.mult))
            V_(lambda e: e.tensor_tensor(out=g(9), in0=g(9), in1=g(10), op=ALU.add))
            V_(lambda e: e.reciprocal(out=g(9), in_=g(9)))
            V_(lambda e: e.tensor_scalar(out=g(10), in0=g(5), scalar1=-1.0, scalar2=None, op0=ALU.add))
            V_(lambda e: e.tensor_tensor(out=g(7), in0=g(10), in1=g(0), op=ALU.mult))
            V_(lambda e: e.tensor_tensor(out=g(11), in0=g(6), in1=g(1), op=ALU.mult))
            V_(lambda e: e.tensor_tensor(out=g(7), in0=g(7), in1=g(11), op=ALU.add))
            V_(lambda e: e.tensor_tensor(out=g(7), in0=g(7), in1=g(9), op=ALU.mult))
            V_(lambda e: e.tensor_tensor(out=g(8), in0=g(6), in1=g(0), op=ALU.mult))
            V_(lambda e: e.tensor_tensor(out=g(11), in0=g(10), in1=g(1), op=ALU.mult))
            V_(lambda e: e.tensor_tensor(out=g(8), in0=g(8), in1=g(11), op=ALU.subtract))
            V_(lambda e: e.tensor_tensor(out=g(8), in0=g(8), in1=g(9), op=ALU.mult))

        def s5_stage(blk, l):
            last = blk == NB - 1
            k.Dm(lambda e: e.dma_start(out=s1.t[:, 0:3, :].rearrange("p a b -> p (a b)"), in_=s5L1.t[l]), w=[s1.s()])
            disc(lambda q: s1.t[:, q, :], [s1.s()], kint.t[:, 0:32])
            L2 = pool20[0:12]
            rw = [t.s() for t in L2]

            def g2(q):
                return L2[q].t[:, 0:512]
            for q in range(3):
                k.Dm(lambda e, q=q: e.dma_start(out=g2(q), in_=s5L2.t[l][:, q * 512:(q + 1) * 512]), r=rw, w=rw)
            disc(g2, rw, kint.t[:, 0:512])
            V_ = lambda fn: k.V(fn, r=rw, w=rw)
            k.Dm(lambda e: e.dma_start(out=g2(0), in_=s5B2.t[l][:, 0:512]), r=rw, w=rw)
            k.Dm(lambda e: e.dma_start(out=g2(1), in_=s5B2.t[l][:, 512:1024]), r=rw, w=rw)
            V_(lambda e: e.tensor_tensor(out=g2(2), in0=g2(7), in1=g2(0), op=ALU.mult))
            V_(lambda e: e.tensor_tensor(out=g2(10), in0=g2(8), in1=g2(1), op=ALU.mult))
            V_(lambda e: e.tensor_tensor(out=g2(2), in0=g2(2), in1=g2(10), op=ALU.subtract))
            V_(lambda e: e.tensor_tensor(out=g2(3), in0=g2(7), in1=g2(1), op=ALU.mult))
            V_(lambda e: e.tensor_tensor(out=g2(10), in0=g2(8), in1=g2(0), op=ALU.mult))
            V_(lambda e: e.tensor_tensor(out=g2(3), in0=g2(3), in1=g2(10), op=ALU.add))
            k.Dm(lambda e: e.dma_start(out=g2(4), in_=s5C1.t[l][:, 0:512]), r=rw, w=rw)
            k.Dm(lambda e: e.dma_start(out=g2(5), in_=s5C1.t[l][:, 512:1024]), r=rw, w=rw)
            V_(lambda e: e.tensor_scalar(out=g2(5), in0=g2(5), scalar1=-1.0, scalar2=None, op0=ALU.mult))
            bbv = [g2(2).rearrange("p (i q) -> p i q", q=64), g2(3).rearrange("p (i q) -> p i q", q=64)]
            ccv = [g2(4).rearrange("p (j c) -> p j c", c=16), g2(5).rearrange("p (j c) -> p j c", c=16)]
            live = [L2[q].s() for q in (2, 3, 4, 5)]
            cs_t, sn_t, y1_t, fr_t, t1_t, t2_t = pool20[0], pool20[1], pool20[6], pool20[7], pool20[8], pool20[9]
            br_t, bi_t, wr_t, wi_t, rt_t = pool20[10], pool20[11], pool20[12], pool20[13], pool20[14]
            N = TBP
            xs5 = [VT(pool20[15 + ri].t[:, 0:512].rearrange("p (a b) -> p a b", b=NS), pool20[15 + ri].b, pool20[15 + ri].lo, pool20[15 + ri].hi) for ri in range(2)]
            xs5b = [VT(apre.t[:, 2 + ri, 0:512].rearrange("p (a b) -> p a b", b=NS), apre.b, 2 + ri, 3 + ri) for ri in range(2)]
            if last:
                for ri in range(2):
                    k.Dm(lambda e, ri=ri: e.dma_start(out=xs5[ri].t.rearrange("p a b -> p (a b)"), in_=sts5.t[l, ri]), w=[xs5[ri].s()])

            def mul(o, a, b, extra_r=()):
                k.V(lambda e: e.tensor_tensor(out=o.t[:, 0:N], in0=a.t[:, 0:N], in1=b.t[:, 0:N], op=ALU.mult), r=[a.s(), b.s()] + list(extra_r), w=[o.s()])

            def addsub(o, a, b, op):
                k.V(lambda e: e.tensor_tensor(out=o.t[:, 0:N], in0=a.t[:, 0:N], in1=b.t[:, 0:N], op=op), r=[a.s(), b.s()], w=[o.s()])

            for i in range(8):
                zb = ZB[0]; zc = ZC[0]
                k.V(lambda e, zc=zc: e.memset(zc.t[:, :, :, :], 0.0), w=[zc.s()])
                for jm in range(4):
                    for ri in range(2):
                        for gg in range(2):
                            k.V(lambda e, zb=zb, jm=jm, ri=ri, gg=gg, i=i: e.tensor_scalar(
                                out=zb.t[:, jm, ri, gg * 64:(gg + 1) * 64], in0=bbv[ri][:, i, :],
                                scalar1=cst.t[:, C_M2 + 2 * jm + gg:C_M2 + 2 * jm + gg + 1], scalar2=None, op0=ALU.mult),
                                r=live + [cst.s()], w=[zb.s()])
                            col = (2 * jm + gg) * 16
                            k.V(lambda e, zc=zc, jm=jm, ri=ri, gg=gg, i=i, col=col: e.tensor_copy(
                                out=zc.t[gg * 64:(gg + 1) * 64, jm, ri, col:col + 16], in_=ccv[ri][gg * 64:(gg + 1) * 64, 4 * i + jm, :]),
                                r=live, w=[zc.s()])
                yps = psA if i % 2 == 0 else psB
                for jm in range(4):
                    j = 4 * i + jm
                    pre = k.bank(); pim = k.bank()
                    k.M(lambda e, zb=zb, jm=jm, i=i, pre=pre: e.matmul(pre.t[:, 0:N], lhsT=zb.t[:, jm, 0, :], rhs=ub.t[:, i, 0:N], start=True, stop=True),
                        r=[zb.s(), ub.s(i, i + 1)], w=[pre.s()])
                    k.M(lambda e, zb=zb, jm=jm, i=i, pim=pim: e.matmul(pim.t[:, 0:N], lhsT=zb.t[:, jm, 1, :], rhs=ub.t[:, i, 0:N], start=True, stop=True),
                        r=[zb.s(), ub.s(i, i + 1)], w=[pim.s()])
                    k.A(lambda e, j=j: e.activation(out=y1_t.t[:, 0:N], in_=jj, func=AF.Copy, scale=s1.t[:, 4, j:j + 1]),
                        r=[cst.s(), s1.s()], w=[y1_t.s()])
                    k.V(lambda e: e.tensor_copy(out=kint.t[:, 0:N], in_=y1_t.t[:, 0:N]), r=[y1_t.s()], w=[kint.s()])
                    k.V(lambda e: e.tensor_tensor(out=fr_t.t[:, 0:N], in0=y1_t.t[:, 0:N], in1=kint.t[:, 0:N], op=ALU.subtract),
                        r=[y1_t.s(), kint.s()], w=[fr_t.s()])
                    k.A(lambda e: e.activation(out=sn_t.t[:, 0:N], in_=fr_t.t[:, 0:N], func=AF.Sin, scale=TWO_PI), r=[fr_t.s()], w=[sn_t.s()])
                    k.A(lambda e: e.activation(out=y1_t.t[:, 0:N], in_=fr_t.t[:, 0:N], func=AF.Abs), r=[fr_t.s()], w=[y1_t.s()])
                    k.A(lambda e: e.activation(out=cs_t.t[:, 0:N], in_=y1_t.t[:, 0:N], func=AF.Sin, scale=-TWO_PI, bias=hpic.t[:, 0:1]),
                        r=[y1_t.s(), hpic.s()], w=[cs_t.s()])
                    k.A(lambda e, j=j: e.activation(out=rt_t.t[:, 0:N], in_=ones_f.t[:, 0:1].to_broadcast([128, N]), func=AF.Copy, scale=s1.t[:, 3, j:j + 1]),
                        r=[ones_f.s(), s1.s()], w=[rt_t.s()])
                    k.V(lambda e, pre=pre: e.tensor_tensor(out=t1_t.t[:, 0:N], in0=pre.t[:, 0:N], in1=cs_t.t[:, 0:N], op=ALU.mult), r=[pre.s(), cs_t.s()], w=[t1_t.s()])
                    k.V(lambda e, pim=pim: e.tensor_tensor(out=t2_t.t[:, 0:N], in0=pim.t[:, 0:N], in1=sn_t.t[:, 0:N], op=ALU.mult), r=[pim.s(), sn_t.s()], w=[t2_t.s()])
                    addsub(br_t, t1_t, t2_t, ALU.add)
                    k.V(lambda e, pim=pim: e.tensor_tensor(out=t1_t.t[:, 0:N], in0=pim.t[:, 0:N], in1=cs_t.t[:, 0:N], op=ALU.mult), r=[pim.s(), cs_t.s()], w=[t1_t.s()])
                    k.V(lambda e, pre=pre: e.tensor_tensor(out=t2_t.t[:, 0:N], in0=pre.t[:, 0:N], in1=sn_t.t[:, 0:N], op=ALU.mult), r=[pre.s(), sn_t.s()], w=[t2_t.s()])
                    addsub(bi_t, t1_t, t2_t, ALU.subtract)
                    k.V(lambda e, j=j: e.tensor_tensor_scan(out=wr_t.t[:, 0:N], data0=rt_t.t[:, 0:N], data1=br_t.t[:, 0:N], initial=x0r.t[:, l, j:j + 1], op0=ALU.mult, op1=ALU.add),
                        r=[rt_t.s(), br_t.s(), x0r.s(l, l + 1)], w=[wr_t.s()])
                    k.V(lambda e, j=j: e.tensor_tensor_scan(out=wi_t.t[:, 0:N], data0=rt_t.t[:, 0:N], data1=bi_t.t[:, 0:N], initial=x0i.t[:, l, j:j + 1], op0=ALU.mult, op1=ALU.add),
                        r=[rt_t.s(), bi_t.s(), x0i.s(l, l + 1)], w=[wi_t.s()])
                    mul(t1_t, wr_t, cs_t); mul(t2_t, wi_t, sn_t); addsub(br_t, t1_t, t2_t, ALU.subtract)
                    mul(t1_t, wr_t, sn_t); mul(t2_t, wi_t, cs_t); addsub(bi_t, t1_t, t2_t, ALU.add)
                    k.V(lambda e, j=j: e.tensor_copy(out=x0r.t[:, l, j:j + 1], in_=br_t.t[:, N - 1:N]), r=[br_t.s()], w=[x0r.s(l, l + 1)])
                    k.V(lambda e, j=j: e.tensor_copy(out=x0i.t[:, l, j:j + 1], in_=bi_t.t[:, N - 1:N]), r=[bi_t.s()], w=[x0i.s(l, l + 1)])
                    xrb = SB[2]; xib = SB[3]
                    k.A(lambda e: e.activation(out=xrb.t[:, 0:N], in_=br_t.t[:, 0:N], func=AF.Copy), r=[br_t.s()], w=[xrb.s()])
                    k.A(lambda e: e.activation(out=xib.t[:, 0:N], in_=bi_t.t[:, 0:N], func=AF.Copy), r=[bi_t.s()], w=[xib.s()])
                    k.M(lambda e, zc=zc, jm=jm, yps=yps: e.matmul(yps.t[:, 0:N], lhsT=zc.t[:, jm, 0, :], rhs=xrb.t[:, 0:N], start=(jm == 0), stop=False),
                        r=[zc.s(), xrb.s()], w=[yps.s()])
                    k.M(lambda e, zc=zc, jm=jm, yps=yps: e.matmul(yps.t[:, 0:N], lhsT=zc.t[:, jm, 1, :], rhs=xib.t[:, 0:N], start=False, stop=(jm == 3)),
                        r=[zc.s(), xib.s()], w=[yps.s()])
                    if last:
                        pb2 = k.bank()
                        k.M(lambda e, zb=zb, jm=jm, i=i, pb2=pb2: e.matmul(pb2.t[:, 0:NS], lhsT=zb.t[:, jm, 0, :], rhs=ub.t[:, i, TBP:TB], start=True, stop=True),
                            r=[zb.s(), ub.s(i, i + 1)], w=[pb2.s()])
                        k.M(lambda e, zb=zb, jm=jm, i=i, pb2=pb2: e.matmul(pb2.t[:, 32:32 + NS], lhsT=zb.t[:, jm, 1, :], rhs=ub.t[:, i, TBP:TB], start=True, stop=True),
                            r=[zb.s(), ub.s(i, i + 1)], w=[pb2.s()])
                        sa = small("s5a", [128, NS]); sbb = small("s5b", [128, NS]); sc_ = small("s5c", [128, NS]); sd_ = small("s5d", [128, NS])
                        k.V(lambda e, j=j, pb2=pb2: e.scalar_tensor_tensor(out=sa.t[:, :], in0=xs5[0].t[:, j, :], scalar=s1.t[:, 5, j:j + 1], in1=pb2.t[:, 0:NS], op0=ALU.mult, op1=ALU.add),
                            r=[xs5[0].s(), s1.s(), pb2.s()], w=[sa.s()])
                        k.V(lambda e, j=j, pb2=pb2: e.scalar_tensor_tensor(out=sbb.t[:, :], in0=xs5[0].t[:, j, :], scalar=s1.t[:, 6, j:j + 1], in1=pb2.t[:, 32:32 + NS], op0=ALU.mult, op1=ALU.add),
                            r=[xs5[0].s(), s1.s(), pb2.s()], w=[sbb.s()])
                        k.V(lambda e, j=j: e.tensor_scalar(out=sc_.t[:, :], in0=xs5[1].t[:, j, :], scalar1=s1.t[:, 6, j:j + 1], scalar2=None, op0=ALU.mult),
                            r=[xs5[1].s(), s1.s()], w=[sc_.s()])
                        k.V(lambda e, j=j: e.scalar_tensor_tensor(out=sd_.t[:, :], in0=xs5[1].t[:, j, :], scalar=s1.t[:, 5, j:j + 1], in1=sbb.t[:, :], op0=ALU.mult, op1=ALU.add),
                            r=[xs5[1].s(), s1.s(), sbb.s()], w=[sd_.s()])
                        k.V(lambda e, j=j: e.tensor_tensor(out=xs5[0].t[:, j, :], in0=sa.t[:, :], in1=sc_.t[:, :], op=ALU.subtract),
                            r=[sa.s(), sc_.s(), xs5[1].s()], w=[xs5[0].s()])
                        k.V(lambda e, j=j: e.tensor_copy(out=xs5[1].t[:, j, :], in_=sd_.t[:, :]), r=[sd_.s()], w=[xs5[1].s()])
                        for ri in range(2):
                            k.A(lambda e, j=j, ri=ri: e.activation(out=xs5b[ri].t[:, j, :], in_=xs5[ri].t[:, j, :], func=AF.Copy), r=[xs5[ri].s()], w=[xs5b[ri].s()])
                        k.M(lambda e, zc=zc, jm=jm, j=j, i=i: e.matmul(psS.t[:, i * 16:(i + 1) * 16], lhsT=zc.t[:, jm, 0, :], rhs=xs5b[0].t[:, j, :], start=(jm == 0), stop=False),
                            r=[zc.s(), xs5b[0].s()], w=[psS.s()])
                        k.M(lambda e, zc=zc, jm=jm, j=j, i=i: e.matmul(psS.t[:, i * 16:(i + 1) * 16], lhsT=zc.t[:, jm, 1, :], rhs=xs5b[1].t[:, j, :], start=False, stop=(jm == 3)),
                            r=[zc.s(), xs5b[1].s()], w=[psS.s()])

                def gelu_epi(src_ps, src_tt, c0, n, i=i):
                    ya, yb2 = t1_t, t2_t
                    k.V(lambda e: e.scalar_tensor_tensor(out=ya.t[:, :n], in0=ub.t[:, i, c0:c0 + n], scalar=lpc(LP_DSK + i), in1=src_ps, op0=ALU.mult, op1=ALU.add),
                        r=[ub.s(i, i + 1), lpt.s(), src_tt.s()], w=[ya.s()])
                    k.V(lambda e: e.tensor_tensor(out=yb2.t[:, :n], in0=ya.t[:, :n], in1=ya.t[:, :n], op=ALU.mult), r=[ya.s()], w=[yb2.s()])
                    k.V(lambda e: e.tensor_scalar(out=yb2.t[:, :n], in0=yb2.t[:, :n], scalar1=0.044715, scalar2=1.0, op0=ALU.mult, op1=ALU.add), r=[yb2.s()], w=[yb2.s()])
                    k.V(lambda e: e.tensor_tensor(out=yb2.t[:, :n], in0=yb2.t[:, :n], in1=ya.t[:, :n], op=ALU.mult), r=[yb2.s(), ya.s()], w=[yb2.s()])
                    k.A(lambda e: e.activation(out=yb2.t[:, :n], in_=yb2.t[:, :n], func=AF.Sigmoid, scale=1.5957691216057308), r=[yb2.s()], w=[yb2.s()])
                    k.V(lambda e: e.tensor_tensor(out=ub.t[:, i, c0:c0 + n], in0=ya.t[:, :n], in1=yb2.t[:, :n], op=ALU.mult), r=[ya.s(), yb2.s()], w=[ub.s(i, i + 1)])
                if last:
                    gelu_epi(psS.t[:, i * 16:(i + 1) * 16], psS, TBP, NS)
                gelu_epi(yps.t[:, 0:N], yps, 0, N)
            if last:
                k.Dm(lambda e: e.dma_start(out=o_s5p.t[l, 0], in_=x0r.t[:, l, :]), r=[x0r.s(l, l + 1)], w=[o_s5p.s(2 * l, 2 * l + 1)])
                k.Dm(lambda e: e.dma_start(out=o_s5p.t[l, 1], in_=x0i.t[:, l, :]), r=[x0i.s(l, l + 1)], w=[o_s5p.s(2 * l + 1, 2 * l + 2)])
                for ri in range(2):
                    k.Dm(lambda e, ri=ri: e.dma_start(out=o_s5s.t[l, ri], in_=xs5[ri].t.rearrange("p a b -> p (a b)")), r=[xs5[ri].s()], w=[o_s5s.s(2 * l + ri, 2 * l + ri + 1)])

        def conv_stage(blk, l):
            last = blk == NB - 1
            if last:
                k.Dm(lambda e: e.dma_start(out=c0t.t[:, :, :, :].rearrange("p a b c -> p (a b c)"), in_=stconv.t[l]), w=[c0t.s()])
            for kt in range(8):
                acc = SC[1 + kt % 2]
                cw = lambda j, kt=kt: lpc(LP_CW + kt * 4 + j)
                k.V(lambda e, kt=kt, acc=acc, cw=cw: e.tensor_scalar(out=acc.t[:, 0:TBP], in0=uab.t[:, kt, 0:TBP], scalar1=cw(0), scalar2=lpc(LP_CB + kt), op0=ALU.mult, op1=ALU.add),
                    r=[uab.s(kt, kt + 1), lpt.s()], w=[acc.s()])
                for j in range(1, 4):
                    k.V(lambda e, kt=kt, acc=acc, cw=cw, j=j: e.scalar_tensor_tensor(out=acc.t[:, 0:TBP], in0=uab.t[:, kt, j:j + TBP], scalar=cw(j), in1=acc.t[:, 0:TBP], op0=ALU.mult, op1=ALU.add),
                        r=[uab.s(kt, kt + 1), lpt.s(), acc.s()], w=[acc.s()])
                k.A(lambda e, kt=kt, acc=acc: e.activation(out=CS.t[:, kt, 0:TBP], in_=acc.t[:, 0:TBP], func=AF.Silu), r=[acc.s()], w=[CS.s(kt, kt + 1)])
                if last:
                    accs = small("accs%d" % (kt % 2), [128, NS])
                    k.V(lambda e, kt=kt, accs=accs, cw=cw: e.tensor_scalar(out=accs.t[:, :], in0=c0t.t[:, kt, :, 0], scalar1=cw(0), scalar2=lpc(LP_CB + kt), op0=ALU.mult, op1=ALU.add),
                        r=[c0t.s(), lpt.s()], w=[accs.s()])
                    for j in range(1, 3):
                        k.V(lambda e, kt=kt, accs=accs, cw=cw, j=j: e.scalar_tensor_tensor(out=accs.t[:, :], in0=c0t.t[:, kt, :, j], scalar=cw(j), in1=accs.t[:, :], op0=ALU.mult, op1=ALU.add),
                            r=[c0t.s(), lpt.s(), accs.s()], w=[accs.s()])
                    k.V(lambda e, kt=kt, accs=accs, cw=cw: e.scalar_tensor_tensor(out=accs.t[:, :], in0=uab.t[:, kt, 3 + TBP:3 + TB], scalar=cw(3), in1=accs.t[:, :], op0=ALU.mult, op1=ALU.add),
                        r=[uab.s(kt, kt + 1), lpt.s(), accs.s()], w=[accs.s()])
                    k.A(lambda e, kt=kt, accs=accs: e.activation(out=CS.t[:, kt, TBP:TB], in_=accs.t[:, :], func=AF.Silu), r=[accs.s()], w=[CS.s(kt, kt + 1)])
            if last:
                k.V(lambda e: e.tensor_copy(out=cpo.t[:, :, :], in_=uab.t[:, :, TBP:TBP + 3]), r=[uab.s()], w=[cpo.s()])
                cv = o_convp.t[l].rearrange("(kt p) r -> p kt r", p=128)
                k.Dm(lambda e, cv=cv: e.dma_start(out=cv, in_=cpo.t[:, :, :]), r=[cpo.s()], w=[o_convp.s(l, l + 1)])
                k.V(lambda e: e.tensor_copy(out=cso.t[:, :, :, 0:2], in_=c0t.t[:, :, :, 1:3]), r=[c0t.s()], w=[cso.s()])
                k.V(lambda e: e.tensor_copy(out=cso.t[:, :, :, 2], in_=uab.t[:, :, 3 + TBP:3 + TB]), r=[uab.s()], w=[cso.s()])
                k.Dm(lambda e: e.dma_start(out=o_convs.t[l], in_=cso.t[:, :, :, :].rearrange("p a b c -> p (a b c)")), r=[cso.s()], w=[o_convs.s(l, l + 1)])
            k.V(lambda e: e.tensor_copy(out=hist.t[:, l, :, :], in_=uab.t[:, :, TBP:TBP + 3]), r=[uab.s()], w=[hist.s(l, l + 1)])

        def logsig(src_ap, dst_ap, tmp_ap, np_, rd, wr):
            k.A(lambda e: e.activation(out=tmp_ap, in_=src_ap, func=AF.Abs), r=rd, w=wr)
            k.A(lambda e: e.activation(out=tmp_ap, in_=tmp_ap, func=AF.Exp, scale=-1.0), r=rd, w=wr)
            k.A(lambda e: e.activation(out=tmp_ap, in_=tmp_ap, func=AF.Ln, bias=onec.t[0:np_, 0:1]), r=rd + [onec.s()], w=wr)
            k.V(lambda e: e.tensor_scalar_min(out=dst_ap, in0=src_ap, scalar1=0.0), r=rd, w=wr)
            k.V(lambda e: e.tensor_tensor(out=dst_ap, in0=dst_ap, in1=tmp_ap, op=ALU.subtract), r=rd, w=wr)

        def mlstm_stage(blk, l):
            last = blk == NB - 1
            cks = chunks(blk)
            wq_s = k.wload(wqkv.t[l, 0]); wk_s = k.wload(wqkv.t[l, 1]); wv_s = k.wload(wqkv.t[l, 2])

            def wsl(ws, hd, kt2, e0, e1):
                b0 = (hd * 2 + kt2) * 256
                return ws.t[:, b0 + e0:b0 + e1]
            gw = small("gw", [128, 40])
            g4 = small("g4", [4, 16])
            GW = [gw.s()]; G4 = [g4.s()]
            for ck in range(4):
                c0 = ck * 128
                bk = k.bank()
                for kt in range(16):
                    k.M(lambda e, kt=kt, c0=c0, bk=bk: e.matmul(bk.t[:, 0:8], lhsT=xn.t[:, kt, c0:c0 + 128], rhs=wif.t[:, kt * 8:kt * 8 + 8], start=(kt == 0), stop=(kt == 15)),
                        r=[xn.s(kt, kt + 1), wif.s()], w=[bk.s()])
                k.V(lambda e, bk=bk: e.tensor_tensor(out=gw.t[:, 0:8], in0=bk.t[:, 0:8], in1=lpt.t[:, LP_BIF:LP_BIF + 8], op=ALU.add), r=[bk.s(), lpt.s()], w=GW)
                logsig(gw.t[:, 4:8], gw.t[:, 8:12], gw.t[:, 12:16], 128, GW, GW)
                b2 = k.bank()
                k.M(lambda e, b2=b2: e.matmul(b2.t[:, 0:4], lhsT=tri_f, rhs=gw.t[:, 8:12], start=True, stop=True), r=[cst.s()] + GW, w=[b2.s()])
                k.V(lambda e, b2=b2: e.tensor_copy(out=gw.t[:, 16:20], in_=b2.t[:, 0:4]), r=[b2.s()], w=GW)
                k.V(lambda e, b2=b2: e.tensor_tensor(out=gw.t[:, 20:24], in0=gw.t[:, 0:4], in1=b2.t[:, 0:4], op=ALU.subtract), r=[b2.s()] + GW, w=GW)
                b3 = k.bank()
                k.M(lambda e, b3=b3: e.matmul(b3.t[0:4, 0:128], lhsT=gw.t[:, 20:24], rhs=ident_f, start=True, stop=True), r=[cst.s()] + GW, w=[b3.s()])
                k.M(lambda e, b3=b3: e.matmul(b3.t[0:4, 128:129], lhsT=gw.t[:, 8:12], rhs=ones_f.t[:, 0:1], start=True, stop=True), r=[ones_f.s()] + GW, w=[b3.s()])
                k.V(lambda e, b3=b3: e.tensor_reduce(out=g4.t[:, 0:1], in_=b3.t[0:4, 0:128], axis=AX.X, op=ALU.max), r=[b3.s()], w=G4)
                k.V(lambda e: e.tensor_tensor(out=g4.t[:, 1:2], in0=g4.t[:, 0:1], in1=mst.t[:, l:l + 1], op=ALU.max), r=G4 + [mst.s(l, l + 1)], w=G4)
                k.V(lambda e: e.tensor_tensor(out=g4.t[:, 2:3], in0=mst.t[:, l:l + 1], in1=g4.t[:, 1:2], op=ALU.subtract), r=G4 + [mst.s(l, l + 1)], w=G4)
                k.V(lambda e: e.tensor_scalar(out=g4.t[:, 4:8], in0=cst.t[0:4, C_ID:C_ID + 4], scalar1=g4.t[:, 1:2], scalar2=None, op0=ALU.mult), r=G4 + [cst.s()], w=G4)
                k.V(lambda e: e.tensor_scalar(out=g4.t[:, 8:12], in0=cst.t[0:4, C_ID:C_ID + 4], scalar1=g4.t[:, 2:3], scalar2=None, op0=ALU.mult), r=G4 + [cst.s()], w=G4)
                b4 = k.bank()
                k.M(lambda e, b4=b4: e.matmul(b4.t[:, 0:8], lhsT=ones_f.t[0:4, 0:128], rhs=g4.t[:, 4:12], start=True, stop=True), r=G4 + [ones_f.s()], w=[b4.s()])
                k.V(lambda e, b3=b3: e.tensor_tensor(out=mst.t[:, l:l + 1], in0=b3.t[0:4, 128:129], in1=g4.t[:, 1:2], op=ALU.add), r=[b3.s()] + G4, w=[mst.s(l, l + 1)])
                k.V(lambda e, b4=b4: e.tensor_tensor(out=gw.t[:, 24:28], in0=gw.t[:, 20:24], in1=b4.t[:, 0:4], op=ALU.subtract), r=[b4.s()] + GW, w=GW)
                k.A(lambda e: e.activation(out=gw.t[:, 24:28], in_=gw.t[:, 24:28], func=AF.Exp), r=GW, w=GW)
                k.V(lambda e, ck=ck: e.tensor_scalar(out=gts.t[:, ck, 0:4], in0=gw.t[:, 24:28], scalar1=0.0625, scalar2=None, op0=ALU.mult), r=GW, w=[gts.s(ck, ck + 1)])
                k.V(lambda e, b4=b4: e.tensor_tensor(out=gw.t[:, 28:32], in0=gw.t[:, 16:20], in1=b4.t[:, 0:4], op=ALU.add), r=[b4.s()] + GW, w=GW)
                k.A(lambda e, ck=ck: e.activation(out=gts.t[:, ck, 4:8], in_=gw.t[:, 28:32], func=AF.Exp, scale=-1.0), r=GW, w=[gts.s(ck, ck + 1)])
                k.A(lambda e, ck=ck, b4=b4: e.activation(out=gts.t[:, ck, 8:12], in_=b4.t[:, 4:8], func=AF.Exp), r=[b4.s()], w=[gts.s(ck, ck + 1)])
            if last:
                gs = small("gs", [NS, 44])
                GS = [gs.s()]
                c0e = small("c0e", [NS, NS, 4])
                bk = k.bank()
                for kt in range(16):
                    k.M(lambda e, kt=kt, bk=bk: e.matmul(bk.t[0:NS, 0:8], lhsT=xn.t[:, kt, TBP:TB], rhs=wif.t[:, kt * 8:kt * 8 + 8], start=(kt == 0), stop=(kt == 15)),
                        r=[xn.s(kt, kt + 1), wif.s()], w=[bk.s()])
                k.Dm(lambda e: e.dma_start(out=gs.t[:, 40:44], in_=stm.t[l]), r=GS, w=GS)
                k.V(lambda e, bk=bk: e.tensor_tensor(out=gs.t[:, 0:8], in0=bk.t[0:NS, 0:8], in1=lpt.t[0:NS, LP_BIF:LP_BIF + 8], op=ALU.add), r=[bk.s(), lpt.s()] + GS, w=GS)
                logsig(gs.t[:, 4:8], gs.t[:, 8:12], gs.t[:, 12:16], NS, GS, GS)
                VS = lambda fn: k.V(fn, r=GS, w=GS)
                AS = lambda fn: k.A(fn, r=GS, w=GS)
                VS(lambda e: e.tensor_tensor(out=gs.t[:, 36:40], in0=gs.t[:, 0:4], in1=gs.t[:, 8:12], op=ALU.subtract))
                VS(lambda e: e.tensor_tensor(out=gs.t[:, 16:20], in0=gs.t[:, 36:40], in1=gs.t[:, 40:44], op=ALU.max))
                VS(lambda e: e.tensor_tensor(out=gs.t[:, 12:16], in0=gs.t[:, 36:40], in1=gs.t[:, 16:20], op=ALU.subtract))
                AS(lambda e: e.activation(out=gs.t[:, 20:24], in_=gs.t[:, 12:16], func=AF.Exp))
                VS(lambda e: e.tensor_tensor(out=gs.t[:, 12:16], in0=gs.t[:, 40:44], in1=gs.t[:, 16:20], op=ALU.subtract))
                AS(lambda e: e.activation(out=gs.t[:, 24:28], in_=gs.t[:, 12:16], func=AF.Exp))
                VS(lambda e: e.tensor_tensor(out=gs.t[:, 28:32], in0=gs.t[:, 8:12], in1=gs.t[:, 16:20], op=ALU.add))
                AS(lambda e: e.activation(out=gs.t[:, 32:36], in_=gs.t[:, 28:32], func=AF.Exp, scale=-1.0))
                k.Dm(lambda e: e.dma_start(out=o_ms.t[l], in_=gs.t[:, 28:32]), r=GS, w=[o_ms.s(l, l + 1)])
                for hd in range(4):
                    k.V(lambda e, hd=hd: e.tensor_scalar(out=c0e.t[:, :, hd], in0=cst.t[0:NS, C_ID:C_ID + NS], scalar1=gs.t[:, 24 + hd:25 + hd], scalar2=None, op0=ALU.mult),
                        r=GS + [cst.s()], w=[c0e.s()])
                bk = k.bank()
                k.M(lambda e, bk=bk: e.matmul(bk.t[:, 0:64], lhsT=ones_f.t[0:NS, 0:128], rhs=c0e.t[:, :, :].rearrange("p a b -> p (a b)"), start=True, stop=True),
                    r=[c0e.s(), ones_f.s()], w=[bk.s()])
                k.V(lambda e, bk=bk: e.tensor_copy(out=c0bc.t[:, :], in_=bk.t[:, 0:64]), r=[bk.s()], w=[c0bc.s()])
                k.Dm(lambda e: e.dma_start(out=nall.t[:, :], in_=stn.t[l]), w=[nall.s()])

            for hd in range(4):
                QK = qk[0]
                for which, ws in ((0, wq_s), (1, wk_s)):
                    for et in range(2):
                        for (c0, n) in cks:
                            bk = k.bank()
                            for kt2 in range(2):
                                k.M(lambda e, ws=ws, kt2=kt2, et=et, c0=c0, n=n, bk=bk, hd=hd: e.matmul(
                                    bk.t[:, :n], lhsT=wsl(ws, hd, kt2, et * 128, et * 128 + 128), rhs=CS.t[:, 2 * hd + kt2, c0:c0 + n], start=(kt2 == 0), stop=(kt2 == 1)),
                                    r=[ws.s(), CS.s(2 * hd + kt2, 2 * hd + kt2 + 1)], w=[bk.s()])
                            k.A(lambda e, QK=QK, which=which, et=et, c0=c0, n=n, bk=bk: e.activation(out=QK.t[:, which * 2 + et, c0:c0 + n], in_=bk.t[:, :n], func=AF.Copy),
                                r=[bk.s()], w=[QK.s(which * 2 + et, which * 2 + et + 1)])
                cslots = [(Cst[l].b, hd, hd + 1), (Cst[l].b, 4 + hd, 5 + hd)]
                for ck in range(4):
                    c0 = ck * 128
                    i2 = (hd * 4 + ck) % 2
                    kt_, ve, sp, cb, hn_, tg = ktok[i2], vext[i2], spT[i2], cbf[i2], hno[i2], tmpg[i2]
                    ek = gts.t[:, ck, hd:hd + 1]; thr = gts.t[:, ck, 4 + hd:5 + hd]; c0b = gts.t[:, ck, 8 + hd:9 + hd]
                    GT = [gts.s(ck, ck + 1)]
                    bk = k.bank()
                    for kt2 in range(2):
                        k.M(lambda e, kt2=kt2, c0=c0, bk=bk, hd=hd: e.matmul(bk.t[:, 0:256], lhsT=CS.t[:, 2 * hd + kt2, c0:c0 + 128], rhs=wsl(wk_s, hd, kt2, 0, 256), start=(kt2 == 0), stop=(kt2 == 1)),
                            r=[wk_s.s(), CS.s(2 * hd + kt2, 2 * hd + kt2 + 1)], w=[bk.s()])
                    k.A(lambda e, kt_=kt_, bk=bk, ek=ek: e.activation(out=kt_.t[:, :], in_=bk.t[:, 0:256], func=AF.Copy, scale=ek), r=[bk.s()] + GT, w=[kt_.s()])
                    bk = k.bank()
                    for kt2 in range(2):
                        k.M(lambda e, kt2=kt2, c0=c0, bk=bk, hd=hd: e.matmul(bk.t[:, 0:256], lhsT=uab.t[:, 2 * hd + kt2, 3 + c0:3 + c0 + 128], rhs=wsl(wv_s, hd, kt2, 0, 256), start=(kt2 == 0), stop=(kt2 == 1)),
                            r=[wv_s.s(), uab.s(2 * hd + kt2, 2 * hd + kt2 + 1)], w=[bk.s()])
                    k.V(lambda e, ve=ve, bk=bk: e.tensor_copy(out=ve.t[:, 0:256], in_=bk.t[:, 0:256]), r=[bk.s()], w=[ve.s()])
                    bk = k.bank()
                    for kt2 in range(2):
                        k.M(lambda e, kt2=kt2, c0=c0, bk=bk, QK=QK: e.matmul(bk.t[:, 0:128], lhsT=QK.t[:, 2 + kt2, c0:c0 + 128], rhs=QK.t[:, kt2, c0:c0 + 128], start=(kt2 == 0), stop=(kt2 == 1)),
                            r=[QK.s(2 + kt2, 3 + kt2), QK.s(kt2, kt2 + 1)], w=[bk.s()])
                    k.V(lambda e, sp=sp, bk=bk, ek=ek: e.scalar_tensor_tensor(out=sp.t[:, :], in0=bk.t[:, 0:128], scalar=ek, in1=tri_f, op0=ALU.mult, op1=ALU.mult),
                        r=[bk.s(), cst.s()] + GT, w=[sp.s()])
                    k.V(lambda e, cb=cb, c0b=c0b, hd=hd: e.tensor_scalar(out=cb.t[:, :, :], in0=Cst[l].t[:, :, hd, :], scalar1=c0b, scalar2=None, op0=ALU.mult),
                        r=cslots + GT, w=[cb.s()])
                    hb = k.bank()
                    k.M(lambda e, hb=hb, sp=sp, ve=ve: e.matmul(hb.t[:, 0:257], lhsT=sp.t[:, :], rhs=ve.t[:, :], start=True, stop=False), r=[sp.s(), ve.s()], w=[hb.s()])
                    for kt2 in range(2):
                        k.M(lambda e, hb=hb, kt2=kt2, c0=c0, cb=cb, QK=QK: e.matmul(hb.t[:, 0:257], lhsT=QK.t[:, kt2, c0:c0 + 128], rhs=cb.t[:, kt2, :], start=False, stop=(kt2 == 1)),
                            r=[QK.s(kt2, kt2 + 1), cb.s()], w=[hb.s()])
                    nr = small("nr%d" % i2, [128, 8])
                    NR = [nr.s()]
                    k.A(lambda e, hb=hb, nr=nr: e.activation(out=nr.t[:, 0:1], in_=hb.t[:, 256:257], func=AF.Abs), r=[hb.s()], w=NR)
                    k.V(lambda e, nr=nr, thr=thr: e.tensor_tensor(out=nr.t[:, 0:1], in0=nr.t[:, 0:1], in1=thr, op=ALU.max), r=NR + GT, w=NR)
                    k.V(lambda e, nr=nr: e.reciprocal(out=nr.t[:, 1:2], in_=nr.t[:, 0:1]), r=NR, w=NR)
                    k.A(lambda e, hb=hb, nr=nr: e.activation(out=sqv.t[:, :], in_=hb.t[:, 0:256], func=AF.Square, scale=nr.t[:, 1:2]), r=[hb.s()] + NR, w=[sqv.s()])
                    k.V(lambda e, nr=nr: e.tensor_reduce(out=nr.t[:, 2:3], in_=sqv.t[:, :], axis=AX.X, op=ALU.add), r=[sqv.s()], w=NR)
                    k.A(lambda e, nr=nr: e.activation(out=nr.t[:, 3:4], in_=nr.t[:, 2:3], func=AF.Sqrt, bias=epsc.t[:, 0:1], scale=1.0 / 256), r=NR + [epsc.s()], w=NR)
                    k.V(lambda e, nr=nr: e.reciprocal(out=nr.t[:, 4:5], in_=nr.t[:, 3:4]), r=NR, w=NR)
                    k.V(lambda e, nr=nr: e.tensor_tensor(out=nr.t[:, 5:6], in0=nr.t[:, 4:5], in1=nr.t[:, 1:2], op=ALU.mult), r=NR, w=NR)
                    k.V(lambda e, hb=hb, nr=nr, hn_=hn_, hd=hd: e.scalar_tensor_tensor(out=hn_.t[:, :], in0=hb.t[:, 0:256], scalar=nr.t[:, 5:6], in1=gh.t[:, hd * 256:(hd + 1) * 256], op0=ALU.mult, op1=ALU.mult),
                        r=[hb.s(), gh.s()] + NR, w=[hn_.s()])
                    for et in range(2):
                        k.M(lambda e, et=et, hn_=hn_: e.transpose(out=psT.t[:, et * 128:(et + 1) * 128], in_=hn_.t[:, et * 128:(et + 1) * 128], identity=ident_bf.t[:, :]),
                            r=[hn_.s(), ident_bf.s()], w=[psT.s()])
                    for et in range(2):
                        ft = 2 * hd + et
                        k.V(lambda e, et=et, ft=ft, tg=tg, c0=c0: e.scalar_tensor_tensor(out=tg.t[:, :], in0=CS.t[:, ft, c0:c0 + 128], scalar=lpc(LP_SKIP + ft), in1=psT.t[:, et * 128:(et + 1) * 128], op0=ALU.mult, op1=ALU.add),
                            r=[CS.s(ft, ft + 1), lpt.s(), psT.s()], w=[tg.s()])
                        k.V(lambda e, ft=ft, tg=tg, c0=c0: e.tensor_tensor(out=apre.t[:, ft, c0:c0 + 128], in0=tg.t[:, :], in1=CS.t[:, 8 + ft, c0:c0 + 128], op=ALU.mult),
                            r=[tg.s(), CS.s(8 + ft, 9 + ft)], w=[apre.s(ft, ft + 1)])
                    for kt2 in range(2):
                        ubk = k.bank()
                        k.M(lambda e, ubk=ubk, kt2=kt2, kt_=kt_, ve=ve: e.matmul(ubk.t[:, 0:257], lhsT=kt_.t[:, kt2 * 128:(kt2 + 1) * 128], rhs=ve.t[:, :], start=True, stop=True),
                            r=[kt_.s(), ve.s()], w=[ubk.s()])
                        sl = (Cst[l].b, kt2 * 4 + hd, kt2 * 4 + hd + 1)
                        k.V(lambda e, ubk=ubk, kt2=kt2, c0b=c0b, hd=hd: e.scalar_tensor_tensor(out=Cst[l].t[:, kt2, hd, :], in0=Cst[l].t[:, kt2, hd, :], scalar=c0b, in1=ubk.t[:, 0:257], op0=ALU.mult, op1=ALU.add),
                            r=[ubk.s(), sl] + GT, w=[sl])
                if last:
                    head_sample(l, hd, QK, wq_s, wk_s, wv_s, wsl)
            if last:
                for hd in range(4):
                    cpv = o_Cp.t[l, hd].rearrange("(kt p) e -> p kt e", p=128)
                    k.Dm(lambda e, cpv=cpv, hd=hd: e.dma_start(out=cpv, in_=Cst[l].t[:, :, hd, 0:256]), r=[Cst[l].s()], w=[o_Cp.s(l * 4 + hd, l * 4 + hd + 1)])
                k.V(lambda e: e.tensor_copy(out=npo.t[:, :].rearrange("p (a b) -> p a b", a=2), in_=Cst[l].t[:, :, :, 256]), r=[Cst[l].s()], w=[npo.s()])
                k.Dm(lambda e: e.dma_start(out=o_np.t[l], in_=npo.t[:, :]), r=[npo.s()], w=[o_np.s(l, l + 1)])
                k.Dm(lambda e: e.dma_start(out=o_mp.t[l].rearrange("(h o) -> h o", o=1), in_=mst.t[:, l:l + 1]), r=[mst.s(l, l + 1)], w=[o_mp.s(l, l + 1)])
                k.Dm(lambda e: e.dma_start(out=o_ns.t[l], in_=nnew.t[:, :]), r=[nnew.s()], w=[o_ns.s(l, l + 1)])

        def head_sample(l, hd, QK, wq_s, wk_s, wv_s, wsl):
            gs = sm["gs"]; GS = [gs.s()]
            e_hd = gs.t[:, 20 + hd:21 + hd]; c0_hd = gs.t[:, 24 + hd:25 + hd]; thr_hd = gs.t[:, 32 + hd:33 + hd]
            Qs = small("Qs", [NS, 256]); Ks = small("Ks", [NS, 256]); Vs = small("Vs", [NS, 256]); Ke = small("Ke", [NS, 256])
            num = small("num", [NS, 256]); hnos = small("hnos", [NS, 256], BF16)
            sr = small("sr", [NS, 12])
            SR = [sr.s()]
            bq = k.bank(); bkk = k.bank(); bv = k.bank()
            for kt2 in range(2):
                k.M(lambda e, kt2=kt2, bq=bq: e.matmul(bq.t[0:NS, 0:256], lhsT=CS.t[:, 2 * hd + kt2, TBP:TB], rhs=wsl(wq_s, hd, kt2, 0, 256), start=(kt2 == 0), stop=(kt2 == 1)),
                    r=[wq_s.s(), CS.s(2 * hd + kt2, 2 * hd + kt2 + 1)], w=[bq.s()])
            for kt2 in range(2):
                k.M(lambda e, kt2=kt2, bkk=bkk: e.matmul(bkk.t[0:NS, 0:256], lhsT=CS.t[:, 2 * hd + kt2, TBP:TB], rhs=wsl(wk_s, hd, kt2, 0, 256), start=(kt2 == 0), stop=(kt2 == 1)),
                    r=[wk_s.s(), CS.s(2 * hd + kt2, 2 * hd + kt2 + 1)], w=[bkk.s()])
            for kt2 in range(2):
                k.M(lambda e, kt2=kt2, bv=bv: e.matmul(bv.t[0:NS, 0:256], lhsT=uab.t[:, 2 * hd + kt2, 3 + TBP:3 + TB], rhs=wsl(wv_s, hd, kt2, 0, 256), start=(kt2 == 0), stop=(kt2 == 1)),
                    r=[wv_s.s(), uab.s(2 * hd + kt2, 2 * hd + kt2 + 1)], w=[bv.s()])
            k.A(lambda e: e.activation(out=Qs.t[:, :], in_=bq.t[0:NS, 0:256], func=AF.Copy), r=[bq.s()], w=[Qs.s()])
            k.A(lambda e: e.activation(out=Ks.t[:, :], in_=bkk.t[0:NS, 0:256], func=AF.Copy, scale=0.0625), r=[bkk.s()], w=[Ks.s()])
            k.V(lambda e: e.tensor_copy(out=Vs.t[:, :], in_=bv.t[0:NS, 0:256]), r=[bv.s()], w=[Vs.s()])
            k.V(lambda e: e.tensor_copy(out=vsb.t[:, 0:256], in_=bv.t[0:NS, 0:256]), r=[bv.s()], w=[vsb.s()])
            k.V(lambda e: e.tensor_tensor(out=num.t[:, :], in0=Qs.t[:, :], in1=Ks.t[:, :], op=ALU.mult), r=[Qs.s(), Ks.s()], w=[num.s()])
            k.V(lambda e: e.tensor_reduce(out=sr.t[:, 0:1], in_=num.t[:, :], axis=AX.X, op=ALU.add), r=[num.s()], w=SR)
            k.V(lambda e: e.tensor_scalar(out=Ke.t[:, :], in0=Ks.t[:, :], scalar1=e_hd, scalar2=None, op0=ALU.mult), r=[Ks.s()] + GS, w=[Ke.s()])
            for kt2 in range(2):
                k.V(lambda e, kt2=kt2: e.tensor_tensor(out=qmask.t[:, kt2, :, :], in0=QK.t[:, kt2:kt2 + 1, TBP:TB].to_broadcast([128, NS, NS]), in1=d16, op=ALU.mult),
                    r=[QK.s(kt2, kt2 + 1), cst.s()], w=[qmask.s()])
            HS = psA
            for j in range(NS):
                cj = VT(G1f[j % 3].t[:, 0:514].rearrange("p (a b) -> p a b", a=2), G1.b, 2 * (j % 3), 2 * (j % 3) + 2)
                cn = VT(G1f[3 + j % 2].t[:, 0:514].rearrange("p (a b) -> p a b", a=2), G1.b, 2 * (3 + j % 2), 2 * (3 + j % 2) + 2)
                rq = 5 + j % 2
                rj = VT(G1.t[:, 2 * rq, 0:514].rearrange("p (a b) -> p a b", a=2), G1.b, 2 * rq, 2 * rq + 1)
                km = kmask[j % 2]
                idx = (j * 4 + hd) * 2
                src = stC.t[l, j, hd].rearrange("(kt p) e -> p kt e", p=128)
                k.Dm(lambda e, cj=cj, src=src: e.dma_start(out=cj.t[:, :, 0:256], in_=src), w=[cj.s()])
                k.V(lambda e, cj=cj, idx=idx: e.tensor_copy(out=cj.t[:, :, 256], in_=nall.t[:, idx:idx + 2]), r=[nall.s()], w=[cj.s()])
                k.A(lambda e, cj=cj, rj=rj: e.activation(out=rj.t[:, :, :], in_=cj.t[:, :, :], func=AF.Copy), r=[cj.s()], w=[rj.s()])
                for kt2 in range(2):
                    k.M(lambda e, j=j, kt2=kt2, rj=rj: e.matmul(HS.t[0:NS, 0:257], lhsT=qmask.t[:, kt2, j, :], rhs=rj.t[:, kt2, :], start=(j == 0 and kt2 == 0), stop=(j == NS - 1 and kt2 == 1)),
                        r=[qmask.s(), rj.s()], w=[HS.s()])
                k.V(lambda e, km=km, j=j: e.tensor_scalar(out=km.t[:, :], in0=Ke.t[:, :], scalar1=cst.t[0:NS, C_ID + j:C_ID + j + 1], scalar2=None, op0=ALU.mult),
                    r=[Ke.s(), cst.s()], w=[km.s()])
                for kt2 in range(2):
                    ubk = k.bank()
                    k.M(lambda e, ubk=ubk, km=km, kt2=kt2: e.matmul(ubk.t[:, 0:257], lhsT=km.t[:, kt2 * 128:(kt2 + 1) * 128], rhs=vsb.t[:, :], start=True, stop=True),
                        r=[km.s(), vsb.s()], w=[ubk.s()])
                    k.V(lambda e, ubk=ubk, kt2=kt2, cj=cj, cn=cn, j=j: e.scalar_tensor_tensor(out=cn.t[:, kt2, :], in0=cj.t[:, kt2, :], scalar=c0bc.t[:, j * 4 + hd:j * 4 + hd + 1], in1=ubk.t[:, 0:257], op0=ALU.mult, op1=ALU.add),
                        r=[ubk.s(), cj.s(), c0bc.s()], w=[cn.s()])
                dst = o_Cs.t[l, j, hd].rearrange("(kt p) e -> p kt e", p=128)
                slot = (l * NS + j) * 4 + hd
                k.Dm(lambda e, cn=cn, dst=dst: e.dma_start(out=dst, in_=cn.t[:, :, 0:256]), r=[cn.s()], w=[o_Cs.s(slot, slot + 1)])
                k.V(lambda e, cn=cn, idx=idx: e.tensor_copy(out=nnew.t[:, idx:idx + 2], in_=cn.t[:, :, 256]), r=[cn.s()], w=[nnew.s()])
            k.V(lambda e: e.tensor_scalar(out=num.t[:, :], in0=HS.t[0:NS, 0:256], scalar1=c0_hd, scalar2=None, op0=ALU.mult), r=[HS.s()] + GS, w=[num.s()])
            k.V(lambda e: e.tensor_tensor(out=sr.t[:, 1:2], in0=sr.t[:, 0:1], in1=e_hd, op=ALU.mult), r=SR + GS, w=SR)
            k.V(lambda e: e.scalar_tensor_tensor(out=num.t[:, :], in0=Vs.t[:, :], scalar=sr.t[:, 1:2], in1=num.t[:, :], op0=ALU.mult, op1=ALU.add), r=[Vs.s(), num.s()] + SR, w=[num.s()])
            k.V(lambda e: e.scalar_tensor_tensor(out=sr.t[:, 2:3], in0=HS.t[0:NS, 256:257], scalar=c0_hd, in1=sr.t[:, 1:2], op0=ALU.mult, op1=ALU.add), r=[HS.s()] + SR + GS, w=SR)
            k.A(lambda e: e.activation(out=sr.t[:, 3:4], in_=sr.t[:, 2:3], func=AF.Abs), r=SR, w=SR)
            k.V(lambda e: e.tensor_tensor(out=sr.t[:, 3:4], in0=sr.t[:, 3:4], in1=thr_hd, op=ALU.max), r=SR + GS, w=SR)
            k.V(lambda e: e.reciprocal(out=sr.t[:, 4:5], in_=sr.t[:, 3:4]), r=SR, w=SR)
            k.A(lambda e: e.activation(out=sqv.t[0:NS, :], in_=num.t[:, :], func=AF.Square, scale=sr.t[:, 4:5]), r=[num.s()] + SR, w=[sqv.s()])
            k.V(lambda e: e.tensor_reduce(out=sr.t[:, 5:6], in_=sqv.t[0:NS, :], axis=AX.X, op=ALU.add), r=[sqv.s()], w=SR)
            k.A(lambda e: e.activation(out=sr.t[:, 6:7], in_=sr.t[:, 5:6], func=AF.Sqrt, bias=epsc.t[0:NS, 0:1], scale=1.0 / 256), r=SR + [epsc.s()], w=SR)
            k.V(lambda e: e.reciprocal(out=sr.t[:, 7:8], in_=sr.t[:, 6:7]), r=SR, w=SR)
            k.V(lambda e: e.tensor_tensor(out=sr.t[:, 8:9], in0=sr.t[:, 7:8], in1=sr.t[:, 4:5], op=ALU.mult), r=SR, w=SR)
            k.V(lambda e: e.scalar_tensor_tensor(out=hnos.t[:, :], in0=num.t[:, :], scalar=sr.t[:, 8:9], in1=gh.t[0:NS, hd * 256:(hd + 1) * 256], op0=ALU.mult, op1=ALU.mult),
                r=[num.s(), gh.s()] + SR, w=[hnos.s()])
            for et in range(2):
                k.M(lambda e, et=et: e.transpose(out=psT.t[:, 512 + et * NS:512 + (et + 1) * NS], in_=hnos.t[:, et * 128:(et + 1) * 128], identity=ident_bf.t[0:NS, 0:NS]),
                    r=[hnos.s(), ident_bf.s()], w=[psT.s()])
            tg = tmpg[0]
            for et in range(2):
                ft = 2 * hd + et
                k.V(lambda e, et=et, ft=ft: e.scalar_tensor_tensor(out=tg.t[:, 0:NS], in0=CS.t[:, ft, TBP:TB], scalar=lpc(LP_SKIP + ft), in1=psT.t[:, 512 + et * NS:512 + (et + 1) * NS], op0=ALU.mult, op1=ALU.add),
                    r=[CS.s(ft, ft + 1), lpt.s(), psT.s()], w=[tg.s()])
                k.V(lambda e, ft=ft: e.tensor_tensor(out=apre.t[:, ft, TBP:TB], in0=tg.t[:, 0:NS], in1=CS.t[:, 8 + ft, TBP:TB], op=ALU.mult),
                    r=[tg.s(), CS.s(8 + ft, 9 + ft)], w=[apre.s(ft, ft + 1)])

        def mixer(blk, l):
            k.Dm(lambda e: e.dma_start(out=lpt.t[:, :], in_=lp.t[l]), w=[lpt.s()])
            k.Dm(lambda e: e.dma_start(out=gh.t[:, :], in_=ghd.t[l]), w=[gh.s()])
            k.Wm(lambda e: e.dma_start(out=wif.t[:, :], in_=w_if.t[l]), w=[wif.s()])
            rms(blk, l, xn)
            k.V(lambda e: e.tensor_copy(out=uab.t[:, :, 0:3], in_=hist.t[:, l, :, :]), r=[hist.s(l, l + 1)], w=[uab.s()])
            fm_proj(blk, [w_in_t.t[l, j] for j in range(8)], 16, xn, epi_act(uab, AF.Copy, 0, col0=3))
            fm_proj(blk, [w_in_t.t[l, 16 + j] for j in range(8)], 16, xn, epi_act(ub, AF.Copy))
            s5_stage(blk, l)
            conv_stage(blk, l)
            fm_proj(blk, [w_in_t.t[l, 8 + j] for j in range(8)], 16, xn, epi_act(CS, AF.Sigmoid, base=8))
            mlstm_stage(blk, l)
            if STOP_AFTER == ("mlstm", l):
                return
            k.banks = rot7
            fm_proj(blk, [w_in_t.t[l, 24 + j] for j in range(16)], 16, xn, epi_act(G1, AF.Sigmoid))

            def epi_mul(mt, c0, n, bk):
                k.V(lambda e, mt=mt, c0=c0, n=n, bk=bk: e.tensor_tensor(out=G1.t[:, mt, c0:c0 + n], in0=bk.t[:, :n], in1=G1.t[:, mt, c0:c0 + n], op=ALU.mult),
                    r=[bk.s(), G1.s(mt, mt + 1)], w=[G1.s(mt, mt + 1)])
            fm_proj(blk, [w_proj_t.t[l, j] for j in range(8)], 8, apre, epi_mul, mt_per_slot=2)
            wv_slot = wg_slot = None
            for j in range(16):
                wb = k.wload(w_in_t.t[l, 40 + j])
                if j % 2 == 0:
                    wv_slot = k.wload(w_glu_t.t[l, j // 2]); wg_slot = k.wload(w_glu_t.t[l, 8 + j // 2])
                mm = j % 2
                for (c0, n) in chunks(blk):
                    bb = k.bank(); bv = k.bank(); bg = k.bank()
                    for kt in range(16):
                        k.M(lambda e, wb=wb, kt=kt, c0=c0, n=n, bb=bb: e.matmul(bb.t[:, :n], lhsT=wb.t[:, kt * 128:(kt + 1) * 128], rhs=xn.t[:, kt, c0:c0 + n], start=(kt == 0), stop=(kt == 15)),
                            r=[wb.s(), xn.s(kt, kt + 1)], w=[bb.s()])
                    for kt in range(8):
                        off = (mm * 8 + kt) * 128
                        k.M(lambda e, ws=wv_slot, off=off, kt=kt, c0=c0, n=n, bv=bv: e.matmul(bv.t[:, :n], lhsT=ws.t[:, off:off + 128], rhs=ub.t[:, kt, c0:c0 + n], start=(kt == 0), stop=(kt == 7)),
                            r=[wv_slot.s(), ub.s(kt, kt + 1)], w=[bv.s()])
                    for kt in range(8):
                        off = (mm * 8 + kt) * 128
                        k.M(lambda e, ws=wg_slot, off=off, kt=kt, c0=c0, n=n, bg=bg: e.matmul(bg.t[:, :n], lhsT=ws.t[:, off:off + 128], rhs=ub.t[:, kt, c0:c0 + n], start=(kt == 0), stop=(kt == 7)),
                            r=[wg_slot.s(), ub.s(kt, kt + 1)], w=[bg.s()])
                    s_b = SC[1]; s_g = SC[2]
                    k.A(lambda e, n=n, bb=bb: e.activation(out=s_b.t[:, :n], in_=bb.t[:, :n], func=AF.Sigmoid), r=[bb.s()], w=[s_b.s()])
                    k.A(lambda e, n=n, bg=bg: e.activation(out=s_g.t[:, :n], in_=bg.t[:, :n], func=AF.Sigmoid), r=[bg.s()], w=[s_g.s()])
                    k.V(lambda e, n=n, bv=bv: e.tensor_tensor(out=s_g.t[:, :n], in0=bv.t[:, :n], in1=s_g.t[:, :n], op=ALU.mult), r=[bv.s(), s_g.s()], w=[s_g.s()])
                    k.V(lambda e, n=n: e.tensor_tensor(out=s_g.t[:, :n], in0=s_g.t[:, :n], in1=s_b.t[:, :n], op=ALU.mult), r=[s_g.s(), s_b.s()], w=[s_g.s()])
                    k.V(lambda e, n=n, c0=c0, j=j: e.tensor_tensor(out=G1.t[:, j, c0:c0 + n], in0=s_g.t[:, :n], in1=G1.t[:, j, c0:c0 + n], op=ALU.add), r=[s_g.s(), G1.s(j, j + 1)], w=[G1.s(j, j + 1)])

            def epi_addh(mt, c0, n, bk):
                k.V(lambda e, mt=mt, c0=c0, n=n, bk=bk: e.tensor_tensor(out=h.t[:, mt, c0:c0 + n], in0=bk.t[:, :n], in1=h.t[:, mt, c0:c0 + n], op=ALU.add),
                    r=[bk.s(), h.s(mt, mt + 1)], w=[h.s(mt, mt + 1)])
            fm_proj(blk, [w_out_t.t[l, j] for j in range(16)], 16, G1, epi_addh)
            k.banks = rot4

        def ffn_group_list():
            gl = []
            f = 0
            while f < DFF_T:
                gl.append(list(range(f, min(f + 4, DFF_T))))
                f += 4
            return gl

        def ffn(blk, wg_t, wu_t, wd_t, gate_tiles=None, parity0=0):
            for gi, grp in enumerate(ffn_group_list()):
                hb0 = ((gi + parity0) % 2) * 4
                for fi, f in enumerate(grp):
                    wg = k.wload(wg_t[f]); wu = k.wload(wu_t[f])
                    for (c0, n) in chunks(blk):
                        bg = k.bank(); bu = k.bank()
                        for kt in range(16):
                            k.M(lambda e, wg=wg, kt=kt, c0=c0, n=n, bg=bg: e.matmul(bg.t[:, :n], lhsT=wg.t[:, kt * 128:(kt + 1) * 128], rhs=xn.t[:, kt, c0:c0 + n], start=(kt == 0), stop=(kt == 15)),
                                r=[wg.s(), xn.s(kt, kt + 1)], w=[bg.s()])
                        for kt in range(16):
                            k.M(lambda e, wu=wu, kt=kt, c0=c0, n=n, bu=bu: e.matmul(bu.t[:, :n], lhsT=wu.t[:, kt * 128:(kt + 1) * 128], rhs=xn.t[:, kt, c0:c0 + n], start=(kt == 0), stop=(kt == 15)),
                                r=[wu.s(), xn.s(kt, kt + 1)], w=[bu.s()])
                        sg = SC[1 + (fi % 2)]
                        k.A(lambda e, n=n, bg=bg, sg=sg: e.activation(out=sg.t[:, :n], in_=bg.t[:, :n], func=AF.Silu), r=[bg.s()], w=[sg.s()])
                        if gate_tiles is None:
                            k.V(lambda e, n=n, c0=c0, bu=bu, sg=sg, hb0=hb0, fi=fi: e.tensor_tensor(out=apre.t[:, hb0 + fi, c0:c0 + n], in0=sg.t[:, :n], in1=bu.t[:, :n], op=ALU.mult),
                                r=[sg.s(), bu.s()], w=[apre.s(hb0 + fi, hb0 + fi + 1)])
                        else:
                            k.V(lambda e, n=n, bu=bu, sg=sg: e.tensor_tensor(out=sg.t[:, :n], in0=sg.t[:, :n], in1=bu.t[:, :n], op=ALU.mult), r=[sg.s(), bu.s()], w=[sg.s()])
                            k.V(lambda e, n=n, c0=c0, sg=sg, hb0=hb0, fi=fi: e.tensor_tensor(out=apre.t[:, hb0 + fi, c0:c0 + n], in0=sg.t[:, :n], in1=gate_tiles.t[:, c0:c0 + n], op=ALU.mult),
                                r=[sg.s(), gate_tiles.s()], w=[apre.s(hb0 + fi, hb0 + fi + 1)])
                wds = [k.wload(wd_t[f]) for f in grp]
                for dt_ in range(16):
                    for (c0, n) in chunks(blk):
                        bk = k.bank()
                        for fi in range(len(grp)):
                            k.M(lambda e, wd=wds[fi], dt_=dt_, fi=fi, c0=c0, n=n, bk=bk, hb0=hb0: e.matmul(bk.t[:, :n], lhsT=wd.t[:, dt_ * 128:(dt_ + 1) * 128], rhs=apre.t[:, hb0 + fi, c0:c0 + n], start=(fi == 0), stop=(fi == len(grp) - 1)),
                                r=[wds[fi].s(), apre.s(hb0 + fi, hb0 + fi + 1)], w=[bk.s()])
                        k.V(lambda e, dt_=dt_, c0=c0, n=n, bk=bk: e.tensor_tensor(out=h.t[:, dt_, c0:c0 + n], in0=bk.t[:, :n], in1=h.t[:, dt_, c0:c0 + n], op=ALU.add),
                            r=[bk.s(), h.s(dt_, dt_ + 1)], w=[h.s(dt_, dt_ + 1)])

        def moe(blk):
            last = blk == NB - 1
            gT = small("gT", [8, TB])
            rt = small("rt", [128, 48])
            RT = [rt.s()]
            tch = [(c * 128, 128) for c in range(4)] + ([(TBP, NS)] if last else [])
            for (c0, n) in tch:
                bk = k.bank()
                for kt in range(16):
                    k.M(lambda e, kt=kt, c0=c0, n=n, bk=bk: e.matmul(bk.t[0:n, 0:8], lhsT=xn.t[:, kt, c0:c0 + n], rhs=wrt.t[:, kt * 8:kt * 8 + 8], start=(kt == 0), stop=(kt == 15)),
                        r=[xn.s(kt, kt + 1), wrt.s()], w=[bk.s()])
                R = (lambda n: (lambda a, b: rt.t[0:n, a:b]))(n)
                VR = lambda fn, extra=(): k.V(fn, r=RT + list(extra), w=RT)
                VR(lambda e, bk=bk, n=n, R=R: e.tensor_tensor(out=R(0, 8), in0=bk.t[0:n, 0:8], in1=lpt.t[0:n, LP_BRT:LP_BRT + 8], op=ALU.add), [bk.s(), lpt.s()])
                VR(lambda e, R=R: e.tensor_reduce(out=R(40, 41), in_=R(0, 8), axis=AX.X, op=ALU.max))
                VR(lambda e, R=R: e.tensor_scalar(out=R(8, 16), in0=R(0, 8), scalar1=R(40, 41), scalar2=None, op0=ALU.is_equal))
                VR(lambda e, R=R: e.scalar_tensor_tensor(out=R(16, 24), in0=R(8, 16), scalar=-1e30, in1=R(0, 8), op0=ALU.mult, op1=ALU.add))
                VR(lambda e, R=R: e.tensor_reduce(out=R(41, 42), in_=R(16, 24), axis=AX.X, op=ALU.max))
                VR(lambda e, R=R: e.tensor_scalar(out=R(24, 32), in0=R(16, 24), scalar1=R(41, 42), scalar2=None, op0=ALU.is_equal))
                VR(lambda e, R=R: e.tensor_tensor(out=R(42, 43), in0=R(41, 42), in1=R(40, 41), op=ALU.subtract))
                k.A(lambda e, R=R: e.activation(out=R(43, 44), in_=R(42, 43), func=AF.Sigmoid, scale=-1.0), r=RT, w=RT)
                VR(lambda e, R=R: e.tensor_scalar(out=R(44, 45), in0=R(43, 44), scalar1=-1.0, scalar2=1.0, op0=ALU.mult, op1=ALU.add))
                VR(lambda e, R=R: e.tensor_scalar(out=R(32, 40), in0=R(8, 16), scalar1=R(43, 44), scalar2=None, op0=ALU.mult))
                VR(lambda e, R=R: e.scalar_tensor_tensor(out=R(32, 40), in0=R(24, 32), scalar=R(44, 45), in1=R(32, 40), op0=ALU.mult, op1=ALU.add))
                b2 = k.bank()
                k.M(lambda e, b2=b2, n=n, R=R: e.matmul(b2.t[0:8, 0:n], lhsT=R(32, 40), rhs=cst.t[0:n, C_ID:C_ID + n], start=True, stop=True), r=RT + [cst.s()], w=[b2.s()])
                k.V(lambda e, b2=b2, n=n, c0=c0: e.tensor_copy(out=gT.t[:, c0:c0 + n], in_=b2.t[0:8, 0:n]), r=[b2.s()], w=[gT.s()])
            sel = small("sel", [8, 128])
            for ex in range(8):
                k.V(lambda e, ex=ex: e.tensor_scalar(out=sel.t[:, :], in0=ones_f.t[0:8, :], scalar1=cst.t[0:8, C_ID + ex:C_ID + ex + 1], scalar2=None, op0=ALU.mult),
                    r=[ones_f.s(), cst.s()], w=[sel.s()])
                for (c0, n) in chunks(blk):
                    bk = k.bank()
                    k.M(lambda e, ex=ex, c0=c0, n=n, bk=bk: e.matmul(bk.t[:, :n], lhsT=sel.t[:, :], rhs=gT.t[:, c0:c0 + n], start=True, stop=True), r=[sel.s(), gT.s()], w=[bk.s()])
                    k.A(lambda e, ex=ex, c0=c0, n=n, bk=bk: e.activation(out=CSf[ex].t[:, c0:c0 + n], in_=bk.t[:, :n], func=AF.Copy), r=[bk.s()], w=[CSf[ex].s()])
            for ex in range(8):
                ffn(blk, moeg.t[ex], moeu.t[ex], moed.t[ex], gate_tiles=CSf[ex], parity0=ex)

        def ple(blk, l):
            last = blk == NB - 1
            pv = pTp.t[l].rearrange("(kt p) t -> p kt t", p=128)
            pbv = [SB[2], SB[3]]
            for kt in range(2):
                k.Wm(lambda e, kt=kt: e.dma_start(out=pbv[kt].t[:, 0:TBP], in_=pv[:, kt, blk * TBP:(blk + 1) * TBP]), w=[pbv[kt].s()])
                if last:
                    psv = pTs.t[l].rearrange("(kt p) t -> p kt t", p=128)
                    k.Wm(lambda e, kt=kt, psv=psv: e.dma_start(out=pbv[kt].t[:, TBP:TB], in_=psv[:, kt, :]), w=[pbv[kt].s()])
            rms(blk, 4 + l, xn)
            wple = [None, None]
            for mt in range(16):
                if mt % 8 == 0:
                    if mt == 8:
                        k.unpin(wple[0])
                    wple[mt // 8] = k.wload(w_ple_t.t[l, mt // 8], pin=True)
                wp = k.wload(w_pg_t.t[l, mt])
                for (c0, n) in chunks(blk):
                    bg = k.bank(); bp = k.bank()
                    for kt in range(16):
                        k.M(lambda e, wp=wp, kt=kt, c0=c0, n=n, bg=bg: e.matmul(bg.t[:, :n], lhsT=wp.t[:, kt * 128:(kt + 1) * 128], rhs=xn.t[:, kt, c0:c0 + n], start=(kt == 0), stop=(kt == 15)),
                            r=[wp.s(), xn.s(kt, kt + 1)], w=[bg.s()])
                    wsl_ = wple[mt // 8]
                    for kt in range(2):
                        off = ((mt % 8) * 2 + kt) * 128
                        k.M(lambda e, wsl_=wsl_, off=off, kt=kt, c0=c0, n=n, bp=bp: e.matmul(bp.t[:, :n], lhsT=wsl_.t[:, off:off + 128], rhs=pbv[kt].t[:, c0:c0 + n], start=(kt == 0), stop=(kt == 1)),
                            r=[wsl_.s(), pbv[kt].s()], w=[bp.s()])
                    sg = SC[1 + mt % 2]
                    k.A(lambda e, n=n, bg=bg, sg=sg: e.activation(out=sg.t[:, :n], in_=bg.t[:, :n], func=AF.Sigmoid), r=[bg.s()], w=[sg.s()])
                    k.V(lambda e, n=n, bp=bp, sg=sg: e.tensor_tensor(out=sg.t[:, :n], in0=bp.t[:, :n], in1=sg.t[:, :n], op=ALU.mult), r=[bp.s(), sg.s()], w=[sg.s()])
                    k.V(lambda e, n=n, c0=c0, sg=sg, mt=mt: e.tensor_tensor(out=h.t[:, mt, c0:c0 + n], in0=sg.t[:, :n], in1=h.t[:, mt, c0:c0 + n], op=ALU.add),
                        r=[sg.s(), h.s(mt, mt + 1)], w=[h.s(mt, mt + 1)])
            k.unpin(wple[1])

        xv = xTp.t.rearrange("(kt p) t -> p kt t", p=128)
        xsv = xTs.t.rearrange("(kt p) t -> p kt t", p=128)
        yv = o_yTp.t.rearrange("(kt p) t -> p kt t", p=128)
        ysv = o_yTs.t.rearrange("(kt p) t -> p kt t", p=128)
        stop = False
        for blk in range(NB):
            last = blk == NB - 1
            k.Dm(lambda e, blk=blk: e.dma_start(out=h.t[:, :, 0:TBP], in_=xv[:, :, blk * TBP:(blk + 1) * TBP]), w=[h.s()])
            if last:
                k.Dm(lambda e: e.dma_start(out=h.t[:, :, TBP:TB], in_=xsv), w=[h.s()])
            for l in range(DEPTH):
                mixer(blk, l)
                if STOP_AFTER in (("mlstm", l), ("mixer", l)):
                    stop = True
                    break
                k.banks = rot7
                rms(blk, 2 + l, xn)
                if l % 2 == 0:
                    ffn(blk, ffg.t, ffu.t, ffd.t)
                else:
                    moe(blk)
                if STOP_AFTER == ("ffn", l):
                    stop = True
                    break
                ple(blk, l)
                k.banks = rot4
                if STOP_AFTER == ("layer", l):
                    stop = True
                    break

            def out32(kt, c0, n, rs, blk=blk):
                yo = SC[1 + kt % 2]
                k.V(lambda e, kt=kt, c0=c0, n=n, rs=rs, yo=yo: e.scalar_tensor_tensor(
                    out=yo.t[:, :n], in0=h.t[:, kt, c0:c0 + n], scalar=gn.t[:, 6 * 16 + kt:6 * 16 + kt + 1], in1=rs.t[:, :n], op0=ALU.mult, op1=ALU.mult),
                    r=[h.s(kt, kt + 1), rs.s(), gn.s()], w=[yo.s()])
                if c0 == 0:
                    k.Dm(lambda e, kt=kt, yo=yo, blk=blk: e.dma_start(out=yv[:, kt, blk * TBP:(blk + 1) * TBP], in_=yo.t[:, 0:TBP]), r=[yo.s()], w=[o_yTp.s(blk * 16 + kt, blk * 16 + kt + 1)])
                else:
                    k.Dm(lambda e, kt=kt, yo=yo: e.dma_start(out=ysv[:, kt, :], in_=yo.t[:, 0:NS]), r=[yo.s()], w=[o_yTs.s(kt, kt + 1)])
            rms(blk, 6, None, out32=out32)
        k.P.op("sp", None, reads=[o.s() for o in all_outs])
        k.P.emit(nc)
        print("instr counts:", {e: len(v) for e, v in k.P.q.items()})
    return nc


def _fm(v):
    return np.ascontiguousarray(v.reshape(-1, 128).T)


def _wt(W, cols=None):
    Wc = W if cols is None else W[:, cols]
    K = Wc.shape[0]
    nt = Wc.shape[1] // 128
    return np.ascontiguousarray(Wc.reshape(K // 128, 128, nt, 128).transpose(2, 1, 0, 3).reshape(nt, 128, K))


def _pack(T, g):
    nt, _, K = T.shape
    return np.ascontiguousarray(T.reshape(nt // g, g, 128, K).transpose(0, 2, 1, 3).reshape(nt // g, 128, g * K))


def _consts():
    c = np.zeros((128, NCONST), np.float32)
    c[:, C_ID:C_ID + 128] = np.eye(128)
    c[:, C_TRI:C_TRI + 128] = np.triu(np.ones((128, 128)))
    c[:, C_JJ:C_JJ + 512] = np.arange(1, 513)[None, :]
    g8 = np.arange(128) // 16
    c[:, C_M2:C_M2 + 8] = (g8[:, None] == np.arange(8)[None, :])
    c[:, C_D16:C_D16 + 256] = np.eye(16).reshape(1, 256)
    return c


def prep_shared(inp):
    f32 = np.float32
    L = DEPTH
    sh = {}
    sh["gains"] = np.concatenate([_fm(inp["g_mix"][0]), _fm(inp["g_mix"][1]), _fm(inp["g_ffn"][0]), _fm(inp["g_ffn"][1]),
                                  _fm(inp["g_ple"][0]), _fm(inp["g_ple"][1]), _fm(inp["g_final"])], axis=1).astype(f32)
    sh["consts"] = _consts()
    lp = np.zeros((L, 128, NLP), f32)
    for l in range(L):
        lp[l, :, LP_CW:LP_CW + 32] = inp["conv_w"][l].reshape(4, 8, 128).transpose(2, 1, 0).reshape(128, 32)
        lp[l, :, LP_CB:LP_CB + 8] = _fm(inp["conv_b"][l])
        lp[l, :, LP_SKIP:LP_SKIP + 8] = _fm(inp["skip_a"][l])
        lp[l, :, LP_DSK:LP_DSK + 8] = _fm(inp["s5_D"][l])
        lp[l, :, LP_BIF:LP_BIF + 4] = inp["b_i"][l][None, :]
        lp[l, :, LP_BIF + 4:LP_BIF + 8] = inp["b_f"][l][None, :]
        lp[l, :, LP_BRT:LP_BRT + 8] = inp["b_router"][0][None, :]
    sh["lp"] = lp
    sh["ghd"] = np.ascontiguousarray(np.broadcast_to(inp["g_head"].reshape(L, 1, 1024), (L, 128, 1024))).astype(f32)
    cols = np.concatenate([np.arange(0, 1024), np.arange(1024, 2048), np.arange(2056, 3080), np.arange(3080, 5128), np.arange(5128, 7176)])
    sh["w_in_t"] = np.stack([_wt(inp["w_in"][l], cols) for l in range(L)])
    sh["w_if"] = np.stack([inp["w_in"][l][:, 2048:2056].reshape(16, 128, 8).transpose(1, 0, 2).reshape(128, 128) for l in range(L)]).astype(f32)
    sh["wqkv"] = np.stack([np.stack([inp[nm][l].reshape(4, 2, 128, 256).transpose(2, 0, 1, 3).reshape(128, 2048) for nm in ("w_q", "w_k", "w_v")]) for l in range(L)]).astype(f32)
    sh["w_proj_t"] = np.stack([_pack(_wt(inp["w_proj_a"][l]), 2) for l in range(L)])
    sh["w_glu_t"] = np.stack([_pack(_wt(inp["w_glu_b"][l]), 2) for l in range(L)])
    sh["w_out_t"] = np.stack([_wt(inp["w_out"][l]) for l in range(L)])
    sh["w_pg_t"] = np.stack([_wt(inp["w_pg"][l]) for l in range(L)])
    sh["w_ple_t"] = np.stack([_pack(_wt(inp["w_ple"][l]), 8) for l in range(L)])
    sh["ffg"] = _wt(inp["w_ff_gate"][0]); sh["ffu"] = _wt(inp["w_ff_up"][0])
    sh["ffd"] = np.ascontiguousarray(inp["w_ff_down"][0].reshape(DFF_T, 128, 2048))
    sh["moeg"] = np.stack([_wt(inp["w_moe_gate"][0, e]) for e in range(8)])
    sh["moeu"] = np.stack([_wt(inp["w_moe_up"][0, e]) for e in range(8)])
    sh["moed"] = np.ascontiguousarray(inp["w_moe_down"][0].reshape(8, DFF_T, 128, 2048))
    sh["w_rt"] = np.ascontiguousarray(inp["w_router"][0].reshape(16, 128, 8).transpose(1, 0, 2).reshape(128, 128)).astype(f32)

    def L1(a):
        return a.reshape(32, 2, 64).transpose(1, 2, 0).reshape(128, 32)

    def L2(a):
        t = a.reshape(8, 8, 64).transpose(1, 0, 2)
        return np.broadcast_to(t[:, None, :, :], (8, 16, 8, 64)).reshape(128, 512)
    s5L1 = np.zeros((L, 128, 96), f32); s5L2 = np.zeros((L, 128, 1536), f32)
    s5B2 = np.zeros((L, 128, 1024), f32); s5C1 = np.zeros((L, 128, 1024), f32)
    for l in range(L):
        ldt = np.broadcast_to(inp["s5_log_dt"][l][:, None], (64, 64))
        for q, a in enumerate((inp["s5_A_re"][l], inp["s5_A_im"][l], ldt)):
            s5L1[l, :, q * 32:(q + 1) * 32] = L1(a)
            s5L2[l, :, q * 512:(q + 1) * 512] = L2(a)
        for q, b in enumerate((inp["s5_B_re"][l], inp["s5_B_im"][l])):
            s5B2[l, :, q * 512:(q + 1) * 512] = b.reshape(8, 8, 64, 16).transpose(1, 3, 0, 2).reshape(128, 512)
        for q, c in enumerate((inp["s5_C_re"][l], inp["s5_C_im"][l])):
            s5C1[l, :, q * 512:(q + 1) * 512] = c.reshape(32, 2, 16, 64).transpose(1, 3, 0, 2).reshape(128, 512)
    sh["s5L1"], sh["s5L2"], sh["s5B2"], sh["s5C1"] = s5L1, s5L2, s5B2, s5C1
    return {k_: np.ascontiguousarray(v, dtype=f32) for k_, v in sh.items()}


def kernel(**inp):
    f32 = np.float32
    L = DEPTH
    sh = prep_shared(inp)
    nc = build_program()
    in_maps = []
    cores = list(range(NCORES)) if RUN_CORES is None else list(RUN_CORES)
    for c in cores:
        sq = PROMPT_CORES.get(c)
        smp = slice(c * NS, (c + 1) * NS)
        m = dict(sh)
        m["xTp"] = np.ascontiguousarray(inp["x_prompt"][sq].T) if sq is not None else np.zeros((D, SEQ), f32)
        m["xTs"] = np.ascontiguousarray(inp["x_sample"][smp, 0, :].T)
        m["pTp"] = np.ascontiguousarray(inp["p_prompt"][:, sq].transpose(0, 2, 1)) if sq is not None else np.zeros((L, 256, SEQ), f32)
        m["pTs"] = np.ascontiguousarray(inp["p_sample"][:, smp, 0, :].transpose(0, 2, 1))
        m["stC"] = np.ascontiguousarray(inp["state_mlstm_C"][:, smp])
        m["stn"] = np.ascontiguousarray(inp["state_mlstm_n"][:, smp].reshape(L, NS, 4, 2, 128).transpose(0, 4, 1, 2, 3).reshape(L, 128, 128))
        m["stm"] = np.ascontiguousarray(inp["state_mlstm_m"][:, smp])
        m["stconv"] = np.ascontiguousarray(inp["state_mlstm_conv"][:, smp].reshape(L, NS, 3, 8, 128).transpose(0, 4, 3, 1, 2).reshape(L, 128, 8 * NS * 3))
        s5 = np.stack([inp["state_s5_re"][:, smp], inp["state_s5_im"][:, smp]], axis=1)
        m["sts5"] = np.ascontiguousarray(s5.reshape(L, 2, NS, 32, 2, 64).transpose(0, 1, 4, 5, 3, 2).reshape(L, 2, 128, 512))
        in_maps.append({k_: np.ascontiguousarray(v, dtype=f32) for k_, v in m.items()})
    res = run_bass_kernel_spmd(nc, in_maps, core_ids=list(range(len(cores))))
    R = res.results
    B, S = 4, SEQ
    y_p = np.zeros((B, S, D), f32); y_s = np.zeros((128, 1, D), f32)
    C_p = np.zeros((L, B, 4, 256, 256), f32); n_p = np.zeros((L, B, 4, 256), f32); m_p = np.zeros((L, B, 4), f32)
    conv_p = np.zeros((L, B, 3, D_A), f32); s5re_p = np.zeros((L, B, 64, 64), f32); s5im_p = np.zeros((L, B, 64, 64), f32)
    C_s = np.zeros((L, 128, 4, 256, 256), f32); n_s = np.zeros((L, 128, 4, 256), f32); m_s = np.zeros((L, 128, 4), f32)
    conv_s = np.zeros((L, 128, 3, D_A), f32); s5re_s = np.zeros((L, 128, 64, 64), f32); s5im_s = np.zeros((L, 128, 64, 64), f32)
    for ci, c in enumerate(cores):
        r = R[ci]
        smp = slice(c * NS, (c + 1) * NS)
        sq = PROMPT_CORES.get(c)
        if sq is not None:
            y_p[sq] = r["o_yTp"].T
            C_p[:, sq] = r["o_Cp"]
            n_p[:, sq] = r["o_np"].reshape(L, 128, 2, 4).transpose(0, 3, 2, 1).reshape(L, 4, 256)
            m_p[:, sq] = r["o_mp"]
            conv_p[:, sq] = r["o_convp"].transpose(0, 2, 1)
            sp = r["o_s5p"].reshape(L, 2, 2, 64, 32).transpose(0, 1, 4, 2, 3).reshape(L, 2, 64, 64)
            s5re_p[:, sq] = sp[:, 0]; s5im_p[:, sq] = sp[:, 1]
        y_s[smp, 0, :] = r["o_yTs"].T
        C_s[:, smp] = r["o_Cs"]
        n_s[:, smp] = r["o_ns"].reshape(L, 128, NS, 4, 2).transpose(0, 2, 3, 4, 1).reshape(L, NS, 4, 256)
        m_s[:, smp] = r["o_ms"]
        conv_s[:, smp] = r["o_convs"].reshape(L, 128, 8, NS, 3).transpose(0, 3, 4, 2, 1).reshape(L, NS, 3, D_A)
        ss = r["o_s5s"].reshape(L, 2, 2, 64, 32, NS).transpose(0, 1, 5, 4, 2, 3).reshape(L, 2, NS, 64, 64)
        s5re_s[:, smp] = ss[:, 0]; s5im_s[:, smp] = ss[:, 1]
    return (y_p, y_s, C_p, n_p, m_p, conv_p, s5re_p, s5im_p, C_s, n_s, m_s, conv_s, s5re_s, s5im_s)
```
